# Optimizing a Trainium2 kernel written in Bass

```python
import math
import jax
import jax.numpy as jnp
from jax import lax
import numpy as np

D_MODEL = 1024
BATCH = 16
SEQ = 2048
DEPTH = 2

GRID_W = 64
CTX_LEN = 256

D_HY = 512
HY_ORDER = 2
HY_SHORT = 3
HY_BANDS = 16
HY_EMB = 1 + 2 * HY_BANDS
HY_HID = 64
HY_COLS = (HY_ORDER + 1) * D_HY
HY_FILT_COLS = HY_ORDER * 2 * D_HY
HY_DECAY_MIN = math.log(1e2) / 1.5
HY_DECAY_MAX = math.log(1e2) / 0.3

D_CF = 512
CF_KERNEL = 31
CF_COLS = 2 * D_CF

D_S5 = 512
S5_GROUP = 16
S5_GROUPS = D_S5 // S5_GROUP
S5_STATE = 64
S5_MAX_RE = -1e-4

N_BRANCH = 3
CF_OFF = HY_COLS
S5_OFF = CF_OFF + CF_COLS
GATE_OFF = S5_OFF + D_S5
IN_COLS = GATE_OFF + N_BRANCH * D_MODEL

D_FF = 2816
N_EXPERTS = 8
TOP_K = 2
D_EXPERT = 3584
N_DENSE = (DEPTH + 1) // 2
N_MOE = DEPTH // 2

ALPHA = (2.0 * DEPTH) ** 0.25
BETA = (8.0 * DEPTH) ** -0.25
LN_EPS = 1e-5

kernel_name = "hybrid_hyena_conformer_s5_moe_dit"


def layer_norm(x, g=None, b=None):
    xf = x.astype(jnp.float32)
    mu = jnp.mean(xf, axis=-1, keepdims=True)
    var = jnp.mean(jnp.square(xf - mu), axis=-1, keepdims=True)
    y = ((xf - mu) * lax.rsqrt(var + LN_EPS)).astype(x.dtype)
    if g is not None:
        y = y * g + b
    return y


def modulate(x, shift, scale):
    return layer_norm(x) * (1 + scale) + shift


def dwconv(u, w, b):
    pad = (w.shape[0] - 1) // 2
    y = lax.conv_general_dilated(
        u, w[:, None, :].astype(u.dtype), window_strides=(1,), padding=[(pad, pad)],
        dimension_numbers=("NWC", "WIO", "NWC"), feature_group_count=u.shape[-1])
    return y + b


def hyena_filters(L, w1, b1, w2, b2, w3, b3, freq, decay):
    f32 = jnp.float32
    t = jnp.arange(L, dtype=f32)[:, None]
    tn = t / max(L - 1, 1)
    bands = jnp.arange(1, HY_BANDS + 1, dtype=f32)
    ang = t * bands * (2.0 * math.pi / L)
    z = jnp.concatenate([tn, jnp.cos(ang), jnp.sin(ang)], axis=-1)
    freq = freq.astype(f32)
    h = jnp.sin(freq[0] * (z @ w1.astype(f32) + b1.astype(f32)))
    h = jnp.sin(freq[1] * (h @ w2.astype(f32) + b2.astype(f32)))
    h = h @ w3.astype(f32) + b3.astype(f32)
    h = h * jnp.exp(-tn * jnp.abs(decay.astype(f32)))
    return h.reshape(L, HY_ORDER, 2, D_HY)


def two_sided_long_conv(u, h_fwd, h_bwd, bias):
    L = u.shape[1]
    n = 2 * L
    k = jnp.concatenate([h_fwd, jnp.zeros_like(h_fwd[:1]), h_bwd[1:][::-1]], axis=0)
    uf = u.astype(jnp.float32)
    y = jnp.fft.irfft(jnp.fft.rfft(uf, n=n, axis=1) * jnp.fft.rfft(k, n=n, axis=0)[None], n=n, axis=1)[:, :L]
    return (y + uf * bias.astype(jnp.float32)).astype(u.dtype)


def hyena_branch(p, short_w, short_b, f_w1, f_b1, f_w2, f_b2, f_w3, f_b3, freq, decay, bias, w_out):
    L = p.shape[1]
    parts = jnp.split(dwconv(p, short_w, short_b), HY_ORDER + 1, axis=-1)
    filt = hyena_filters(L, f_w1, f_b1, f_w2, f_b2, f_w3, f_b3, freq, decay)
    z = parts[-1]
    for o in range(HY_ORDER):
        z = parts[o] * two_sided_long_conv(z, filt[:, o, 0], filt[:, o, 1], bias[o])
    return z @ w_out


def conformer_branch(p, grid_rows, dw_w, dw_b, ln_g, ln_b, w_out):
    a, g = jnp.split(p, 2, axis=-1)
    u = a * jax.nn.sigmoid(g)
    bsz, L, C = u.shape
    if grid_rows is not None:
        u = dwconv(u.reshape(bsz * grid_rows, GRID_W, C), dw_w, dw_b).reshape(bsz, L, C)
    else:
        u = dwconv(u, dw_w, dw_b)
    u = jax.nn.silu(layer_norm(u, ln_g, ln_b))
    return u @ w_out


def _ssm_combine(left, right):
    a1r, a1i, b1r, b1i = left
    a2r, a2i, b2r, b2i = right
    return (a2r * a1r - a2i * a1i, a2r * a1i + a2i * a1r,
            a2r * b1r - a2i * b1i + b2r, a2r * b1i + a2i * b1r + b2i)


def s5_discretize(a_re, a_im, log_dt, b_re, b_im):
    f32 = jnp.float32
    lam_re = jnp.minimum(a_re.astype(f32), S5_MAX_RE)
    lam_im = a_im.astype(f32)
    dt = jnp.exp(log_dt.astype(f32))[:, None]
    mag = jnp.exp(lam_re * dt)
    ang = lam_im * dt
    abar_re = mag * jnp.cos(ang)
    abar_im = mag * jnp.sin(ang)
    den = lam_re * lam_re + lam_im * lam_im
    q_re = ((abar_re - 1.0) * lam_re + abar_im * lam_im) / den
    q_im = (abar_im * lam_re - (abar_re - 1.0) * lam_im) / den
    br = b_re.astype(f32)
    bi = b_im.astype(f32)
    bb_re = q_re[..., None] * br - q_im[..., None] * bi
    bb_im = q_re[..., None] * bi + q_im[..., None] * br
    return abar_re, abar_im, bb_re, bb_im


def s5_scans(u, a_re, a_im, log_dt, b_re, b_im, c_re, c_im, init, want_final):
    f32 = jnp.float32
    bsz, L, _ = u.shape
    ug = u.astype(f32).reshape(bsz, L, S5_GROUPS, S5_GROUP)
    ys, finals = [], []
    for d, reverse in enumerate((False, True)):
        abr, abi, bbr, bbi = s5_discretize(a_re[d], a_im[d], log_dt[d], b_re[d], b_im[d])
        bur = jnp.einsum("blgc,gpc->blgp", ug, bbr)
        bui = jnp.einsum("blgc,gpc->blgp", ug, bbi)
        if init is not None:
            s0r, s0i = init[d]
            first = L - 1 if reverse else 0
            bur = bur.at[:, first].add(abr * s0r - abi * s0i)
            bui = bui.at[:, first].add(abr * s0i + abi * s0r)
        ar = jnp.broadcast_to(abr, (1, L) + abr.shape)
        ai = jnp.broadcast_to(abi, (1, L) + abi.shape)
        _, _, sr, si = lax.associative_scan(_ssm_combine, (ar, ai, bur, bui), reverse=reverse, axis=1)
        ys.append(jnp.einsum("blgp,gcp->blgc", sr, c_re[d].astype(f32))
                  - jnp.einsum("blgp,gcp->blgc", si, c_im[d].astype(f32)))
        if want_final:
            last = 0 if reverse else L - 1
            finals.append((sr[:, last], si[:, last]))
    y = (ys[0] + ys[1]).reshape(bsz, L, D_S5)
    return y, finals


def s5_branch(p, s5_p, d_skip, w_glu, b_glu, w_out, init, want_final):
    y, finals = s5_scans(p, *s5_p, init, want_final)
    y = y.astype(p.dtype) + d_skip * p
    g = jax.nn.gelu(y)
    y = g * jax.nn.sigmoid(g @ w_glu + b_glu)
    return y @ w_out, finals


def token_mixer(p, grid_rows, hy_p, cf_p, s5_p, s5_out_p, w_o, init, want_final):
    y_hy = hyena_branch(p[..., :CF_OFF], *hy_p)
    y_cf = conformer_branch(p[..., CF_OFF:S5_OFF], grid_rows, *cf_p)
    y_s5, finals = s5_branch(p[..., S5_OFF:GATE_OFF], s5_p, *s5_out_p, init, want_final)
    g_hy, g_cf, g_s5 = jnp.split(jax.nn.sigmoid(p[..., GATE_OFF:]), N_BRANCH, axis=-1)
    return (g_hy * y_hy + g_cf * y_cf + g_s5 * y_s5) @ w_o, finals


def swiglu(h, w1, w3, w2):
    return (jax.nn.silu(h @ w1) * (h @ w3)) @ w2


def moe_swiglu(h, router, w1, w3, w2):
    logits = (h @ router).astype(jnp.float32)
    top_v, top_i = lax.top_k(logits, TOP_K)
    top_w = jax.nn.softmax(top_v, axis=-1)
    comb = jnp.einsum("blk,blke->ble", top_w,
                      jax.nn.one_hot(top_i, N_EXPERTS, dtype=jnp.float32)).astype(h.dtype)
    out = comb[..., 0:1] * swiglu(h, w1[0], w3[0], w2[0])
    for e in range(1, N_EXPERTS):
        out = out + comb[..., e:e + 1] * swiglu(h, w1[e], w3[e], w2[e])
    return out


def channel_mixer(h, layer, ffn_w1, ffn_w3, ffn_w2, moe_router, moe_w1, moe_w3, moe_w2):
    i = layer // 2
    if layer % 2 == 0:
        return swiglu(h, ffn_w1[i], ffn_w3[i], ffn_w2[i])
    return moe_swiglu(h, moe_router[i], moe_w1[i], moe_w3[i], moe_w2[i])


def setup_inputs(seed: int = 0) -> dict:
    key = jax.random.key(seed)
    keys = iter(jax.random.split(key, 64))
    f32 = jnp.float32

    def nrm(shape, scale):
        return jax.random.normal(next(keys), shape, f32) * scale

    def unif(shape, lo, hi):
        return jax.random.uniform(next(keys), shape, f32, lo, hi)

    D = D_MODEL
    G, P, K = S5_GROUPS, S5_STATE, S5_GROUP
    n_idx = jnp.arange(S5_STATE, dtype=f32)
    return {
        "x": nrm((BATCH, SEQ, D), 1.0),
        "c": nrm((BATCH, D), 1.0),
        "ctx": nrm((BATCH, CTX_LEN, D), 1.0),
        "c_ctx": nrm((D,), 1.0),
        "w_mod": nrm((DEPTH, D, 6 * D), 0.5 * D ** -0.5),
        "b_mod": nrm((DEPTH, 6 * D), 0.02),
        "w_in": nrm((DEPTH, D, IN_COLS), D ** -0.5),
        "hy_short_w": nrm((DEPTH, HY_SHORT, HY_COLS), HY_SHORT ** -0.5),
        "hy_short_b": nrm((DEPTH, HY_COLS), 0.02),
        "hy_f_w1": nrm((DEPTH, HY_EMB, HY_HID), HY_EMB ** -0.5),
        "hy_f_b1": nrm((DEPTH, HY_HID), 0.1),
        "hy_f_w2": nrm((DEPTH, HY_HID, HY_HID), HY_HID ** -0.5),
        "hy_f_b2": nrm((DEPTH, HY_HID), 0.1),
        "hy_f_w3": nrm((DEPTH, HY_HID, HY_FILT_COLS), 0.05 * HY_HID ** -0.5),
        "hy_f_b3": nrm((DEPTH, HY_FILT_COLS), 0.01),
        "hy_freq": 1.0 + nrm((DEPTH, 2, HY_HID), 0.01),
        "hy_decay": unif((DEPTH, HY_FILT_COLS), HY_DECAY_MIN, HY_DECAY_MAX),
        "hy_bias": nrm((DEPTH, HY_ORDER, D_HY), 0.5),
        "w_hy_out": nrm((DEPTH, D_HY, D), D_HY ** -0.5),
        "cf_dw_w": nrm((DEPTH, CF_KERNEL, D_CF), CF_KERNEL ** -0.5),
        "cf_dw_b": nrm((DEPTH, D_CF), 0.02),
        "cf_ln_g": 1.0 + nrm((DEPTH, D_CF), 0.02),
        "cf_ln_b": nrm((DEPTH, D_CF), 0.02),
        "w_cf_out": nrm((DEPTH, D_CF, D), D_CF ** -0.5),
        "s5_a_re": -0.5 + nrm((DEPTH, 2, G, P), 0.01),
        "s5_a_im": math.pi * n_idx + nrm((DEPTH, 2, G, P), 0.01),
        "s5_log_dt": unif((DEPTH, 2, G), math.log(1e-3), math.log(1e-1)),
        "s5_b_re": nrm((DEPTH, 2, G, P, K), K ** -0.5),
        "s5_b_im": nrm((DEPTH, 2, G, P, K), K ** -0.5),
        "s5_c_re": nrm((DEPTH, 2, G, K, P), P ** -0.5),
        "s5_c_im": nrm((DEPTH, 2, G, K, P), P ** -0.5),
        "s5_d": nrm((DEPTH, D_S5), 1.0),
        "s5_w_glu": nrm((DEPTH, D_S5, D_S5), D_S5 ** -0.5),
        "s5_b_glu": nrm((DEPTH, D_S5), 0.02),
        "w_s5_out": nrm((DEPTH, D_S5, D), D_S5 ** -0.5),
        "w_o": nrm((DEPTH, D, D), BETA * D ** -0.5),
        "ln1_g": 1.0 + nrm((DEPTH, D), 0.02),
        "ln1_b": nrm((DEPTH, D), 0.02),
        "ln2_g": 1.0 + nrm((DEPTH, D), 0.02),
        "ln2_b": nrm((DEPTH, D), 0.02),
        "ffn_w1": nrm((N_DENSE, D, D_FF), D ** -0.5),
        "ffn_w3": nrm((N_DENSE, D, D_FF), D ** -0.5),
        "ffn_w2": nrm((N_DENSE, D_FF, D), BETA * D_FF ** -0.5),
        "moe_router": nrm((N_MOE, D, N_EXPERTS), D ** -0.5),
        "moe_w1": nrm((N_MOE, N_EXPERTS, D, D_EXPERT), D ** -0.5),
        "moe_w3": nrm((N_MOE, N_EXPERTS, D, D_EXPERT), D ** -0.5),
        "moe_w2": nrm((N_MOE, N_EXPERTS, D_EXPERT, D), BETA * D_EXPERT ** -0.5),
    }


def reference(x, c, ctx, c_ctx, w_mod, b_mod, w_in,
              hy_short_w, hy_short_b, hy_f_w1, hy_f_b1, hy_f_w2, hy_f_b2, hy_f_w3, hy_f_b3,
              hy_freq, hy_decay, hy_bias, w_hy_out,
              cf_dw_w, cf_dw_b, cf_ln_g, cf_ln_b, w_cf_out,
              s5_a_re, s5_a_im, s5_log_dt, s5_b_re, s5_b_im, s5_c_re, s5_c_im,
              s5_d, s5_w_glu, s5_b_glu, w_s5_out,
              w_o, ln1_g, ln1_b, ln2_g, ln2_b,
              ffn_w1, ffn_w3, ffn_w2, moe_router, moe_w1, moe_w3, moe_w2):
    rows = x.shape[1] // GRID_W
    xc = ctx
    silu_c = jax.nn.silu(c)
    silu_cc = jax.nn.silu(c_ctx)
    ffn_p = (ffn_w1, ffn_w3, ffn_w2, moe_router, moe_w1, moe_w3, moe_w2)
    for l in range(DEPTH):
        last = l == DEPTH - 1
        sh1, sc1, g1, sh2, sc2, g2 = [m[:, None, :] for m in
                                      jnp.split(silu_c @ w_mod[l] + b_mod[l], 6, axis=-1)]
        csh1, csc1, cg1, csh2, csc2, cg2 = jnp.split(silu_cc @ w_mod[l] + b_mod[l], 6, axis=-1)
        hy_p = (hy_short_w[l], hy_short_b[l], hy_f_w1[l], hy_f_b1[l], hy_f_w2[l], hy_f_b2[l],
                hy_f_w3[l], hy_f_b3[l], hy_freq[l], hy_decay[l], hy_bias[l], w_hy_out[l])
        cf_p = (cf_dw_w[l], cf_dw_b[l], cf_ln_g[l], cf_ln_b[l], w_cf_out[l])
        s5_p = (s5_a_re[l], s5_a_im[l], s5_log_dt[l], s5_b_re[l], s5_b_im[l], s5_c_re[l], s5_c_im[l])
        s5_out_p = (s5_d[l], s5_w_glu[l], s5_b_glu[l], w_s5_out[l])

        hc = modulate(xc, csh1, csc1)
        if last:
            _, finals = s5_scans(hc @ w_in[l][:, S5_OFF:GATE_OFF], *s5_p, None, True)
        else:
            yc, finals = token_mixer(hc @ w_in[l], None, hy_p, cf_p, s5_p, s5_out_p, w_o[l], None, True)
            xc = layer_norm(ALPHA * xc + cg1 * yc, ln1_g[l], ln1_b[l])
            fc = channel_mixer(modulate(xc, csh2, csc2), l, *ffn_p)
            xc = layer_norm(ALPHA * xc + cg2 * fc, ln2_g[l], ln2_b[l])

        hx = modulate(x, sh1, sc1)
        yx, _ = token_mixer(hx @ w_in[l], rows, hy_p, cf_p, s5_p, s5_out_p, w_o[l], finals, False)
        x = layer_norm(ALPHA * x + g1 * yx, ln1_g[l], ln1_b[l])
        fx = channel_mixer(modulate(x, sh2, sc2), l, *ffn_p)
        x = layer_norm(ALPHA * x + g2 * fx, ln2_g[l], ln2_b[l])
    return x
```

```python
import math
from contextlib import ExitStack
import numpy as np
import concourse.bass as bass
import concourse.mybir as mybir
from concourse.bass_utils import run_bass_kernel_spmd

F32 = mybir.dt.float32
BF16 = mybir.dt.bfloat16
I32 = mybir.dt.int32
ALU = mybir.AluOpType
AF = mybir.ActivationFunctionType
AX = mybir.AxisListType

D = 1024
L = 2048
LC = 256
NB = 2
DEPTH = 2
IN_COLS = 6144
D_FF = 2816
D_EXP = 3584
NEXP = 8
ALPHA = (2.0 * DEPTH) ** 0.25
LN_EPS = 1e-5
TWO_PI = 2.0 * math.pi

ENGS = ("pe", "dve", "act", "pool", "sp")


class Res:
    __slots__ = ("w", "r", "sem", "epoch", "pr")

    def __init__(self):
        self.w = None
        self.r = {}
        self.sem = None
        self.epoch = -1
        self.pr = {}


class Prog:
    def __init__(self, nc):
        self.nc = nc
        self.ops = {e: [] for e in ENGS}
        self.cnt = {}
        self.waited = {e: {} for e in ENGS}
        self.semkeys = list(ENGS)

    def new_dma_sem(self):
        k = "dma%d" % (len(self.semkeys) - len(ENGS))
        self.semkeys.append(k)
        return k

    def op(self, eng, fn, reads=(), writes=(), dma_sem=None):
        need = {}
        rawself = 0
        for r in reads:
            if r.w is not None:
                k, v = r.w
                if need.get(k, 0) < v:
                    need[k] = v
                if k == eng and v > rawself:
                    rawself = v
        for w in writes:
            if w.w is not None:
                k, v = w.w
                if dma_sem is not None and k == dma_sem:
                    for k2, v2 in w.pr.items():
                        if need.get(k2, 0) < v2:
                            need[k2] = v2
                elif need.get(k, 0) < v:
                    need[k] = v
            for k, v in w.r.items():
                if need.get(k, 0) < v:
                    need[k] = v
        key = eng if dma_sem is None else dma_sem
        amt = 1 if dma_sem is None else 16
        waits = []
        wd = self.waited[eng]
        for k, v in need.items():
            if k == eng and dma_sem is None:
                if eng == "pe" or rawself == 0:
                    continue
                v = rawself
            if wd.get(k, 0) < v:
                waits.append((k, v))
                wd[k] = v
        val = self.cnt.get(key, 0) + amt
        self.cnt[key] = val
        self.ops[eng].append((waits, fn, key, amt))
        for r in reads:
            if r.r.get(key, 0) < val:
                r.r[key] = val
        for w in writes:
            same_group = dma_sem is not None and w.w is not None and w.w[0] == dma_sem
            npr = dict(w.r)
            if same_group:
                for k2, v2 in w.pr.items():
                    if npr.get(k2, 0) < v2:
                        npr[k2] = v2
            elif w.w is not None:
                if npr.get(w.w[0], 0) < w.w[1]:
                    npr[w.w[0]] = w.w[1]
            w.pr = npr
            w.w = (key, val)
            w.r = {}
        return val

    def wait_all(self, eng, res_list):
        need = {}
        for r in res_list:
            if r.w is not None:
                need[r.w[0]] = max(need.get(r.w[0], 0), r.w[1])
            for k, v in r.r.items():
                need[k] = max(need.get(k, 0), v)
        self.ops[eng].append((list(need.items()), None, None, 0))

    def emit(self, stack):
        nc = self.nc
        sems = {}
        for k in self.semkeys:
            sems[k] = stack.enter_context(nc.semaphore("s_" + k))
        stack.enter_context(nc.allow_non_contiguous_dma(reason="small strided parameter loads"))
        block = stack.enter_context(nc.Block())
        ops = self.ops

        targets = {k: set() for k in ENGS}
        for en in ENGS:
            for waits, fn, key, amt in ops[en]:
                for k, v in waits:
                    if k in targets:
                        targets[k].add(v)
        remap = {}
        for en in ENGS:
            pos = 0
            c = 0
            m = {}
            for waits, fn, key, amt in ops[en]:
                if fn is None or key != en:
                    continue
                pos += 1
                if pos in targets[en]:
                    c += 1
                    m[pos] = c
            remap[en] = m

        def run(name, e):
            pos = 0
            for waits, fn, key, amt in ops[name]:
                for k, v in waits:
                    e.wait_ge(sems[k], remap[k][v] if k in remap else v)
                if fn is not None:
                    ins = fn(e)
                    if key == name:
                        pos += 1
                        if pos in targets[name]:
                            ins.then_inc(sems[key], 1)
                    else:
                        ins.then_inc(sems[key], amt)

        @block.tensor
        def _(e):
            run("pe", e)

        @block.vector
        def _(e):
            run("dve", e)

        @block.scalar
        def _(e):
            run("act", e)

        @block.gpsimd
        def _(e):
            run("pool", e)

        @block.sync
        def _(e):
            run("sp", e)


class Buf:
    def __init__(self, t):
        self.t = t
        self._r = {}

    def res(self, key=None):
        r = self._r.get(key)
        if r is None:
            r = self._r[key] = Res()
        return r

    def __getitem__(self, idx):
        return self.t[idx]


class View(Buf):
    pass


INPUT_NAMES = ["x", "c", "ctx", "c_ctx", "w_mod", "b_mod", "w_in",
               "hy_short_w", "hy_short_b", "hy_f_w1", "hy_f_b1", "hy_f_w2", "hy_f_b2", "hy_f_w3", "hy_f_b3",
               "hy_freq", "hy_decay", "hy_bias", "w_hy_out",
               "cf_dw_w", "cf_dw_b", "cf_ln_g", "cf_ln_b", "w_cf_out",
               "s5_a_re", "s5_a_im", "s5_log_dt", "s5_b_re", "s5_b_im", "s5_c_re", "s5_c_im",
               "s5_d", "s5_w_glu", "s5_b_glu", "w_s5_out",
               "w_o", "ln1_g", "ln1_b", "ln2_g", "ln2_b",
               "ffn_w1", "ffn_w3", "ffn_w2", "moe_router", "moe_w1", "moe_w3", "moe_w2"]


class K:
    def __init__(self, shapes, debug=None):
        self.debug = debug or {}
        nc = self.nc = bass.Bass("TRN2", target_bir_lowering=False)
        self.P = Prog(nc)
        self.st = ExitStack()
        self.din = {}
        for n in INPUT_NAMES:
            self.din[n] = nc.dram_tensor(n, list(shapes[n]), F32, kind="ExternalInput").ap()
        self.out = nc.dram_tensor("out", [NB, L, D], F32, kind="ExternalOutput").ap()
        self.hc = {"L": {}, "C": {}}
        for name, arr in _consts().items():
            _, tag, k_ = name.split("_", 2)
            dt_ = F32 if arr.dtype == np.float32 else BF16
            self.hc[tag][k_] = nc.dram_tensor(name, list(arr.shape), dt_, kind="ExternalInput").ap()
        self.dbg_out = {}
        for n, shp in self.debug.items():
            if n.startswith("_"):
                continue
            self.dbg_out[n] = nc.dram_tensor("dbg_" + n, list(shp), F32, kind="ExternalOutput").ap()
        self.out_sem = self.P.new_dma_sem()
        self.out_sem_sw = self.P.new_dma_sem()
        self.out_res = Res()
        self.out_res_sw = Res()
        self.scr_sem = self.P.new_dma_sem()
        self.sem_pool = {"hw": [self.P.new_dma_sem() for _ in range(44)],
                         "sw": [self.P.new_dma_sem() for _ in range(40)]}
        self.sem_next = {"hw": 0, "sw": 0}
        self.epoch = 0

    def sb(self, name, shape, dt):
        return Buf(self.st.enter_context(self.nc.sbuf_tensor(name, list(shape), dt)))

    def ps(self, name, shape, dt):
        return Buf(self.st.enter_context(self.nc.psum_tensor(name, list(shape), dt)))

    def view(self, off, shape, dt):
        esz = 4 if dt in (F32, I32) else 2
        n = 1
        for d_ in shape[1:]:
            n *= d_
        nbytes = n * esz
        assert off % 4 == 0 and off + nbytes <= self.ARENA_BYTES, (off, nbytes, self.ARENA_BYTES)
        self.arena_hi = max(getattr(self, "arena_hi", 0), off + nbytes)
        ap = self.arena.t[0:shape[0], off // 4:(off + nbytes + 3) // 4]
        if dt != F32:
            ap = ap.bitcast(dt)
        if len(shape) > 2:
            names = "abcdefg"[:len(shape) - 1]
            kw = {names[i]: shape[1 + i] for i in range(len(shape) - 2)}
            ap = ap.rearrange("p (%s) -> p %s" % (" ".join(names), " ".join(names)), **kw)
        return View(ap)

    def mark(self, name):
        if not hasattr(self, "marks"):
            self.marks = []
        self.marks.append((name, sum(1 for o_ in self.P.ops["pe"] if o_[1] is not None)))

    def barrier(self):
        cur = dict(self.P.cnt)
        for e in ENGS:
            waits = []
            for k, v in cur.items():
                if k == e and e == "pe":
                    continue
                if self.P.waited[e].get(k, 0) < v:
                    waits.append((k, v))
                    self.P.waited[e][k] = v
            if waits:
                self.P.ops[e].append((waits, None, None, 0))
        self.epoch += 1
        self.sem_next = {"hw": 0, "sw": 0}

    def op(self, eng, fn, reads=(), writes=(), dma_sem=None):
        return self.P.op(eng, fn, reads, writes, dma_sem)

    def dma(self, eng, out, in_, reads=(), writes=(), sem=None, **kw):
        if sem is None:
            w = writes[0]
            kind = "sw" if eng == "pool" else "hw"
            if w.sem is None or w.epoch != (self.epoch, kind):
                assert self.sem_next[kind] < len(self.sem_pool[kind]), "out of DMA semaphores in this phase"
                w.sem = self.sem_pool[kind][self.sem_next[kind]]
                self.sem_next[kind] += 1
                w.epoch = (self.epoch, kind)
            sem = w.sem
        return self.P.op(eng, lambda e: e.dma_start(out=out, in_=in_, **kw), reads, writes, dma_sem=sem)

    def dbg(self, name, src_ap, res):
        if name in self.dbg_out:
            self.dma("sp", self.dbg_out[name], src_ap, reads=[res], writes=[Res()])

    def build(self):
        nc = self.nc
        P = self.P
        din = self.din
        self.ident_f = self.sb("ident_f", [128, 128], F32)
        self.ident_b = self.sb("ident_b", [128, 128], BF16)
        self.ones_f = self.sb("ones_f", [128, 128], F32)
        rI = self.ident_f.res()
        self.op("pool", lambda e: e.memset(self.ident_f[:], 0.0), writes=[rI])
        self.op("pool", lambda e: e.affine_select(self.ident_f[:], self.ident_f[:], pattern=[[-1, 128]],
                                                  compare_op=ALU.not_equal, fill=1.0, base=0, channel_multiplier=1),
                reads=[rI], writes=[rI])
        self.op("pool", lambda e: e.tensor_copy(self.ident_b[:], self.ident_f[:]), reads=[rI],
                writes=[self.ident_b.res()])
        self.op("pool", lambda e: e.memset(self.ones_f[:], 1.0 / D), writes=[self.ones_f.res()])
        self.psb = [self.ps("psb%d" % i, [128, 512], F32) for i in range(8)]
        self._psi = 0
        self.XT = self.sb("XT", [128, 8, L], F32)
        self.ARENA_BYTES = 102 * 1024
        self.arena = self.sb("arena", [128, self.ARENA_BYTES // 4], F32)

        self.ones_cf = self.sb("ones_cf", [128, 128], F32)
        self.op("pool", lambda e: e.memset(self.ones_cf[:], 1.0 / 512), writes=[self.ones_cf.res()])
        self.HT = self.sb("HT", [128, 8, L + LC], BF16)
        self.s5fin = self.sb("s5fin", [128, 2, 2, 16], F32)
        stop = self.debug.get("_stop", "")
        self.compute_mods()
        for l in range(DEPTH):
            self.s5_prologue(l)
            self.hyena_prologue(l, L, "L")
        self.hyena_prologue(0, LC, "C")
        XC = self.view(self.ARENA_BYTES - 8192, [128, 8, LC], F32)
        for bi in range(NB):
            self.load_tokens(din["ctx"][bi], LC, XC)
            self.ln_arena_off = 0
            self.layer_norm(XC, LC, self.HT, L, lambda dc: self.mod_ap(0, 1, dc, 2), lambda dc: self.mod_ap(0, 0, dc, 2), LN_EPS,
                            extra_reads=[self.modT.res()])
            self.barrier()
            ZHc = self.view(0, [128, 4, LC], BF16)
            UCFc = self.view(2048, [128, 4, LC], BF16)
            YSc = self.view(4096, [128, 4, LC], BF16)
            self.hyena_branch(0, L, LC, "C", 8192, ZHc)
            self.barrier()
            self.conformer(0, L, LC, LC, 8192, UCFc)
            self.barrier()
            self.s5_branch(0, L, LC, 8192, YSc, False, True)
            self.barrier()
            self.mixer_out(0, L, LC, XC, 2, ZHc, UCFc, YSc, 8192)
            self.ffn(0, LC, XC, 2, False, hoff=L)
            self.ln_arena_off = 0
            self.layer_norm(XC, LC, self.HT, L, lambda dc: self.mod_ap(1, 1, dc, 2), lambda dc: self.mod_ap(1, 0, dc, 2), LN_EPS,
                            extra_reads=[self.modT.res()])
            self.load_tokens(din["x"][bi], L, self.XT)
            for l in range(DEPTH):
                if l == 1:
                    self.barrier()
                    YSc = self.view(4096, [128, 4, LC], BF16)
                    self.s5_branch(1, L, LC, 8192, YSc, False, True)
                self.ln_arena_off = 0
                self.layer_norm(self.XT, L, self.HT, 0, (lambda dc, l=l, bi=bi: self.mod_ap(l, 1, dc, bi)),
                                (lambda dc, l=l, bi=bi: self.mod_ap(l, 0, dc, bi)), LN_EPS, extra_reads=[self.modT.res()])
                self.barrier()
                ZH = self.view(0, [128, 4, L], BF16)
                UCF = self.view(16384, [128, 4, L], BF16)
                YS = self.view(32768, [128, 4, L], BF16)
                self.hyena_branch(l, 0, L, "L", 16384, ZH)
                self.barrier()
                self.conformer(l, 0, L, 64, 32768, UCF)
                self.barrier()
                self.s5_branch(l, 0, L, 49152, YS, True, False)
                self.barrier()
                self.mixer_out(l, 0, L, self.XT, bi, ZH, UCF, YS, 49152)
                if stop == "mix%d" % l and bi == 0:
                    self.dbg("xt", self.XT[:, :, :], self.XT.res())
                self.ffn(l, L, self.XT, bi, moe=(l == 1))
                if stop == "ffn%d" % l and bi == 0:
                    self.dbg("xt", self.XT[:, :, :], self.XT.res())
            self.store_out(bi)
        self.barrier()
        P.wait_all("sp", [self.out_res, self.out_res_sw])
        self.mark("end")
        P.emit(self.st)
        self.st.close()
        return nc

    def next_ps(self, reserve=False):
        if not hasattr(self, "_reserved"):
            self._reserved = set()
        while True:
            i = self._psi % 8
            self._psi += 1
            if i not in self._reserved:
                break
        if reserve:
            self._reserved.add(i)
        return self.psb[i]

    def release_ps(self, bufs):
        for b in bufs:
            self._reserved.discard(self.psb.index(b))

    def load_tokens(self, src, T, dst, stage_off=0):
        self.mark("load_tokens")
        self.barrier()
        G = min(512, T)
        nj = G // 128
        xstage = [self.view(stage_off + i * 16384, [128, 4, D], F32) for i in range(2)]
        for tg in range(T // G):
            stg = xstage[tg % 2]
            sv = src[tg * G:(tg + 1) * G, :].rearrange("(j p) d -> p j d", p=128)
            for jj in range(nj):
                self.dma("sp" if jj % 2 == 0 else "act", stg[:, jj, :], src[tg * G + jj * 128:tg * G + (jj + 1) * 128, :], writes=[stg.res()])
            for dc in range(8):
                pb = self.next_ps()
                for j in range(nj):
                    self.op("pe", (lambda e, pb=pb, stg=stg, j=j, dc=dc:
                                   e.transpose(pb[:, j * 128:(j + 1) * 128], stg[:, j, dc * 128:(dc + 1) * 128],
                                               self.ident_f[:])),
                            reads=[stg.res(), self.ident_f.res()], writes=[pb.res()])
                d_ap = dst[:, dc, tg * G:(tg + 1) * G]
                if dc % 2 == 0:
                    self.op("dve", (lambda e, d_ap=d_ap, pb=pb: e.tensor_copy(d_ap, pb[:, 0:G])),
                            reads=[pb.res()], writes=[dst.res()])
                else:
                    self.op("act", (lambda e, d_ap=d_ap, pb=pb: e.copy(d_ap, pb[:, 0:G])),
                            reads=[pb.res()], writes=[dst.res()])

    def compute_mods(self):
        self.mark("compute_mods")
        din = self.din
        self.cT = self.sb("cT", [128, 8, 3], F32)
        self.modT = self.sb("modT", [128, DEPTH, 48, 3], F32)
        self.bmodT = self.sb("bmodT", [128, DEPTH, 48], F32)
        rc = self.cT.res()
        for bi in range(NB):
            self.dma("sp", self.cT[:, :, bi], din["c"][bi].rearrange("(j p) -> p j", p=128), writes=[rc])
        self.dma("sp", self.cT[:, :, 2], din["c_ctx"].rearrange("(j p) -> p j", p=128), writes=[rc])
        for l in range(DEPTH):
            self.dma("sp", self.bmodT[:, l, :], din["b_mod"][l].rearrange("(j p) -> p j", p=128),
                     writes=[self.bmodT.res()])
        self.op("act", lambda e: e.activation(out=self.cT[:, :, :], in_=self.cT[:, :, :], func=AF.Silu),
                reads=[rc], writes=[rc])
        self.barrier()
        self.wmod_stage = [self.view(i * 16384, [128, 8, 512], F32) for i in range(2)]
        gi = 0
        for l in range(DEPTH):
            for mg in range(12):
                stg = self.wmod_stage[gi % 2]
                gi += 1
                src = din["w_mod"][l][:, mg * 512:(mg + 1) * 512].rearrange("(k p) m -> p k m", p=128)
                self.dma("sp", stg[:, 0:4, :], src[:, 0:4, :], writes=[stg.res()])
                self.dma("act", stg[:, 4:8, :], src[:, 4:8, :], writes=[stg.res()])
                pb = self.next_ps()
                for mt in range(4):
                    for kc in range(8):
                        self.op("pe", (lambda e, pb=pb, stg=stg, mt=mt, kc=kc:
                                       e.matmul(pb[:, mt * 3:(mt + 1) * 3], stg[:, kc, mt * 128:(mt + 1) * 128],
                                                self.cT[:, kc, :], start=(kc == 0), stop=(kc == 7))),
                                reads=[stg.res(), rc], writes=[pb.res()])
                for mt in range(4):
                    j = mg * 4 + mt
                    self.op("dve", (lambda e, pb=pb, mt=mt, j=j, l=l:
                                    e.tensor_scalar(self.modT[:, l, j, :], pb[:, mt * 3:(mt + 1) * 3],
                                                    self.bmodT[:, l, j:j + 1], None, ALU.add)),
                            reads=[pb.res(), self.bmodT.res()], writes=[self.modT.res()])
        rm = self.modT.res()
        for l in range(DEPTH):
            for i in (1, 4):
                self.op("dve", (lambda e, l=l, i=i: e.tensor_scalar(self.modT[:, l, 8 * i:8 * i + 8, :],
                                                                     self.modT[:, l, 8 * i:8 * i + 8, :],
                                                                     1.0, None, ALU.add)),
                        reads=[rm], writes=[rm])
            for i in (2, 5):
                self.op("dve", (lambda e, l=l, i=i: e.tensor_scalar(self.modT[:, l, 8 * i:8 * i + 8, :],
                                                                     self.modT[:, l, 8 * i:8 * i + 8, :],
                                                                     1.0 / ALPHA, None, ALU.mult)),
                        reads=[rm], writes=[rm])

    def mod_ap(self, l, which, dc, col):
        return self.modT[:, l, 8 * which + dc, col:col + 1]

    def layer_norm(self, src, T, dst, dst_off, scale_fn, bias_fn, eps, extra_reads=(), no_barrier=False, hf32=None, post_group=None):
        if not no_barrier:
            self.barrier()
        ab = getattr(self, "ln_arena_off", 0)
        self.ln_sq = self.view(ab, [128, 8, 512], F32)
        self.ln_mean = self.view(ab + 16384, [128, 512], F32)
        self.ln_rstd = self.view(ab + 18432, [128, 512], F32)
        self.ln_t = [self.view(ab + 20480 + i * 2048, [128, 512], F32) for i in range(2)]
        rs = src.res()
        rd = dst.res()
        ntg = (T + 511) // 512
        for tg in range(ntg):
            n = min(512, T - tg * 512)
            sl = slice(tg * 512, tg * 512 + n)
            sq = self.ln_sq
            self.op("act", (lambda e, sl=sl, n=n: e.activation(out=sq[:, :, 0:n], in_=src[:, :, sl], func=AF.Square)),
                    reads=[rs], writes=[sq.res()])
            pm = self.next_ps()
            pe2 = self.next_ps()
            for dc in range(8):
                self.op("pe", (lambda e, dc=dc, sl=sl, n=n, pm=pm:
                               e.matmul(pm[:, 0:n], self.ones_f[:, :], src[:, dc, sl], start=(dc == 0), stop=(dc == 7))),
                        reads=[rs, self.ones_f.res()], writes=[pm.res()])
            for dc in range(8):
                self.op("pe", (lambda e, dc=dc, n=n, pe2=pe2:
                               e.matmul(pe2[:, 0:n], self.ones_f[:, :], sq[:, dc, 0:n], start=(dc == 0), stop=(dc == 7))),
                        reads=[sq.res(), self.ones_f.res()], writes=[pe2.res()])
            mean = self.ln_mean
            rstd = self.ln_rstd
            self.rstd_from_stats(pm, pe2, mean, rstd, n, eps)
            for dc in range(8):
                t = self.ln_t[dc % 2]
                self.op("dve", (lambda e, dc=dc, sl=sl, n=n, t=t:
                                e.tensor_tensor(t[:, 0:n], src[:, dc, sl], mean[:, 0:n], ALU.subtract)),
                        reads=[rs, mean.res()], writes=[t.res()])
                self.op("dve", (lambda e, n=n, t=t: e.tensor_tensor(t[:, 0:n], t[:, 0:n], rstd[:, 0:n], ALU.mult)),
                        reads=[t.res(), rstd.res()], writes=[t.res()])
                sc_ap = scale_fn(dc)
                bi_ap = bias_fn(dc)
                if hf32 is not None:
                    self.op("act", (lambda e, dc=dc, n=n, t=t, sc_ap=sc_ap, bi_ap=bi_ap:
                                    e.activation(out=hf32[:, dc, 0:n], in_=t[:, 0:n], func=AF.Identity, scale=sc_ap, bias=bi_ap)),
                            reads=[t.res()] + list(extra_reads), writes=[hf32.res()])
                    self.op("pool", (lambda e, dc=dc, n=n, tg=tg: e.tensor_copy(dst[:, dc, dst_off + tg * 512:dst_off + tg * 512 + n], hf32[:, dc, 0:n])),
                            reads=[hf32.res()], writes=[rd])
                else:
                    self.op("act", (lambda e, dc=dc, n=n, t=t, tg=tg, sc_ap=sc_ap, bi_ap=bi_ap:
                                    e.activation(out=dst[:, dc, dst_off + tg * 512:dst_off + tg * 512 + n], in_=t[:, 0:n],
                                                 func=AF.Identity, scale=sc_ap, bias=bi_ap)),
                            reads=[t.res()] + list(extra_reads), writes=[rd])
            if post_group is not None:
                post_group(tg, n)


    def load_w_bf16(self, dst_ap, src_ap, res):
        self.dma("pool", dst_ap, src_ap, writes=[res])

    def load_vec_T(self, dst_ap, src_1d, res):
        self.dma("sp", dst_ap, src_1d.rearrange("(j p) -> p j", p=128), writes=[res])

    def conformer(self, l, hoff, T, RW, ab, out_view):
        self.mark("conformer")
        din = self.din
        CF_OFF = 1536
        nrows = T // RW
        PW = RW + 30
        G = 256
        rpg = G // RW
        ng = T // G
        o = ab
        Wcf = self.view(o, [128, 8, 1024], BF16); o += 16384
        DIAG = self.view(o, [128, 4, 31, 128], BF16); o += 4 * 31 * 128 * 2
        dwT = self.view(o, [128, 4, 31], F32); o += 4 * 31 * 4
        vecs = self.view(o, [128, 3, 4], F32); o += 48
        UPs = [self.view(o + i * 4 * rpg * PW * 2, [128, 4, rpg * PW], BF16) for i in range(2)]; o += 2 * 4 * rpg * PW * 2
        o = (o + 3) // 4 * 4
        Cb = self.view(o, [128, 4, G], F32); o += 4 * G * 4
        SQ = self.view(o, [128, 4, G], F32); o += 4 * G * 4
        mean = self.view(o, [128, G], F32); o += G * 4
        rstd = self.view(o, [128, G], F32); o += G * 4
        tmp = [self.view(o + i * G * 4, [128, G], F32) for i in range(2)]; o += 2 * G * 4
        sig = [self.view(o + i * G * 4, [128, G], F32) for i in range(2)]; o += 2 * G * 4
        for h in range(2):
            self.load_w_bf16(Wcf[:, :, h * 512:(h + 1) * 512],
                             din["w_in"][l][:, CF_OFF + h * 512:CF_OFF + (h + 1) * 512].rearrange("(k p) m -> p k m", p=128),
                             Wcf.res(h))
        for j in range(4):
            self.dma("sp", dwT[:, j, :], din["cf_dw_w"][l][:, j * 128:(j + 1) * 128].rearrange("k p -> p k"),
                     writes=[dwT.res()])
        self.load_vec_T(vecs[:, 0, :], din["cf_dw_b"][l], vecs.res())
        self.load_vec_T(vecs[:, 1, :], din["cf_ln_g"][l], vecs.res())
        self.load_vec_T(vecs[:, 2, :], din["cf_ln_b"][l], vecs.res())
        for UP in UPs:
            self.op("pool", (lambda e, UP=UP: e.memset(UP[:, :, :], 0.0)), writes=[UP.res()])
        for j in range(4):
            for k in range(31):
                eng_ = "act" if k % 4 != 3 else "dve"
                if eng_ == "act":
                    self.op("act", (lambda e, j=j, k=k: e.activation(out=DIAG[:, j, k, :], in_=self.ident_b[:, :], func=AF.Identity,
                                                                     scale=dwT[:, j, k:k + 1])),
                            reads=[dwT.res(), self.ident_b.res()], writes=[DIAG.res(j)])
                else:
                    self.op("dve", (lambda e, j=j, k=k: e.tensor_scalar(DIAG[:, j, k, :], self.ident_b[:, :], dwT[:, j, k:k + 1],
                                                                        None, ALU.mult)),
                            reads=[dwT.res(), self.ident_b.res()], writes=[DIAG.res(j)])
        rH = self.HT.res()
        for g in range(ng):
            UP = UPs[g % 2]
            tsl = slice(hoff + g * G, hoff + (g + 1) * G)
            for j in range(4):
                UPj = UP[:, j, :].rearrange("p (r w) -> p r w", w=PW)
                pa = self.next_ps()
                pg = self.next_ps()
                for kc in range(8):
                    self.op("pe", (lambda e, kc=kc, pa=pa, j=j, tsl=tsl:
                                   e.matmul(pa[:, 0:G], Wcf[:, kc, j * 128:(j + 1) * 128], self.HT[:, kc, tsl],
                                            start=(kc == 0), stop=(kc == 7))),
                            reads=[Wcf.res(0), rH], writes=[pa.res()])
                for kc in range(8):
                    self.op("pe", (lambda e, kc=kc, pg=pg, j=j, tsl=tsl:
                                   e.matmul(pg[:, 0:G], Wcf[:, kc, 512 + j * 128:512 + (j + 1) * 128], self.HT[:, kc, tsl],
                                            start=(kc == 0), stop=(kc == 7))),
                            reads=[Wcf.res(1), rH], writes=[pg.res()])
                sg = sig[j % 2]
                self.op("act", (lambda e, sg=sg, pg=pg: e.activation(out=sg[:, 0:G], in_=pg[:, 0:G], func=AF.Sigmoid)),
                        reads=[pg.res()], writes=[sg.res()])
                self.op("dve", (lambda e, UPj=UPj, pa=pa, sg=sg:
                                e.tensor_tensor(UPj[:, :, 15:15 + RW],
                                                pa[:, 0:G].rearrange("p (r w) -> p r w", w=RW),
                                                sg[:, 0:G].rearrange("p (r w) -> p r w", w=RW), ALU.mult)),
                        reads=[pa.res(), sg.res()], writes=[UP.res()])
            for j in range(4):
                UPj = UP[:, j, :].rearrange("p (r w) -> p r w", w=PW)
                pc = self.next_ps()
                for k in range(31):
                    self.op("pe", (lambda e, k=k, pc=pc, j=j, UPj=UPj:
                                   e.matmul(pc[:, 0:G], DIAG[:, j, k, :], UPj[:, :, k:k + RW],
                                            start=(k == 0), stop=(k == 30))),
                            reads=[DIAG.res(j), UP.res()], writes=[pc.res()])
                self.op("act", (lambda e, j=j, pc=pc: e.activation(out=Cb[:, j, :], in_=pc[:, 0:G], func=AF.Identity,
                                                                bias=vecs[:, 0, j:j + 1], scale=1.0)),
                        reads=[pc.res(), vecs.res()], writes=[Cb.res()])
                self.op("act", (lambda e, j=j, pc=pc: e.activation(out=SQ[:, j, :], in_=pc[:, 0:G], func=AF.Square,
                                                                bias=vecs[:, 0, j:j + 1], scale=1.0)),
                        reads=[pc.res(), vecs.res()], writes=[SQ.res()])
            pm = self.next_ps()
            pe2 = self.next_ps()
            for j in range(4):
                self.op("pe", (lambda e, j=j, pm=pm: e.matmul(pm[:, 0:G], self.ones_cf[:, :], Cb[:, j, :],
                                                             start=(j == 0), stop=(j == 3))),
                        reads=[Cb.res(), self.ones_cf.res()], writes=[pm.res()])
            for j in range(4):
                self.op("pe", (lambda e, j=j, pe2=pe2: e.matmul(pe2[:, 0:G], self.ones_cf[:, :], SQ[:, j, :],
                                                               start=(j == 0), stop=(j == 3))),
                        reads=[SQ.res(), self.ones_cf.res()], writes=[pe2.res()])
            self.rstd_from_stats(pm, pe2, mean, rstd, G, LN_EPS)
            for j in range(4):
                t = tmp[j % 2]
                self.op("dve", (lambda e, j=j, t=t: e.tensor_tensor(t[:, :], Cb[:, j, :], mean[:, :], ALU.subtract)),
                        reads=[Cb.res(), mean.res()], writes=[t.res()])
                self.op("pool", (lambda e, t=t: e.tensor_tensor(t[:, :], t[:, :], rstd[:, :], ALU.mult)),
                        reads=[t.res(), rstd.res()], writes=[t.res()])
                self.op("act", (lambda e, j=j, t=t, g=g:
                                e.activation(out=out_view[:, j, g * G:(g + 1) * G], in_=t[:, :], func=AF.Silu,
                                             scale=vecs[:, 1, j:j + 1], bias=vecs[:, 2, j:j + 1])),
                        reads=[t.res(), vecs.res()], writes=[out_view.res()])

    def rstd_from_stats(self, pm, pe2, mean, rstd, n, eps):
        self.op("act", (lambda e: e.copy(mean[:, 0:n], pm[:, 0:n])), reads=[pm.res()], writes=[mean.res()])
        self.op("dve", (lambda e: e.tensor_tensor(rstd[:, 0:n], mean[:, 0:n], mean[:, 0:n], ALU.mult)),
                reads=[mean.res()], writes=[rstd.res()])
        self.op("dve", (lambda e: e.tensor_tensor(rstd[:, 0:n], pe2[:, 0:n], rstd[:, 0:n], ALU.subtract)),
                reads=[pe2.res(), rstd.res()], writes=[rstd.res()])
        self.op("dve", (lambda e: e.tensor_scalar(rstd[:, 0:n], rstd[:, 0:n], float(eps), None, ALU.add)),
                reads=[rstd.res()], writes=[rstd.res()])
        self.op("act", (lambda e: e.activation(out=rstd[:, 0:n], in_=rstd[:, 0:n], func=AF.Sqrt)),
                reads=[rstd.res()], writes=[rstd.res()])
        self.op("dve", (lambda e: e.reciprocal(rstd[:, 0:n], rstd[:, 0:n])),
                reads=[rstd.res()], writes=[rstd.res()])

    def dbg_bf16(self, name, src_ap, res):
        if name in self.dbg_out:
            self.dma("pool", self.dbg_out[name], src_ap, reads=[res], writes=[Res()])


    def range_reduce(self, eng, x_ap, ti_ap, reads, res):
        PI_C = 3.14159
        self.op(eng, (lambda e: e.tensor_scalar(ti_ap, x_ap, 1.0 / TWO_PI, None, ALU.mult)), reads=reads, writes=[res])
        self.op("dve", (lambda e: e.scalar_tensor_tensor(x_ap, ti_ap, -TWO_PI, x_ap, ALU.mult, ALU.add)),
                reads=[res], writes=[res])
        self.op(eng, (lambda e: e.tensor_scalar(x_ap, x_ap, PI_C, -PI_C, ALU.min, ALU.max)), reads=[res], writes=[res])

    def s5_prologue(self, l):
        self.mark("s5_prologue")
        nc = self.nc
        din = self.din
        self.barrier()
        if not hasattr(self, "s5p"):
            self.s5p = [self.sb("s5p%d" % i, [128, 6, 2, 16], F32) for i in range(DEPTH)]
            self.halfpi = self.sb("halfpi", [128, 1], F32)
            self.op("pool", lambda e: e.memset(self.halfpi[:], math.pi / 2), writes=[self.halfpi.res()])
            self.s5_bmat = [nc.dram_tensor("s5_bmat%d" % i, [128, 16, 2, 2, 128], BF16, kind="Internal").ap() for i in range(DEPTH)]
            self.s5_cmat = [nc.dram_tensor("s5_cmat%d" % i, [128, 16, 2, 2, 128], BF16, kind="Internal").ap() for i in range(DEPTH)]
            self.s5_tab = [nc.dram_tensor("s5_tab%d" % i, [2, 16, 128, 2, L], F32, kind="Internal").ap() for i in range(DEPTH)]
            self.s5_res = [Res() for _ in range(DEPTH)]
        sp = self.s5p[l]
        rs = sp.res()
        o = 0
        raw = self.view(o, [128, 4, 2, 16], F32); o += 4 * 32 * 4
        tmp = self.view(o, [128, 6, 2, 16], F32); o += 6 * 32 * 4
        ti = self.view(o, [128, 2, 16], I32); o += 128
        iota_i = self.view(o, [128, L], I32); o += L * 4
        self.iota_t = self.view(o, [128, L], F32); o += L * 4
        rr = raw.res()
        for d in range(2):
            for gl in range(2):
                ps_ = slice(gl * 64, (gl + 1) * 64)
                self.dma("sp", raw[ps_, 0, d, :], din["s5_a_re"][l][d][gl::2, :].rearrange("k p -> p k"), writes=[rr])
                self.dma("sp", raw[ps_, 1, d, :], din["s5_a_im"][l][d][gl::2, :].rearrange("k p -> p k"), writes=[rr])
                self.dma("sp", raw[ps_, 2, d, :], din["s5_log_dt"][l][d][gl::2].partition_broadcast(64), writes=[rr])
        A = lambda i: raw[:, i, :, :]
        S = lambda i: sp[:, i, :, :]
        Tm = lambda i: tmp[:, i, :, :]
        rt = tmp.res()
        self.op("dve", lambda e: e.tensor_scalar(A(0), A(0), -1e-4, None, ALU.min), reads=[rr], writes=[rr])
        self.op("act", lambda e: e.activation(out=A(2), in_=A(2), func=AF.Exp), reads=[rr], writes=[rr])
        self.op("dve", lambda e: e.tensor_tensor(Tm(0), A(0), A(2), ALU.mult), reads=[rr], writes=[rt])
        self.op("act", lambda e: e.activation(out=S(0), in_=Tm(0), func=AF.Exp), reads=[rt], writes=[rs])
        self.op("dve", lambda e: e.tensor_tensor(S(1), A(1), A(2), ALU.mult), reads=[rr], writes=[rs])
        self.op("dve", lambda e: e.tensor_copy(Tm(1), S(1)), reads=[rs], writes=[rt])
        self.range_reduce("dve", Tm(1), ti[:, :, :], [rt], rt)
        self.op("act", lambda e: e.activation(out=S(3), in_=Tm(1), func=AF.Sin), reads=[rt], writes=[rs])
        self.op("act", lambda e: e.activation(out=Tm(1), in_=Tm(1), func=AF.Abs), reads=[rt], writes=[rt])
        self.op("act", lambda e: e.activation(out=S(2), in_=Tm(1), func=AF.Sin, scale=-1.0, bias=self.halfpi[:, 0:1]),
                reads=[rt, self.halfpi.res()], writes=[rs])
        self.op("dve", lambda e: e.tensor_tensor(Tm(2), S(0), S(2), ALU.mult), reads=[rs], writes=[rt])
        self.op("dve", lambda e: e.tensor_scalar(Tm(2), Tm(2), -1.0, None, ALU.add), reads=[rt], writes=[rt])
        self.op("dve", lambda e: e.tensor_tensor(Tm(3), S(0), S(3), ALU.mult), reads=[rs], writes=[rt])
        self.op("dve", lambda e: e.tensor_tensor(Tm(4), A(0), A(0), ALU.mult), reads=[rr], writes=[rt])
        self.op("dve", lambda e: e.tensor_tensor(Tm(5), A(1), A(1), ALU.mult), reads=[rr], writes=[rt])
        self.op("dve", lambda e: e.tensor_tensor(Tm(4), Tm(4), Tm(5), ALU.add), reads=[rt], writes=[rt])
        self.op("dve", lambda e: e.reciprocal(Tm(4), Tm(4)), reads=[rt], writes=[rt])
        self.op("dve", lambda e: e.tensor_tensor(S(4), Tm(2), A(0), ALU.mult), reads=[rt, rr], writes=[rs])
        self.op("dve", lambda e: e.tensor_tensor(Tm(5), Tm(3), A(1), ALU.mult), reads=[rt, rr], writes=[rt])
        self.op("dve", lambda e: e.tensor_tensor(S(4), S(4), Tm(5), ALU.add), reads=[rt, rs], writes=[rs])
        self.op("dve", lambda e: e.tensor_tensor(S(4), S(4), Tm(4), ALU.mult), reads=[rt, rs], writes=[rs])
        self.op("dve", lambda e: e.tensor_tensor(S(5), Tm(3), A(0), ALU.mult), reads=[rt, rr], writes=[rs])
        self.op("dve", lambda e: e.tensor_tensor(Tm(5), Tm(2), A(1), ALU.mult), reads=[rt, rr], writes=[rt])
        self.op("dve", lambda e: e.tensor_tensor(S(5), S(5), Tm(5), ALU.subtract), reads=[rt, rs], writes=[rs])
        self.op("dve", lambda e: e.tensor_tensor(S(5), S(5), Tm(4), ALU.mult), reads=[rt, rs], writes=[rs])
        if True:
            self.op("pool", lambda e: e.iota(iota_i[:, :], pattern=[[1, L]], base=0, channel_multiplier=0),
                    writes=[iota_i.res()])
            self.op("dve", lambda e: e.tensor_copy(self.iota_t[:, :], iota_i[:, :]), reads=[iota_i.res()],
                    writes=[self.iota_t.res()])
        o = (o + 3) // 4 * 4
        bpad = [self.view(o + i * 1024, [128, 2, 128], F32) for i in range(2)]; o += 2048
        cpad = [self.view(o + i * 1024, [128, 2, 128], F32) for i in range(2)]; o += 2048
        bbar = [self.view(o + i * 512, [128, 2, 128], BF16) for i in range(2)]; o += 1024
        cbf = [self.view(o + i * 512, [128, 2, 128], BF16) for i in range(2)]; o += 1024
        tq = [self.view(o + i * 1024, [128, 2, 128], F32) for i in range(2)]; o += 2048
        mT = [self.view(o + i * 1024, [128, 2, 2, 128], BF16) for i in range(2)]; o += 2048
        it = 0
        for d in range(2):
            for k in range(16):
                bp = bpad[it % 2]; cp = cpad[it % 2]; bb = bbar[it % 2]; cb = cbf[it % 2]; t2 = tq[it % 2]; mt = mT[it % 2]
                it += 1
                self.op("pool", (lambda e, bp=bp: e.memset(bp[:, :, :], 0.0)), writes=[bp.res()])
                self.op("pool", (lambda e, cp=cp: e.memset(cp[:, :, :], 0.0)), writes=[cp.res()])
                for gl in range(2):
                    g = 2 * k + gl
                    c0 = ((2 * k) % 8 + gl) * 16
                    self.dma("sp", bp[gl * 64:(gl + 1) * 64, 0, c0:c0 + 16], din["s5_b_re"][l][d][g], writes=[bp.res()])
                    self.dma("sp", bp[gl * 64:(gl + 1) * 64, 1, c0:c0 + 16], din["s5_b_im"][l][d][g], writes=[bp.res()])
                    self.dma("sp", cp[c0:c0 + 16, 0, gl * 64:(gl + 1) * 64], din["s5_c_re"][l][d][g], writes=[cp.res()])
                    self.dma("sp", cp[c0:c0 + 16, 1, gl * 64:(gl + 1) * 64], din["s5_c_im"][l][d][g], writes=[cp.res()])
                qre = sp[:, 4, d, k:k + 1]
                qim = sp[:, 5, d, k:k + 1]
                self.op("dve", (lambda e, t2=t2, bp=bp, qim=qim: e.tensor_scalar(t2[:, 0, :], bp[:, 1, :], qim, -1.0, ALU.mult, ALU.mult)),
                        reads=[bp.res(), rs], writes=[t2.res()])
                self.op("dve", (lambda e, t2=t2, bp=bp, qim=qim: e.tensor_scalar(t2[:, 1, :], bp[:, 0, :], qim, None, ALU.mult)),
                        reads=[bp.res(), rs], writes=[t2.res()])
                self.op("dve", (lambda e, t2=t2, bp=bp, bb=bb, qre=qre:
                                e.scalar_tensor_tensor(bb[:, 0, :], bp[:, 0, :], qre, t2[:, 0, :], ALU.mult, ALU.add)),
                        reads=[bp.res(), t2.res(), rs], writes=[bb.res()])
                self.op("dve", (lambda e, t2=t2, bp=bp, bb=bb, qre=qre:
                                e.scalar_tensor_tensor(bb[:, 1, :], bp[:, 1, :], qre, t2[:, 1, :], ALU.mult, ALU.add)),
                        reads=[bp.res(), t2.res(), rs], writes=[bb.res()])
                self.op("act", (lambda e, cb=cb, cp=cp: e.copy(cb[:, 0, :], cp[:, 0, :])), reads=[cp.res()], writes=[cb.res()])
                self.op("act", (lambda e, cb=cb, cp=cp: e.mul(cb[:, 1, :], cp[:, 1, :], -1.0)), reads=[cp.res()], writes=[cb.res()])
                pt = self.next_ps()
                ptb = pt[:, 0:256].bitcast(BF16)
                for ri in range(2):
                    self.op("pe", (lambda e, ptb=ptb, bb=bb, ri=ri: e.transpose(ptb[:, ri * 128:(ri + 1) * 128], bb[:, ri, :], self.ident_b[:, :])),
                            reads=[bb.res(), self.ident_b.res()], writes=[pt.res()])
                for ri in range(2):
                    self.op("pe", (lambda e, ptb=ptb, cb=cb, ri=ri: e.transpose(ptb[:, 256 + ri * 128:256 + (ri + 1) * 128], cb[:, ri, :], self.ident_b[:, :])),
                            reads=[cb.res(), self.ident_b.res()], writes=[pt.res()])
                self.op("dve", (lambda e, mt=mt, ptb=ptb: e.tensor_copy(mt[:, :, :, :].rearrange("p a b c -> p (a b c)"), ptb[:, 0:512])),
                        reads=[pt.res()], writes=[mt.res()])
                self.dma("sp", self.s5_bmat[l][:, k, d, :, :], mt[:, 0, :, :], reads=[mt.res()], writes=[mt.res("out")])
                self.dma("sp", self.s5_cmat[l][:, k, d, :, :], mt[:, 1, :, :], reads=[mt.res()], writes=[mt.res("out")])
        o = (o + 3) // 4 * 4
        ph = [self.view(o + i * (L * 4), [128, L], F32) for i in range(2)]; o += 2 * L * 4
        phi = [self.view(o + i * (L * 4), [128, L], I32) for i in range(2)]; o += 2 * L * 4
        cs = [self.view(o + i * (2 * L * 4), [128, 2, L], F32) for i in range(2)]; o += 4 * L * 4
        it = 0
        for d in range(2):
            for k in range(16):
                p_ = ph[it % 2]; pi_ = phi[it % 2]; c_ = cs[it % 2]
                it += 1
                th = sp[:, 1, d, k:k + 1]
                self.op("dve", (lambda e, p_=p_, th=th: e.tensor_scalar(p_[:, :], self.iota_t[:, :], th, None, ALU.mult)),
                        reads=[self.iota_t.res(), rs], writes=[p_.res()])
                self.range_reduce("dve", p_[:, :], pi_[:, :], [p_.res()], p_.res())
                self.op("act", (lambda e, p_=p_, c_=c_: e.activation(out=c_[:, 1, :], in_=p_[:, :], func=AF.Sin)),
                        reads=[p_.res()], writes=[c_.res()])
                self.op("act", (lambda e, p_=p_: e.activation(out=p_[:, :], in_=p_[:, :], func=AF.Abs)),
                        reads=[p_.res()], writes=[p_.res()])
                self.op("act", (lambda e, p_=p_, c_=c_: e.activation(out=c_[:, 0, :], in_=p_[:, :], func=AF.Sin, scale=-1.0,
                                                                     bias=self.halfpi[:, 0:1])),
                        reads=[p_.res(), self.halfpi.res()], writes=[c_.res()])
                self.dma("sp", self.s5_tab[l][d, k], c_[:, :, :], reads=[c_.res()], writes=[c_.res("out")])


    def s5_branch(self, l, hoff, T, ab, out_view, use_init, want_final):
        self.mark("s5_branch")
        din = self.din
        S5_OFF = 2560
        N = min(512, T)
        nch = T // N
        sp = self.s5p[l]
        rs = sp.res()
        fin = self.s5fin
        o = ab
        uT = out_view
        mats = self.view(o, [128, 2, 4, 2, 2, 128], BF16); o += 8192
        wdiag = self.view(o, [128, 4, 128], BF16); o += 1024
        vecs = self.view(o, [128, 2, 4], F32); o += 32
        ini = self.view(o, [128, 2, 2], F32); o += 16
        cb0 = o
        Ws5 = self.view(o, [128, 8, 512], BF16)
        self.load_w_bf16(Ws5[:, :, :], din["w_in"][l][:, S5_OFF:S5_OFF + 512].rearrange("(k p) m -> p k m", p=128), Ws5.res())
        rH = self.HT.res()
        G = min(512, T)
        for j in range(4):
            for tg in range(T // G):
                pb = self.next_ps()
                for kc in range(8):
                    self.op("pe", (lambda e, kc=kc, pb=pb, j=j, tg=tg:
                                   e.matmul(pb[:, 0:G], Ws5[:, kc, j * 128:(j + 1) * 128],
                                            self.HT[:, kc, hoff + tg * G:hoff + (tg + 1) * G], start=(kc == 0), stop=(kc == 7))),
                            reads=[Ws5.res(), rH], writes=[pb.res()])
                self.op("act", (lambda e, pb=pb, j=j, tg=tg: e.copy(uT[:, j, tg * G:(tg + 1) * G], pb[:, 0:G])),
                        reads=[pb.res()], writes=[uT.res()])
        self.barrier()
        o = cb0
        NB4 = N * 4
        tb = [self.view(o + i * 2 * NB4, [128, 2, N], F32) for i in range(3)]; o += 6 * NB4
        pp = [self.view(o + i * 2 * NB4, [128, 2, N], F32) for i in range(2)]; o += 4 * NB4
        vv = [self.view(o + i * 2 * NB4, [128, 2, N], F32) for i in range(2)]; o += 4 * NB4
        tt = [self.view(o + i * 2 * NB4, [128, 2, N], F32) for i in range(2)]; o += 4 * NB4
        rr_ = self.view(o, [128, 2, N], F32); o += 2 * NB4
        ss = self.view(o, [128, 4, N], BF16); o += N * 8
        carry = self.view(o, [128, 2, 1], F32); o += 8
        self.load_vec_T(vecs[:, 0, :], din["s5_d"][l], vecs.res())
        self.load_vec_T(vecs[:, 1, :], din["s5_b_glu"][l], vecs.res())
        for j in range(4):
            self.op("act", (lambda e, j=j: e.activation(out=wdiag[:, j, :], in_=self.ident_b[:, :], func=AF.Identity, scale=vecs[:, 0, j:j + 1])),
                    reads=[vecs.res(), self.ident_b.res()], writes=[wdiag.res()])
        ci = 0
        nbank = (T + 511) // 512
        for j in range(4):
            self.dma("sp", mats[:, 0, :, :, :, :].rearrange("p a b c d -> p (a b c d)"),
                     self.s5_bmat[l][:, 4 * j:4 * j + 4, :, :, :].rearrange("p a b c d -> p (a b c d)"), writes=[mats.res()])
            self.dma("sp", mats[:, 1, :, :, :, :].rearrange("p a b c d -> p (a b c d)"),
                     self.s5_cmat[l][:, 4 * j:4 * j + 4, :, :, :].rearrange("p a b c d -> p (a b c d)"), writes=[mats.res()])
            acc = [self.next_ps(reserve=True) for _ in range(nbank)]
            started = set()
            units = [(k4, d, cc) for k4 in range(4) for d in range(2) for cc in range(nch)]
            ctxs = {}

            def tload(ui, j=j):
                k4, d, cc = units[ui]
                k = 4 * j + k4
                c = cc if d == 0 else nch - 1 - cc
                tbl = tb[ui % 3]
                tcol = slice(c * N, (c + 1) * N) if d == 0 else slice(T - (c + 1) * N, T - c * N)
                self.dma("sp", tbl[:, :, :], self.s5_tab[l][d, k][:, :, tcol], writes=[tbl.res()])

            def front(ui, j=j, acc=acc):
                k4, d, cc = units[ui]
                k = 4 * j + k4
                c = cc if d == 0 else nch - 1 - cc
                tsl = slice(c * N, (c + 1) * N)
                tbl = tb[ui % 3]; p_ = pp[ui % 2]; v_ = vv[ui % 2]; t_ = tt[ui % 2]
                rv = (lambda ap, d=d: ap if d == 0 else ap[:, ::-1])
                pre = [self.next_ps(), self.next_ps()]
                for ri in range(2):
                    self.op("pe", (lambda e, ri=ri, pre=pre, k4=k4, d=d, tsl=tsl:
                                   e.matmul(pre[ri][:, 0:N], mats[:, 0, k4, d, ri, :], uT[:, j, tsl], start=True, stop=True)),
                            reads=[mats.res(), uT.res()], writes=[pre[ri].res()])
                    self.op("act", (lambda e, ri=ri, pre=pre, p_=p_: e.copy(p_[:, ri, :], pre[ri][:, 0:N])),
                            reads=[pre[ri].res()], writes=[p_.res(ri)])
                cosT = rv(tbl[:, 0, :]); sinT = rv(tbl[:, 1, :])
                self.op("pool", (lambda e, v_=v_, p_=p_, cosT=cosT: e.tensor_tensor(v_[:, 0, :], p_[:, 0, :], cosT, ALU.mult)),
                        reads=[p_.res(0), tbl.res()], writes=[v_.res(0)])
                self.op("pool", (lambda e, t_=t_, p_=p_, sinT=sinT: e.tensor_tensor(t_[:, 0, :], p_[:, 1, :], sinT, ALU.mult)),
                        reads=[p_.res(1), tbl.res()], writes=[t_.res(0)])
                self.op("pool", (lambda e, v_=v_, p_=p_, cosT=cosT: e.tensor_tensor(v_[:, 1, :], p_[:, 1, :], cosT, ALU.mult)),
                        reads=[p_.res(1), tbl.res()], writes=[v_.res(1)])
                self.op("pool", (lambda e, t_=t_, p_=p_, sinT=sinT: e.tensor_tensor(t_[:, 1, :], p_[:, 0, :], sinT, ALU.mult)),
                        reads=[p_.res(0), tbl.res()], writes=[t_.res(1)])
                ctxs[ui] = (tbl, v_, t_, cosT, sinT, rv)

            def back(ui, j=j, acc=acc):
                k4, d, cc = units[ui]
                k = 4 * j + k4
                c = cc if d == 0 else nch - 1 - cc
                tbl, v_, t_, cosT, sinT, rv = ctxs.pop(ui)
                r_ = rr_; s_ = ss
                rho = sp[:, 0, d, k:k + 1]
                if cc == 0 and use_init:
                    c1 = sp[:, 2, d, k:k + 1]; s1 = sp[:, 3, d, k:k + 1]
                    self.op("dve", (lambda e, c1=c1: e.tensor_scalar(carry[:, 0, 0:1], fin[:, 0, d, k:k + 1], c1, None, ALU.mult)),
                            reads=[fin.res(), rs], writes=[carry.res()])
                    self.op("dve", (lambda e, s1=s1: e.scalar_tensor_tensor(carry[:, 0, 0:1], fin[:, 1, d, k:k + 1], s1, carry[:, 0, 0:1], ALU.mult, ALU.subtract)),
                            reads=[fin.res(), rs, carry.res()], writes=[carry.res()])
                    self.op("dve", (lambda e: e.tensor_scalar(carry[:, 0, 0:1], carry[:, 0, 0:1], -1.0, None, ALU.mult)),
                            reads=[carry.res()], writes=[carry.res()])
                    self.op("dve", (lambda e, s1=s1: e.tensor_scalar(carry[:, 1, 0:1], fin[:, 0, d, k:k + 1], s1, None, ALU.mult)),
                            reads=[fin.res(), rs], writes=[carry.res()])
                    self.op("dve", (lambda e, c1=c1: e.scalar_tensor_tensor(carry[:, 1, 0:1], fin[:, 1, d, k:k + 1], c1, carry[:, 1, 0:1], ALU.mult, ALU.add)),
                            reads=[fin.res(), rs, carry.res()], writes=[carry.res()])
                self.op("dve", (lambda e: e.tensor_tensor(v_[:, 0, :], v_[:, 0, :], t_[:, 0, :], ALU.add)),
                        reads=[t_.res(0), v_.res(0)], writes=[v_.res(0)])
                self.op("dve", (lambda e: e.tensor_tensor(v_[:, 1, :], v_[:, 1, :], t_[:, 1, :], ALU.subtract)),
                        reads=[t_.res(1), v_.res(1)], writes=[v_.res(1)])
                for ri in range(2):
                    init_ap = 0.0 if (cc == 0 and not use_init) else carry[:, ri, 0:1]
                    self.op("dve", (lambda e, ri=ri, init_ap=init_ap:
                                    e.tensor_tensor_scan(rv(r_[:, ri, :]), rho.to_broadcast([128, N]), rv(v_[:, ri, :]), init_ap, ALU.mult, ALU.add)),
                            reads=[v_.res(ri), rs, carry.res()], writes=[r_.res()])
                if cc < nch - 1:
                    lc = N - 1 if d == 0 else 0
                    self.op("act", (lambda e, lc=lc: e.copy(carry[:, :, 0:1], r_[:, :, lc:lc + 1])), reads=[r_.res()], writes=[carry.res()])
                self.op("dve", (lambda e: e.tensor_tensor(s_[:, 0, :], r_[:, 0, :], cosT, ALU.mult)), reads=[r_.res(), tbl.res()], writes=[s_.res()])
                self.op("dve", (lambda e: e.scalar_tensor_tensor(s_[:, 1, :], r_[:, 1, :], -1.0, sinT, ALU.mult, ALU.mult)),
                        reads=[r_.res(), tbl.res()], writes=[s_.res()])
                self.op("dve", (lambda e: e.tensor_tensor(s_[:, 2, :], r_[:, 0, :], sinT, ALU.mult)), reads=[r_.res(), tbl.res()], writes=[s_.res()])
                self.op("dve", (lambda e: e.tensor_tensor(s_[:, 3, :], r_[:, 1, :], cosT, ALU.mult)), reads=[r_.res(), tbl.res()], writes=[s_.res()])
                if want_final and cc == nch - 1:
                    col = N - 1 if d == 0 else 0
                    csl = slice(col, col + 1)
                    tcs = slice(N - 1, N)
                    cF = tbl[:, 0, tcs]; sF = tbl[:, 1, tcs]
                    fr = fin[:, 0, d, k:k + 1]; fi = fin[:, 1, d, k:k + 1]
                    f2 = carry[:, 0, 0:1]
                    self.op("dve", (lambda e: e.tensor_tensor(fr, r_[:, 0, csl], cF, ALU.mult)), reads=[r_.res(), tbl.res()], writes=[fin.res()])
                    self.op("dve", (lambda e: e.tensor_tensor(fi, r_[:, 1, csl], sF, ALU.mult)), reads=[r_.res(), tbl.res()], writes=[fin.res()])
                    self.op("dve", (lambda e: e.tensor_tensor(fr, fr, fi, ALU.subtract)), reads=[fin.res()], writes=[fin.res()])
                    self.op("dve", (lambda e: e.tensor_tensor(fi, r_[:, 0, csl], sF, ALU.mult)), reads=[r_.res(), tbl.res(), fin.res()], writes=[fin.res()])
                    self.op("dve", (lambda e: e.tensor_tensor(f2, r_[:, 1, csl], cF, ALU.mult)), reads=[r_.res(), tbl.res()], writes=[carry.res()])
                    self.op("dve", (lambda e: e.tensor_tensor(fi, fi, f2, ALU.add)), reads=[carry.res(), fin.res()], writes=[fin.res()])
                bnk = (c * N) // 512
                off = (c * N) % 512
                for q in range(4):
                    st_ = bnk not in started
                    started.add(bnk)
                    ri = q // 2
                    self.op("pe", (lambda e, ri=ri, q=q, st_=st_:
                                   e.matmul(acc[bnk][:, off:off + N], mats[:, 1, k4, d, ri, :], s_[:, q, :],
                                            start=st_, stop=False, skip_group_check=True)),
                            reads=[mats.res(), s_.res()], writes=[acc[bnk].res()])

            tload(0)
            if len(units) > 1:
                tload(1)
            front(0)
            for ui in range(len(units)):
                if ui + 2 < len(units):
                    tload(ui + 2)
                if ui + 1 < len(units):
                    front(ui + 1)
                back(ui)
            for c in range(nch):
                bnk = (c * N) // 512
                off = (c * N) % 512
                self.op("pe", (lambda e, j=j, c=c, bnk=bnk, off=off, acc=acc:
                               e.matmul(acc[bnk][:, off:off + N], wdiag[:, j, :], uT[:, j, c * N:(c + 1) * N], start=False, stop=True,
                                        skip_group_check=True)),
                        reads=[wdiag.res(), uT.res()], writes=[acc[bnk].res()])
            for bnk in range(nbank):
                n = min(512, T - bnk * 512)
                self.op("act", (lambda e, bnk=bnk, n=n, j=j, acc=acc: e.activation(out=out_view[:, j, bnk * 512:bnk * 512 + n],
                                                                          in_=acc[bnk][:, 0:n], func=(AF.Copy if "_s5raw" in self.debug else AF.Gelu))),
                        reads=[acc[bnk].res()], writes=[out_view.res()])
            self.release_ps(acc)
        self.dbg_bf16("ys5g", out_view[:, :, :], out_view.res())
        sg = [self.view(cb0 + i * 2048, [128, 512], F32) for i in range(4)]
        self.barrier()
        wglu = self.view(cb0 + 8192, [128, 4, 512], BF16)
        self.load_w_bf16(wglu[:, :, :], din["s5_w_glu"][l].rearrange("(k p) m -> p k m", p=128), wglu.res())
        for tg in range((T + 511) // 512):
            n = min(512, T - tg * 512)
            tsl = slice(tg * 512, tg * 512 + n)
            pz = [self.next_ps(reserve=True) for _ in range(4)]
            self.release_ps(pz)
            for jo in range(4):
                for ji in range(4):
                    self.op("pe", (lambda e, jo=jo, ji=ji, tsl=tsl, n=n, pz=pz: e.matmul(pz[jo][:, 0:n], wglu[:, ji, jo * 128:(jo + 1) * 128],
                                                                                  out_view[:, ji, tsl], start=(ji == 0), stop=(ji == 3))),
                            reads=[wglu.res(), out_view.res()], writes=[pz[jo].res()])
            for jo in range(4):
                self.op("act", (lambda e, jo=jo, n=n, pz=pz: e.activation(out=sg[jo][:, 0:n], in_=pz[jo][:, 0:n], func=AF.Sigmoid,
                                                                   bias=vecs[:, 1, jo:jo + 1], scale=1.0)),
                        reads=[pz[jo].res(), vecs.res()], writes=[sg[jo].res()])
            for jo in range(4):
                self.op("dve", (lambda e, jo=jo, n=n, tsl=tsl: e.tensor_tensor(out_view[:, jo, tsl], out_view[:, jo, tsl], sg[jo][:, 0:n], ALU.mult)),
                        reads=[sg[jo].res(), out_view.res()], writes=[out_view.res()])


    def mixer_out(self, l, hoff, T, X, col, ZH, UCF, YS, ab):
        self.mark("mixer_out")
        din = self.din
        GATE_OFF = 3072
        G = min(512, T)
        ntg = T // G
        o = ab
        MIX = self.view(o, [128, 8, T], BF16); o += 8 * T * 2
        Wg = [self.view(o, [128, 8, 3, 128], BF16)] * 2; o += 6144
        Wb = [self.view(o, [128, 4, 3, 128], BF16)] * 2; o += 3072
        sig = [self.view(o + i * 2048, [128, 512], F32) for i in range(3)]; o += 3 * 2048
        mm = sig
        lnv = self.view(o, [128, 2, 8], F32); o += 64
        rH = self.HT.res()
        branches = (ZH, UCF, YS)
        wnames = ("w_hy_out", "w_cf_out", "w_s5_out")
        self.load_vec_T(lnv[:, 0, :], din["ln1_g"][l], lnv.res())
        self.load_vec_T(lnv[:, 1, :], din["ln1_b"][l], lnv.res())
        for dt in range(8):
            wg = Wg[dt % 2]
            wb = Wb[dt % 2]
            for br in range(3):
                c0 = GATE_OFF + br * 1024 + dt * 128
                self.load_w_bf16(wg[:, :, br, :], din["w_in"][l][:, c0:c0 + 128].rearrange("(k p) m -> p k m", p=128), wg.res())
                self.load_w_bf16(wb[:, :, br, :], din[wnames[br]][l][:, dt * 128:(dt + 1) * 128].rearrange("(k p) m -> p k m", p=128), wb.res())
            for tg in range(ntg):
                tsl = slice(tg * G, (tg + 1) * G)
                hsl = slice(hoff + tg * G, hoff + (tg + 1) * G)
                pg = [self.next_ps(reserve=True) for _ in range(3)]
                po = [self.next_ps(reserve=True) for _ in range(3)]
                self.release_ps(pg + po)
                for br in range(3):
                    for kc in range(8):
                        self.op("pe", (lambda e, pg=pg, br=br, kc=kc, wg=wg, hsl=hsl:
                                       e.matmul(pg[br][:, 0:G], wg[:, kc, br, :], self.HT[:, kc, hsl], start=(kc == 0), stop=(kc == 7))),
                                reads=[wg.res(), rH], writes=[pg[br].res()])
                    for kc in range(4):
                        self.op("pe", (lambda e, po=po, br=br, kc=kc, wb=wb, tsl=tsl:
                                       e.matmul(po[br][:, 0:G], wb[:, kc, br, :], branches[br][:, kc, tsl], start=(kc == 0), stop=(kc == 3))),
                                reads=[wb.res(), branches[br].res()], writes=[po[br].res()])
                for br in range(3):
                    self.op("act", (lambda e, br=br, pg=pg: e.activation(out=sig[br][:, 0:G], in_=pg[br][:, 0:G], func=AF.Sigmoid)),
                            reads=[pg[br].res()], writes=[sig[br].res()])
                    self.op("dve", (lambda e, br=br, po=po: e.tensor_tensor(mm[br][:, 0:G], po[br][:, 0:G], sig[br][:, 0:G], ALU.mult)),
                            reads=[po[br].res(), sig[br].res()], writes=[mm[br].res()])
                self.op("pool", (lambda e: e.tensor_tensor(mm[0][:, 0:G], mm[0][:, 0:G], mm[1][:, 0:G], ALU.add)),
                        reads=[mm[0].res(), mm[1].res()], writes=[mm[0].res()])
                self.op("pool", (lambda e, dt=dt, tsl=tsl: e.tensor_tensor(MIX[:, dt, tsl], mm[0][:, 0:G], mm[2][:, 0:G], ALU.add)),
                        reads=[mm[0].res(), mm[2].res()], writes=[MIX.res()])
        self.barrier()
        WO = self.view(0, [128, 8, 1024], BF16)
        self.ln_arena_off = 16384
        self.load_w_bf16(WO[:, :, :], din["w_o"][l].rearrange("(k p) m -> p k m", p=128), WO.res())
        for dt in range(8):
            g1 = self.mod_ap(l, 2, dt, col)
            for tg in range(ntg):
                tsl = slice(tg * G, (tg + 1) * G)
                pb = self.next_ps()
                for kc in range(8):
                    self.op("pe", (lambda e, pb=pb, kc=kc, dt=dt, tsl=tsl:
                                   e.matmul(pb[:, 0:G], WO[:, kc, dt * 128:(dt + 1) * 128], MIX[:, kc, tsl], start=(kc == 0), stop=(kc == 7))),
                            reads=[WO.res(), MIX.res()], writes=[pb.res()])
                self.op("dve", (lambda e, pb=pb, dt=dt, tsl=tsl, g1=g1:
                                e.scalar_tensor_tensor(X[:, dt, tsl], pb[:, 0:G], g1, X[:, dt, tsl], ALU.mult, ALU.add)),
                        reads=[pb.res(), X.res(), self.modT.res()], writes=[X.res()])
        self.layer_norm(X, T, X, 0, lambda dc: lnv[:, 0, dc:dc + 1], lambda dc: lnv[:, 1, dc:dc + 1], LN_EPS / (ALPHA * ALPHA),
                        extra_reads=[lnv.res()], no_barrier=True)

    def ffn(self, l, T, X, col, moe, hoff=0):
        self.mark("ffn")
        din = self.din
        G = min(512, T)
        ntg = T // G
        NTT = T // 128
        self.barrier()
        self.ln_arena_off = 0
        hook = None
        o = 24576 + 16384
        comb = combT = sel = None
        if moe:
            hf32 = self.view(24576, [128, 8, 512], F32)
            rtr = self.view(o, [128, 8, 8], F32); o += 256
            comb = self.view(o, [128, NTT, 8], F32); o += NTT * 32
            rsc = self.view(o, [128, 8, 8], F32); o += 256
            combT = self.view(o, [8, T], F32); o += T * 4
            sel = self.view(o, [8, 8, 128], F32); o += 4096
            self.dma("sp", rtr[:, :, :], din["moe_router"][0].rearrange("(k p) e -> p k e", p=128), writes=[rtr.res()])
            self.op("dve", (lambda e: e.tensor_copy(sel[:, :, :], self.ident_f[0:8, 0:8].unsqueeze(2).to_broadcast([8, 8, 128]))),
                    reads=[self.ident_f.res()], writes=[sel.res()])

            def hook(tg, n):
                for q in range(n // 128):
                    tt = tg * (512 // 128) + q
                    pl = self.next_ps()
                    for kc in range(8):
                        self.op("pe", (lambda e, pl=pl, kc=kc, q=q: e.matmul(pl[:, 0:8], hf32[:, kc, q * 128:(q + 1) * 128], rtr[:, kc, :],
                                                                          start=(kc == 0), stop=(kc == 7))),
                                reads=[hf32.res(), rtr.res()], writes=[pl.res()])
                    lg = rsc[:, 0, :]; m1 = rsc[:, 1, 0:1]; mk1 = rsc[:, 2, :]; msk = rsc[:, 3, :]; m2 = rsc[:, 1, 1:2]; mk2 = rsc[:, 4, :]
                    dd = rsc[:, 1, 2:3]; w1 = rsc[:, 1, 3:4]; w2 = rsc[:, 1, 4:5]
                    rr = rsc.res()
                    self.op("act", (lambda e, pl=pl: e.copy(lg, pl[:, 0:8])), reads=[pl.res()], writes=[rr])
                    self.op("dve", (lambda e: e.tensor_reduce(m1, lg, AX.X, ALU.max)), reads=[rr], writes=[rr])
                    self.op("dve", (lambda e: e.tensor_scalar(mk1, lg, m1, None, ALU.is_equal)), reads=[rr], writes=[rr])
                    self.op("dve", (lambda e: e.scalar_tensor_tensor(msk, mk1, -1e30, lg, ALU.mult, ALU.add)), reads=[rr], writes=[rr])
                    self.op("dve", (lambda e: e.tensor_reduce(m2, msk, AX.X, ALU.max)), reads=[rr], writes=[rr])
                    self.op("dve", (lambda e: e.tensor_scalar(mk2, msk, m2, None, ALU.is_equal)), reads=[rr], writes=[rr])
                    self.op("dve", (lambda e: e.tensor_tensor(dd, m2, m1, ALU.subtract)), reads=[rr], writes=[rr])
                    self.op("act", (lambda e: e.activation(out=dd, in_=dd, func=AF.Exp)), reads=[rr], writes=[rr])
                    self.op("dve", (lambda e: e.tensor_scalar(w1, dd, 1.0, None, ALU.add)), reads=[rr], writes=[rr])
                    self.op("dve", (lambda e: e.reciprocal(w1, w1)), reads=[rr], writes=[rr])
                    self.op("dve", (lambda e: e.tensor_tensor(w2, dd, w1, ALU.mult)), reads=[rr], writes=[rr])
                    self.op("dve", (lambda e: e.tensor_scalar(mk1, mk1, w1, None, ALU.mult)), reads=[rr], writes=[rr])
                    self.op("dve", (lambda e, tt=tt: e.scalar_tensor_tensor(comb[:, tt, :], mk2, w2, mk1, ALU.mult, ALU.add)),
                            reads=[rr], writes=[comb.res()])
                    pt = self.next_ps()
                    self.op("pe", (lambda e, pt=pt, tt=tt: e.transpose(pt[0:8, 0:128], comb[:, tt, :], self.ident_f[:, :])),
                            reads=[comb.res(), self.ident_f.res()], writes=[pt.res()])
                    self.op("act", (lambda e, pt=pt, tt=tt: e.copy(combT[:, tt * 128:(tt + 1) * 128], pt[0:8, 0:128])),
                            reads=[pt.res()], writes=[combT.res()])
        self.layer_norm(X, T, self.HT, hoff, lambda dc: self.mod_ap(l, 4, dc, col), lambda dc: self.mod_ap(l, 3, dc, col), LN_EPS,
                        extra_reads=[self.modT.res()], hf32=(hf32 if moe else None), post_group=hook)
        self.barrier()
        FF = D_EXP if moe else D_FF
        nchunk = FF // 128
        F = 4
        CB = None
        if moe:
            CB = self.view(o, [128, T], F32); o += T * 4
        lnv = self.view(o, [128, 2, 8], F32); o += 64
        W1 = [self.view(o + i * 8192, [128, 8, F * 128], BF16) for i in range(2)]; o += 16384
        W3 = [self.view(o + i * 8192, [128, 8, F * 128], BF16) for i in range(2)]; o += 16384
        W2 = [self.view(i * 8192, [128, F, 1024], BF16) for i in range(2)]
        st = [self.view(16384 + i * 2048, [128, 512], F32) for i in range(2)]
        tt_ = [self.view(20480 + i * 2048, [128, 512], F32) for i in range(2)]
        GT = self.view(24576, [128, F, T], BF16)
        self.load_vec_T(lnv[:, 0, :], din["ln2_g"][l], lnv.res())
        self.load_vec_T(lnv[:, 1, :], din["ln2_b"][l], lnv.res())
        rH = self.HT.res()
        gi = 0
        ii = 0
        for ex in range(NEXP if moe else 1):
            if moe:
                w1d, w3d, w2d = din["moe_w1"][0][ex], din["moe_w3"][0][ex], din["moe_w2"][0][ex]
                for tg in range(ntg):
                    pc = self.next_ps()
                    self.op("pe", (lambda e, pc=pc, ex=ex, tg=tg: e.matmul(pc[:, 0:G], sel[:, ex, :], combT[:, tg * G:(tg + 1) * G], start=True, stop=True)),
                            reads=[sel.res(), combT.res()], writes=[pc.res()])
                    self.op("act", (lambda e, pc=pc, tg=tg: e.copy(CB[:, tg * G:(tg + 1) * G], pc[:, 0:G])), reads=[pc.res()], writes=[CB.res()])
            else:
                w1d, w3d, w2d = din["ffn_w1"][0], din["ffn_w3"][0], din["ffn_w2"][0]
            for g0 in range(0, nchunk, F):
                nf = min(F, nchunk - g0)
                w1 = W1[gi % 2]; w3 = W3[gi % 2]; w2 = W2[gi % 2]
                gi += 1
                c0 = g0 * 128
                self.load_w_bf16(w1[:, :, 0:nf * 128], w1d[:, c0:c0 + nf * 128].rearrange("(k p) m -> p k m", p=128), w1.res())
                self.load_w_bf16(w3[:, :, 0:nf * 128], w3d[:, c0:c0 + nf * 128].rearrange("(k p) m -> p k m", p=128), w3.res())
                self.load_w_bf16(w2[:, 0:nf, :], w2d[c0:c0 + nf * 128, :].rearrange("(f p) m -> p f m", p=128), w2.res())
                for f in range(nf):
                    for tg in range(ntg):
                        tsl = slice(tg * G, (tg + 1) * G)
                        p1 = self.next_ps()
                        p3 = self.next_ps()
                        for kc in range(8):
                            self.op("pe", (lambda e, p1=p1, kc=kc, f=f, w1=w1, tg=tg:
                                           e.matmul(p1[:, 0:G], w1[:, kc, f * 128:(f + 1) * 128], self.HT[:, kc, hoff + tg * G:hoff + (tg + 1) * G], start=(kc == 0), stop=(kc == 7))),
                                    reads=[w1.res(), rH], writes=[p1.res()])
                        for kc in range(8):
                            self.op("pe", (lambda e, p3=p3, kc=kc, f=f, w3=w3, tg=tg:
                                           e.matmul(p3[:, 0:G], w3[:, kc, f * 128:(f + 1) * 128], self.HT[:, kc, hoff + tg * G:hoff + (tg + 1) * G], start=(kc == 0), stop=(kc == 7))),
                                    reads=[w3.res(), rH], writes=[p3.res()])
                        s_ = st[ii % 2]; t_ = tt_[ii % 2]
                        ii += 1
                        self.op("act", (lambda e, s_=s_, p1=p1: e.activation(out=s_[:, 0:G], in_=p1[:, 0:G], func=AF.Silu)),
                                reads=[p1.res()], writes=[s_.res()])
                        if moe:
                            self.op("dve", (lambda e, t_=t_, p3=p3, tsl=tsl: e.tensor_tensor(t_[:, 0:G], p3[:, 0:G], CB[:, tsl], ALU.mult)),
                                    reads=[p3.res(), CB.res()], writes=[t_.res()])
                            self.op("pool", (lambda e, t_=t_, s_=s_, f=f, tsl=tsl: e.tensor_tensor(GT[:, f, tsl], t_[:, 0:G], s_[:, 0:G], ALU.mult)),
                                    reads=[t_.res(), s_.res()], writes=[GT.res()])
                        else:
                            self.op("dve", (lambda e, s_=s_, p3=p3, f=f, tsl=tsl: e.tensor_tensor(GT[:, f, tsl], p3[:, 0:G], s_[:, 0:G], ALU.mult)),
                                    reads=[p3.res(), s_.res()], writes=[GT.res()])
                for dt in range(8):
                    g2 = self.mod_ap(l, 5, dt, col)
                    for tg in range(ntg):
                        tsl = slice(tg * G, (tg + 1) * G)
                        pb = self.next_ps()
                        for f in range(nf):
                            self.op("pe", (lambda e, pb=pb, f=f, dt=dt, w2=w2, tsl=tsl, nf=nf:
                                           e.matmul(pb[:, 0:G], w2[:, f, dt * 128:(dt + 1) * 128], GT[:, f, tsl], start=(f == 0), stop=(f == nf - 1))),
                                    reads=[w2.res(), GT.res()], writes=[pb.res()])
                        self.op("dve", (lambda e, pb=pb, dt=dt, tsl=tsl, g2=g2:
                                        e.scalar_tensor_tensor(X[:, dt, tsl], pb[:, 0:G], g2, X[:, dt, tsl], ALU.mult, ALU.add)),
                                reads=[pb.res(), X.res(), self.modT.res()], writes=[X.res()])
        self.ln_arena_off = 0
        self.layer_norm(X, T, X, 0, lambda dc: lnv[:, 0, dc:dc + 1], lambda dc: lnv[:, 1, dc:dc + 1], LN_EPS / (ALPHA * ALPHA),
                        extra_reads=[lnv.res()])

    def store_out(self, bi):
        self.mark("store_out")
        self.barrier()
        stg = [self.view(i * 16384, [128, 4, D], F32) for i in range(2)]
        for tg in range(L // 512):
            sg_ = stg[tg % 2]
            for j in range(4):
                for half in range(2):
                    pb = self.next_ps()
                    for q in range(4):
                        dc = half * 4 + q
                        self.op("pe", (lambda e, pb=pb, q=q, dc=dc, tg=tg, j=j:
                                       e.transpose(pb[:, q * 128:(q + 1) * 128], self.XT[:, dc, tg * 512 + j * 128:tg * 512 + (j + 1) * 128],
                                                   self.ident_f[:, :])),
                                reads=[self.XT.res(), self.ident_f.res()], writes=[pb.res()])
                    if half == 0:
                        self.op("dve", (lambda e, pb=pb, sg_=sg_, j=j: e.tensor_copy(sg_[:, j, 0:512], pb[:, :])), reads=[pb.res()], writes=[sg_.res()])
                    else:
                        self.op("act", (lambda e, pb=pb, sg_=sg_, j=j: e.copy(sg_[:, j, 512:1024], pb[:, :])), reads=[pb.res()], writes=[sg_.res()])
            dst = self.out[bi, tg * 512:(tg + 1) * 512, :].rearrange("(j p) d -> p j d", p=128)
            self.dma("sp", dst, sg_[:, :, :], reads=[sg_.res()], writes=[sg_.res("out")])


    def hyena_prologue(self, l, T, tag):
        self.mark("hyena_prologue")
        nc = self.nc
        din = self.din
        self.barrier()
        NT = T // 128
        n = 2 * T
        key = (l, tag)
        if not hasattr(self, "khat"):
            self.khat = {}
        kh = nc.dram_tensor("khat_%d_%s" % (l, tag), [2, NT, 128, 2, 512], F32, kind="Internal").ap()
        self.khat[key] = kh
        cst = self.hc[tag]
        o = 0
        z0 = o
        zT = self.view(o, [33, T], F32); o += max(T * 4, 2 * NT * 256 + 8192)
        w1 = self.view(o, [33, 64], F32); o += 256
        w2 = self.view(o, [64, 64], F32); o += 256
        w3 = self.view(o, [64, 2048], F32); o += 8192
        vec = self.view(o, [64, 6], F32); o += 24
        tn = self.view(o, [128, NT], F32); o += NT * 4
        h1 = self.view(o, [64, T], F32); o += T * 4
        h2 = self.view(o, [64, T], F32); o += T * 4
        ti = self.view(o, [64, 512], I32); o += 2048
        b3b = self.view(o, [128, 1024], F32); o += 4096
        dcb = self.view(o, [128, 1024], F32); o += 4096
        biasb = self.view(o, [128, 512], F32); o += 2048
        hrow = [self.view(o + i * 4096, [128, 1024], F32) for i in range(2)]; o += 8192
        win = [self.view(o + i * 2048, [128, 512], F32) for i in range(2)]; o += 4096
        HF = self.view(o, [128, NT, 1024], BF16); o += NT * 2048
        self.dma("sp", zT[:, :], cst["z"], writes=[zT.res()])
        self.dma("sp", w1[:, :], din["hy_f_w1"][l], writes=[w1.res()])
        self.dma("sp", w2[:, :], din["hy_f_w2"][l], writes=[w2.res()])
        self.dma("sp", w3[:, :], din["hy_f_w3"][l], writes=[w3.res()])
        rv_ = vec.res()
        self.dma("sp", vec[:, 0:1], din["hy_f_b1"][l].rearrange("(p o) -> p o", o=1), writes=[rv_])
        self.dma("sp", vec[:, 1:2], din["hy_f_b2"][l].rearrange("(p o) -> p o", o=1), writes=[rv_])
        self.dma("sp", vec[:, 2:3], din["hy_freq"][l][0].rearrange("(p o) -> p o", o=1), writes=[rv_])
        self.dma("sp", vec[:, 3:4], din["hy_freq"][l][1].rearrange("(p o) -> p o", o=1), writes=[rv_])
        self.dma("sp", tn[:, :], cst["tn"], writes=[tn.res()])
        self.op("dve", lambda e: e.tensor_tensor(vec[:, 4:6], vec[:, 0:2], vec[:, 2:4], ALU.mult), reads=[rv_], writes=[rv_])
        self.op("dve", lambda e: e.tensor_scalar(tn[:, :], tn[:, :], -1.0, None, ALU.mult), reads=[tn.res()], writes=[tn.res()])
        G = min(512, T)
        for (src, wt, kk, dst, fcol, bcol) in ((zT, w1, 33, h1, 2, 4), (h1, w2, 64, h2, 3, 5)):
            for tg in range(T // G):
                pb = self.next_ps()
                self.op("pe", (lambda e, pb=pb, wt=wt, src=src, kk=kk, tg=tg:
                               e.matmul(pb[0:64, 0:G], wt[0:kk, :], src[0:kk, tg * G:(tg + 1) * G], start=True, stop=True)),
                        reads=[wt.res(), src.res()], writes=[pb.res()])
                d_ap = dst[:, tg * G:(tg + 1) * G]
                self.op("dve", (lambda e, pb=pb, d_ap=d_ap, fcol=fcol, bcol=bcol:
                                e.tensor_scalar(d_ap, pb[0:64, 0:G], vec[:, fcol:fcol + 1], vec[:, bcol:bcol + 1], ALU.mult, ALU.add)),
                        reads=[pb.res(), rv_], writes=[dst.res()])
                self.range_reduce("dve", d_ap, ti[:, 0:G], [dst.res()], dst.res())
                self.op("act", (lambda e, d_ap=d_ap: e.activation(out=d_ap, in_=d_ap, func=AF.Sin)),
                        reads=[dst.res()], writes=[dst.res()])
        self.barrier()
        fw = [self.view(z0 + i * NT * 256, [128, NT, 128], BF16) for i in range(2)]
        kst = [self.view(z0 + 2 * NT * 256 + i * 4096, [128, 2, 512], F32) for i in range(2)]
        it = 0
        for oo in range(2):
            self.dma("sp", b3b[:, :], din["hy_f_b3"][l][oo * 1024:(oo + 1) * 1024].partition_broadcast(128), writes=[b3b.res()])
            self.dma("sp", dcb[:, :], din["hy_decay"][l][oo * 1024:(oo + 1) * 1024].partition_broadcast(128), writes=[dcb.res()])
            self.dma("sp", biasb[:, :], din["hy_bias"][l][oo].partition_broadcast(128), writes=[biasb.res()])
            self.op("act", lambda e: e.activation(out=dcb[:, :], in_=dcb[:, :], func=AF.Abs), reads=[dcb.res()], writes=[dcb.res()])
            for tt in range(NT):
                hr = hrow[tt % 2]
                for cg in range(2):
                    pb = self.next_ps()
                    self.op("pe", (lambda e, pb=pb, tt=tt, cg=cg, oo=oo:
                                   e.matmul(pb[:, :], h2[:, tt * 128:(tt + 1) * 128], w3[:, oo * 1024 + cg * 512:oo * 1024 + (cg + 1) * 512],
                                            start=True, stop=True)),
                            reads=[h2.res(), w3.res()], writes=[pb.res()])
                    wn = win[cg % 2]
                    self.op("act", (lambda e, wn=wn, cg=cg, tt=tt: e.activation(out=wn[:, :], in_=dcb[:, cg * 512:(cg + 1) * 512], func=AF.Exp,
                                                                            scale=tn[:, tt:tt + 1])),
                            reads=[dcb.res(), tn.res()], writes=[wn.res()])
                    self.op("dve", (lambda e, pb=pb, hr=hr, cg=cg: e.tensor_tensor(hr[:, cg * 512:(cg + 1) * 512], pb[:, :], b3b[:, cg * 512:(cg + 1) * 512], ALU.add)),
                            reads=[pb.res(), b3b.res()], writes=[hr.res()])
                    self.op("pool", (lambda e, hr=hr, wn=wn, cg=cg: e.tensor_tensor(hr[:, cg * 512:(cg + 1) * 512], hr[:, cg * 512:(cg + 1) * 512], wn[:, :], ALU.mult)),
                            reads=[hr.res(), wn.res()], writes=[hr.res()])
                if tt == 0:
                    self.op("pool", (lambda e, hr=hr: e.memset(hr[0:1, 512:1024], 0.0)), reads=[hr.res()], writes=[hr.res()])
                self.op("dve", (lambda e, tt=tt, hr=hr: e.tensor_tensor(HF[:, tt, 0:512], hr[:, 0:512], hr[:, 512:1024], ALU.add)),
                        reads=[hr.res()], writes=[HF.res()])
                self.op("pool", (lambda e, tt=tt, hr=hr: e.tensor_tensor(HF[:, tt, 512:1024], hr[:, 0:512], hr[:, 512:1024], ALU.subtract)),
                        reads=[hr.res()], writes=[HF.res()])
            for j in range(NT):
                ks = kst[j % 2]
                for cs in range(2):
                    f_ = fw[it % 2]
                    it += 1
                    ft = cs * NT + j
                    self.dma("sp", f_[:, :, :], cst["fwd"][ft], writes=[f_.res()])
                    pb = self.next_ps()
                    for c in range(NT):
                        self.op("pe", (lambda e, pb=pb, f_=f_, c=c, cs=cs:
                                       e.matmul(pb[:, :], f_[:, c, :], HF[:, c, cs * 512:cs * 512 + 512],
                                                start=(c == 0), stop=(c == NT - 1))),
                                reads=[f_.res(), HF.res()], writes=[pb.res()])
                    if cs == 0:
                        self.op("dve", (lambda e, ks=ks, pb=pb: e.tensor_tensor(ks[:, 0, :], pb[:, :], biasb[:, :], ALU.add)),
                                reads=[pb.res(), biasb.res()], writes=[ks.res()])
                        self.op("act", (lambda e, ks=ks: e.mul(ks[:, 0, :], ks[:, 0, :], 2.0 / n)),
                                reads=[ks.res()], writes=[ks.res()])
                    else:
                        self.op("act", (lambda e, ks=ks, pb=pb: e.mul(ks[:, 1, :], pb[:, :], 2.0 / n)),
                                reads=[pb.res()], writes=[ks.res()])
                self.dma("sp", kh[oo, j], ks[:, :, :], reads=[ks.res()], writes=[ks.res("out")])

    def hyena_inproj_conv(self, l, col0, hoff, T, Wh, shw, stage, ftmp, dst, dst_is_tok):
        din = self.din
        self.load_w_bf16(Wh[:, :, :], din["w_in"][l][:, col0:col0 + 512].rearrange("(k p) m -> p k m", p=128), Wh.res())
        rH = self.HT.res()
        G = min(512, T)
        cbuf = ftmp["cbuf"]
        for j in range(4):
            jt = col0 // 128 + j
            self.op("pool", (lambda e: e.memset(stage[:, 0:1], 0.0)), writes=[stage.res()])
            self.op("pool", (lambda e: e.memset(stage[:, T + 1:T + 2], 0.0)), writes=[stage.res()])
            for tg in range(T // G):
                pb = self.next_ps()
                for kc in range(8):
                    self.op("pe", (lambda e, kc=kc, pb=pb, j=j, tg=tg:
                                   e.matmul(pb[:, 0:G], Wh[:, kc, j * 128:(j + 1) * 128],
                                            self.HT[:, kc, hoff + tg * G:hoff + (tg + 1) * G], start=(kc == 0), stop=(kc == 7))),
                            reads=[Wh.res(), rH], writes=[pb.res()])
                self.op("act", (lambda e, pb=pb, tg=tg: e.copy(stage[:, 1 + tg * G:1 + (tg + 1) * G], pb[:, 0:G])),
                        reads=[pb.res()], writes=[stage.res()])
            w0 = shw[:, jt, 0:1]; w1 = shw[:, jt, 1:2]; w2 = shw[:, jt, 2:3]; bb = shw[:, jt, 3:4]
            tmp = ftmp["tmp"]
            self.op("dve", (lambda e, w1=w1, bb=bb: e.tensor_scalar(tmp[:, 0:T], stage[:, 1:T + 1], w1, bb, ALU.mult, ALU.add)),
                    reads=[stage.res(), shw.res()], writes=[tmp.res()])
            self.op("dve", (lambda e, w0=w0: e.scalar_tensor_tensor(tmp[:, 0:T], stage[:, 0:T], w0, tmp[:, 0:T], ALU.mult, ALU.add)),
                    reads=[stage.res(), shw.res(), tmp.res()], writes=[tmp.res()])
            o_ap = cbuf[:, j, 0:T] if dst_is_tok else dst[:, j, 0:T]
            o_res = cbuf.res() if dst_is_tok else dst.res()
            self.op("dve", (lambda e, w2=w2, o_ap=o_ap: e.scalar_tensor_tensor(o_ap, stage[:, 2:T + 2], w2, tmp[:, 0:T], ALU.mult, ALU.add)),
                    reads=[stage.res(), shw.res(), tmp.res()], writes=[o_res])
        if dst_is_tok:
            for tt in range(T // 128):
                pb = self.next_ps()
                pbb = pb[:, 0:256].bitcast(BF16)
                for j in range(4):
                    self.op("pe", (lambda e, pbb=pbb, j=j, tt=tt: e.transpose(pbb[:, j * 128:(j + 1) * 128], cbuf[:, j, tt * 128:(tt + 1) * 128],
                                                                              self.ident_b[:, :])),
                            reads=[cbuf.res(), self.ident_b.res()], writes=[pb.res()])
                if tt % 2 == 0:
                    self.op("dve", (lambda e, pbb=pbb, tt=tt: e.tensor_copy(dst[:, tt, :], pbb[:, 0:512])), reads=[pb.res()], writes=[dst.res()])
                else:
                    self.op("act", (lambda e, pbb=pbb, tt=tt: e.copy(dst[:, tt, :], pbb[:, 0:512])), reads=[pb.res()], writes=[dst.res()])

    def hyena_fwd_mul(self, l, tag, oo, T, U, YH, fw, kst, ctmp):
        NT = T // 128
        cst = self.hc[tag]
        kh = self.khat[(l, tag)]
        it = 0
        for j in range(NT):
            ks = kst[j % 2]
            self.dma("sp", ks[:, :, :], kh[oo, j], writes=[ks.res()])
            pab = []
            for cs in range(2):
                f_ = fw[it % 2]
                it += 1
                self.dma("sp", f_[:, 0:NT, :], cst["fwd"][cs * NT + j], writes=[f_.res()])
                pb = self.next_ps(reserve=True)
                pab.append(pb)
                for c in range(NT):
                    self.op("pe", (lambda e, pb=pb, f_=f_, c=c: e.matmul(pb[:, :], f_[:, c, :], U[:, c, :], start=(c == 0), stop=(c == NT - 1))),
                            reads=[f_.res(), U.res()], writes=[pb.res()])
            self.release_ps(pab)
            pa, pbm = pab
            t0, t1, t2, t3 = (ctmp[:, i, :] for i in range(4))
            rc = ctmp.res()
            self.op("dve", (lambda e, pa=pa, ks=ks, t0=t0: e.tensor_tensor(t0, pa[:, :], ks[:, 0, :], ALU.mult)), reads=[pa.res(), ks.res()], writes=[rc])
            self.op("dve", (lambda e, pbm=pbm, ks=ks, t1=t1: e.tensor_tensor(t1, pbm[:, :], ks[:, 1, :], ALU.mult)), reads=[pbm.res(), ks.res()], writes=[rc])
            self.op("dve", (lambda e, pa=pa, ks=ks, t2=t2: e.tensor_tensor(t2, pa[:, :], ks[:, 1, :], ALU.mult)), reads=[pa.res(), ks.res()], writes=[rc])
            self.op("dve", (lambda e, pbm=pbm, ks=ks, t3=t3: e.tensor_tensor(t3, pbm[:, :], ks[:, 0, :], ALU.mult)), reads=[pbm.res(), ks.res()], writes=[rc])
            self.op("pool", (lambda e, j=j, t0=t0, t1=t1: e.tensor_tensor(YH[:, j, :], t0, t1, ALU.subtract)), reads=[rc], writes=[YH.res()])
            self.op("pool", (lambda e, j=j, t2=t2, t3=t3: e.tensor_tensor(YH[:, NT + j, :], t2, t3, ALU.add)), reads=[rc], writes=[YH.res()])

    def hyena_branch(self, l, hoff, T, tag, ab, out_view):
        self.mark("hyena_branch")
        din = self.din
        NT = T // 128
        NF = 2 * NT
        cst = self.hc[tag]
        o = ab
        VT = self.view(o, [128, NT, 512], BF16); o += NT * 1024
        X1 = View(out_view.t.rearrange("p a b -> p (a b)").rearrange("p (n c) -> p n c", c=512))
        X1._r = out_view._r
        YH = self.view(o, [128, NF, 512], BF16); yh0 = o; o += NF * 1024
        ctmp = self.view(o, [128, 4, 512], F32); ct0 = o; o += 8192
        shw = self.view(o, [128, 12, 4], F32); o += 192
        r1 = o
        Wh = self.view(r1, [128, 8, 512], BF16)
        stage = self.view(r1 + 8192, [128, T + 2], F32)
        tmp = self.view(ct0, [128, T], F32)
        tmp._r = ctmp._r
        cbuf = self.view(yh0, [128, 4, T], BF16)
        cbuf._r = YH._r
        ft = {"tmp": tmp, "cbuf": cbuf}
        o2 = r1
        fw = [self.view(o2 + i * NF * 256, [128, NF, 128], BF16) for i in range(2)]; o2 += 2 * NF * 256
        kst = [self.view(o2 + i * 4096, [128, 2, 512], F32) for i in range(2)]; o2 += 8192
        ivb = [self.view(r1 + i * 8 * min(512, T) * 2, [128, 8, min(512, T)], BF16) for i in range(2)]
        for k in range(3):
            self.dma("sp", shw[:, :, k], din["hy_short_w"][l][k].rearrange("(j p) -> p j", p=128), writes=[shw.res()])
        self.dma("sp", shw[:, :, 3], din["hy_short_b"][l].rearrange("(j p) -> p j", p=128), writes=[shw.res()])
        self.hyena_inproj_conv(l, 1024, hoff, T, Wh, shw, stage, ft, VT, True)
        self.hyena_inproj_conv(l, 0, hoff, T, Wh, shw, stage, ft, X1, True)
        self.barrier()
        self.hyena_fwd_mul(l, tag, 0, T, VT, YH, fw, kst, ctmp)
        for tt in range(NT):
            f_ = fw[tt % 2]
            self.dma("sp", f_[:, :, :], cst["inva"][tt], writes=[f_.res()])
            pb = self.next_ps()
            for c in range(NF):
                self.op("pe", (lambda e, pb=pb, f_=f_, c=c: e.matmul(pb[:, :], f_[:, c, :], YH[:, c, :], start=(c == 0), stop=(c == NF - 1))),
                        reads=[f_.res(), YH.res()], writes=[pb.res()])
            self.op("dve", (lambda e, pb=pb, tt=tt: e.tensor_tensor(X1[:, tt, :], pb[:, :], X1[:, tt, :], ALU.mult)),
                    reads=[pb.res(), X1.res()], writes=[X1.res()])
        self.hyena_fwd_mul(l, tag, 1, T, X1, YH, fw, kst, ctmp)
        self.barrier()
        X2 = View(VT.t.rearrange("p a b -> p (a b)").rearrange("p (j t) -> p j t", j=4))
        self.hyena_inproj_conv(l, 512, hoff, T, Wh, shw, stage, ft, X2, False)
        self.barrier()
        G = min(512, T)
        for tg in range(T // G):
            pj = [self.next_ps(reserve=True) for _ in range(4)]
            self.release_ps(pj)
            ngrp = (NF + 7) // 8
            for gq in range(ngrp):
                nq = min(8, NF - gq * 8)
                iv = ivb[(tg * ngrp + gq) % 2]
                self.dma("sp", iv[:, 0:nq, :], cst["invb"][tg][:, gq * 8:gq * 8 + nq, :], writes=[iv.res()])
                for j in range(4):
                    for q in range(nq):
                        c = gq * 8 + q
                        self.op("pe", (lambda e, pj=pj, j=j, q=q, c=c, iv=iv: e.matmul(pj[j][:, 0:G], YH[:, c, j * 128:(j + 1) * 128], iv[:, q, :],
                                                                                     start=(c == 0), stop=(c == NF - 1))),
                                reads=[iv.res(), YH.res()], writes=[pj[j].res()])
            for j in range(4):
                self.op("dve", (lambda e, pj=pj, j=j, tg=tg: e.tensor_tensor(out_view[:, j, tg * G:(tg + 1) * G], pj[j][:, 0:G],
                                                                            X2[:, j, tg * G:(tg + 1) * G], ALU.mult)),
                        reads=[pj[j].res(), X2.res()], writes=[out_view.res()])


def _dft_consts(T):
    import ml_dtypes
    n = 2 * T
    NT = T // 128
    t = np.arange(T, dtype=np.float64)[:, None]
    f = np.arange(T, dtype=np.float64)[None, :]
    ang = 2.0 * np.pi * (f + 0.5) * t / n
    fwd = np.concatenate([np.cos(ang), np.sin(ang)], axis=1)
    fwd_t = fwd.reshape(NT, 128, 2 * NT, 128).transpose(2, 1, 0, 3)
    inv = fwd.T
    inva = inv.reshape(2 * NT, 128, NT, 128).transpose(2, 1, 0, 3)
    G = min(512, T)
    invb = inv.reshape(2 * NT, 128, T // G, G).transpose(2, 1, 0, 3)
    bf = ml_dtypes.bfloat16
    tt_ = np.arange(T, dtype=np.float32)[:, None]
    tn = tt_ / max(T - 1, 1)
    bands = np.arange(1, 17, dtype=np.float32)
    a2 = tt_ * bands * np.float32(2.0 * math.pi / T)
    z = np.concatenate([tn, np.cos(a2), np.sin(a2)], axis=-1).astype(np.float32)
    return {"fwd": np.ascontiguousarray(fwd_t).astype(bf), "inva": np.ascontiguousarray(inva).astype(bf),
            "invb": np.ascontiguousarray(invb).astype(bf), "z": np.ascontiguousarray(z.T),
            "tn": np.ascontiguousarray(tn.reshape(NT, 128).T)}


_CONSTS = {}


def _consts():
    if not _CONSTS:
        for tag, T in (("L", L), ("C", LC)):
            for k_, v_ in _dft_consts(T).items():
                _CONSTS["hc_%s_%s" % (tag, k_)] = v_
    return _CONSTS


def _shapes(inputs):
    shp = {n: tuple(inputs[n].shape) for n in INPUT_NAMES}
    shp["x"] = (NB, L, D)
    shp["c"] = (NB, D)
    shp["ctx"] = (NB, LC, D)
    return shp


def run(inputs, debug=None, cores=8):
    inputs = {k: np.ascontiguousarray(np.asarray(v, dtype=np.float32)) for k, v in inputs.items()}
    k = K(_shapes(inputs), debug=debug)
    nc = k.build()
    in_maps = []
    for ci in range(cores):
        m = {n: inputs[n] for n in INPUT_NAMES}
        m["x"] = inputs["x"][ci * NB:(ci + 1) * NB]
        m["c"] = inputs["c"][ci * NB:(ci + 1) * NB]
        m["ctx"] = inputs["ctx"][ci * NB:(ci + 1) * NB]
        m.update(_consts())
        in_maps.append(m)
    res = run_bass_kernel_spmd(nc, in_maps, core_ids=list(range(cores)))
    return res


def kernel(**inputs):
    res = run(inputs)
    return np.concatenate([r["out"] for r in res.results], axis=0)
```

```python
import math
from contextlib import ExitStack
import numpy as np
import concourse.bass as bass
import concourse.mybir as mybir
from concourse.bass_utils import run_bass_kernel_spmd

F32 = mybir.dt.float32
BF16 = mybir.dt.bfloat16
I32 = mybir.dt.int32
ALU = mybir.AluOpType
AF = mybir.ActivationFunctionType
AX = mybir.AxisListType

D = 1024
L = 2048
LC = 256
NB = 2
DEPTH = 2
IN_COLS = 6144
D_FF = 2816
D_EXP = 3584
NEXP = 8
ALPHA = (2.0 * DEPTH) ** 0.25
LN_EPS = 1e-5
TWO_PI = 2.0 * math.pi

ENGS = ("pe", "dve", "act", "pool", "sp")


class Res:
    __slots__ = ("w", "r", "sem", "epoch", "pr")

    def __init__(self):
        self.w = None
        self.r = {}
        self.sem = None
        self.epoch = -1
        self.pr = {}


class Prog:
    def __init__(self, nc):
        self.nc = nc
        self.ops = {e: [] for e in ENGS}
        self.cnt = {}
        self.waited = {e: {} for e in ENGS}
        self.semkeys = list(ENGS)

    def new_dma_sem(self):
        k = "dma%d" % (len(self.semkeys) - len(ENGS))
        self.semkeys.append(k)
        return k

    def op(self, eng, fn, reads=(), writes=(), dma_sem=None):
        need = {}
        rawself = 0
        for r in reads:
            if r.w is not None:
                k, v = r.w
                if need.get(k, 0) < v:
                    need[k] = v
                if k == eng and v > rawself:
                    rawself = v
        for w in writes:
            if w.w is not None:
                k, v = w.w
                if dma_sem is not None and k == dma_sem:
                    for k2, v2 in w.pr.items():
                        if need.get(k2, 0) < v2:
                            need[k2] = v2
                elif need.get(k, 0) < v:
                    need[k] = v
            for k, v in w.r.items():
                if need.get(k, 0) < v:
                    need[k] = v
        key = eng if dma_sem is None else dma_sem
        amt = 1 if dma_sem is None else 16
        waits = []
        wd = self.waited[eng]
        for k, v in need.items():
            if k == eng and dma_sem is None:
                if eng == "pe" or rawself == 0:
                    continue
                v = rawself
            if wd.get(k, 0) < v:
                waits.append((k, v))
                wd[k] = v
        val = self.cnt.get(key, 0) + amt
        self.cnt[key] = val
        self.ops[eng].append((waits, fn, key, amt))
        for r in reads:
            if r.r.get(key, 0) < val:
                r.r[key] = val
        for w in writes:
            same_group = dma_sem is not None and w.w is not None and w.w[0] == dma_sem
            npr = dict(w.r)
            if same_group:
                for k2, v2 in w.pr.items():
                    if npr.get(k2, 0) < v2:
                        npr[k2] = v2
            elif w.w is not None:
                if npr.get(w.w[0], 0) < w.w[1]:
                    npr[w.w[0]] = w.w[1]
            w.pr = npr
            w.w = (key, val)
            w.r = {}
        return val

    def wait_all(self, eng, res_list):
        need = {}
        for r in res_list:
            if r.w is not None:
                need[r.w[0]] = max(need.get(r.w[0], 0), r.w[1])
            for k, v in r.r.items():
                need[k] = max(need.get(k, 0), v)
        self.ops[eng].append((list(need.items()), None, None, 0))

    def emit(self, stack):
        nc = self.nc
        sems = {}
        for k in self.semkeys:
            sems[k] = stack.enter_context(nc.semaphore("s_" + k))
        stack.enter_context(nc.allow_non_contiguous_dma(reason="small strided parameter loads"))
        block = stack.enter_context(nc.Block())
        ops = self.ops

        def run(name, e):
            for waits, fn, key, amt in ops[name]:
                for k, v in waits:
                    e.wait_ge(sems[k], v)
                if fn is not None:
                    fn(e).then_inc(sems[key], amt)

        @block.tensor
        def _(e):
            run("pe", e)

        @block.vector
        def _(e):
            run("dve", e)

        @block.scalar
        def _(e):
            run("act", e)

        @block.gpsimd
        def _(e):
            run("pool", e)

        @block.sync
        def _(e):
            run("sp", e)


class Buf:
    def __init__(self, t):
        self.t = t
        self._r = {}

    def res(self, key=None):
        r = self._r.get(key)
        if r is None:
            r = self._r[key] = Res()
        return r

    def __getitem__(self, idx):
        return self.t[idx]


class View(Buf):
    pass


INPUT_NAMES = ["x", "c", "ctx", "c_ctx", "w_mod", "b_mod", "w_in",
               "hy_short_w", "hy_short_b", "hy_f_w1", "hy_f_b1", "hy_f_w2", "hy_f_b2", "hy_f_w3", "hy_f_b3",
               "hy_freq", "hy_decay", "hy_bias", "w_hy_out",
               "cf_dw_w", "cf_dw_b", "cf_ln_g", "cf_ln_b", "w_cf_out",
               "s5_a_re", "s5_a_im", "s5_log_dt", "s5_b_re", "s5_b_im", "s5_c_re", "s5_c_im",
               "s5_d", "s5_w_glu", "s5_b_glu", "w_s5_out",
               "w_o", "ln1_g", "ln1_b", "ln2_g", "ln2_b",
               "ffn_w1", "ffn_w3", "ffn_w2", "moe_router", "moe_w1", "moe_w3", "moe_w2"]


class K:
    def __init__(self, shapes, debug=None):
        self.debug = debug or {}
        nc = self.nc = bass.Bass("TRN2", target_bir_lowering=False)
        self.P = Prog(nc)
        self.st = ExitStack()
        self.din = {}
        for n in INPUT_NAMES:
            self.din[n] = nc.dram_tensor(n, list(shapes[n]), F32, kind="ExternalInput").ap()
        self.out = nc.dram_tensor("out", [NB, L, D], F32, kind="ExternalOutput").ap()
        self.hc = {"L": {}, "C": {}}
        for name, arr in _consts().items():
            _, tag, k_ = name.split("_", 2)
            dt_ = F32 if arr.dtype == np.float32 else BF16
            self.hc[tag][k_] = nc.dram_tensor(name, list(arr.shape), dt_, kind="ExternalInput").ap()
        self.dbg_out = {}
        for n, shp in self.debug.items():
            if n.startswith("_"):
                continue
            self.dbg_out[n] = nc.dram_tensor("dbg_" + n, list(shp), F32, kind="ExternalOutput").ap()
        self.out_sem = self.P.new_dma_sem()
        self.out_sem_sw = self.P.new_dma_sem()
        self.out_res = Res()
        self.out_res_sw = Res()
        self.scr_sem = self.P.new_dma_sem()
        self.sem_pool = {"hw": [self.P.new_dma_sem() for _ in range(44)],
                         "sw": [self.P.new_dma_sem() for _ in range(40)]}
        self.sem_next = {"hw": 0, "sw": 0}
        self.epoch = 0

    def sb(self, name, shape, dt):
        return Buf(self.st.enter_context(self.nc.sbuf_tensor(name, list(shape), dt)))

    def ps(self, name, shape, dt):
        return Buf(self.st.enter_context(self.nc.psum_tensor(name, list(shape), dt)))

    def view(self, off, shape, dt):
        esz = 4 if dt in (F32, I32) else 2
        n = 1
        for d_ in shape[1:]:
            n *= d_
        nbytes = n * esz
        assert off % 4 == 0 and off + nbytes <= self.ARENA_BYTES, (off, nbytes, self.ARENA_BYTES)
        self.arena_hi = max(getattr(self, "arena_hi", 0), off + nbytes)
        ap = self.arena.t[0:shape[0], off // 4:(off + nbytes + 3) // 4]
        if dt != F32:
            ap = ap.bitcast(dt)
        if len(shape) > 2:
            names = "abcdefg"[:len(shape) - 1]
            kw = {names[i]: shape[1 + i] for i in range(len(shape) - 2)}
            ap = ap.rearrange("p (%s) -> p %s" % (" ".join(names), " ".join(names)), **kw)
        return View(ap)

    def mark(self, name):
        if not hasattr(self, "marks"):
            self.marks = []
        self.marks.append((name, sum(1 for o_ in self.P.ops["pe"] if o_[1] is not None)))

    def barrier(self):
        cur = dict(self.P.cnt)
        for e in ENGS:
            waits = []
            for k, v in cur.items():
                if k == e and e == "pe":
                    continue
                if self.P.waited[e].get(k, 0) < v:
                    waits.append((k, v))
                    self.P.waited[e][k] = v
            if waits:
                self.P.ops[e].append((waits, None, None, 0))
        self.epoch += 1
        self.sem_next = {"hw": 0, "sw": 0}

    def op(self, eng, fn, reads=(), writes=(), dma_sem=None):
        return self.P.op(eng, fn, reads, writes, dma_sem)

    def dma(self, eng, out, in_, reads=(), writes=(), sem=None, **kw):
        if sem is None:
            w = writes[0]
            kind = "sw" if eng == "pool" else "hw"
            if w.sem is None or w.epoch != (self.epoch, kind):
                assert self.sem_next[kind] < len(self.sem_pool[kind]), "out of DMA semaphores in this phase"
                w.sem = self.sem_pool[kind][self.sem_next[kind]]
                self.sem_next[kind] += 1
                w.epoch = (self.epoch, kind)
            sem = w.sem
        return self.P.op(eng, lambda e: e.dma_start(out=out, in_=in_, **kw), reads, writes, dma_sem=sem)

    def dbg(self, name, src_ap, res):
        if name in self.dbg_out:
            self.dma("sp", self.dbg_out[name], src_ap, reads=[res], writes=[Res()])

    def build(self):
        nc = self.nc
        P = self.P
        din = self.din
        self.ident_f = self.sb("ident_f", [128, 128], F32)
        self.ident_b = self.sb("ident_b", [128, 128], BF16)
        self.ones_f = self.sb("ones_f", [128, 128], F32)
        rI = self.ident_f.res()
        self.op("pool", lambda e: e.memset(self.ident_f[:], 0.0), writes=[rI])
        self.op("pool", lambda e: e.affine_select(self.ident_f[:], self.ident_f[:], pattern=[[-1, 128]],
                                                  compare_op=ALU.not_equal, fill=1.0, base=0, channel_multiplier=1),
                reads=[rI], writes=[rI])
        self.op("pool", lambda e: e.tensor_copy(self.ident_b[:], self.ident_f[:]), reads=[rI],
                writes=[self.ident_b.res()])
        self.op("pool", lambda e: e.memset(self.ones_f[:], 1.0 / D), writes=[self.ones_f.res()])
        self.psb = [self.ps("psb%d" % i, [128, 512], F32) for i in range(8)]
        self._psi = 0
        self.XT = self.sb("XT", [128, 8, L], F32)
        self.ARENA_BYTES = 102 * 1024
        self.arena = self.sb("arena", [128, self.ARENA_BYTES // 4], F32)

        self.ones_cf = self.sb("ones_cf", [128, 128], F32)
        self.op("pool", lambda e: e.memset(self.ones_cf[:], 1.0 / 512), writes=[self.ones_cf.res()])
        self.HT = self.sb("HT", [128, 8, L + LC], BF16)
        self.s5fin = self.sb("s5fin", [128, 2, 2, 16], F32)
        stop = self.debug.get("_stop", "")
        self.compute_mods()
        for l in range(DEPTH):
            self.s5_prologue(l)
            self.hyena_prologue(l, L, "L")
        self.hyena_prologue(0, LC, "C")
        XC = self.view(self.ARENA_BYTES - 8192, [128, 8, LC], F32)
        for bi in range(NB):
            self.load_tokens(din["ctx"][bi], LC, XC)
            self.ln_arena_off = 0
            self.layer_norm(XC, LC, self.HT, L, lambda dc: self.mod_ap(0, 1, dc, 2), lambda dc: self.mod_ap(0, 0, dc, 2), LN_EPS,
                            extra_reads=[self.modT.res()])
            self.barrier()
            ZHc = self.view(0, [128, 4, LC], BF16)
            UCFc = self.view(2048, [128, 4, LC], BF16)
            YSc = self.view(4096, [128, 4, LC], BF16)
            self.hyena_branch(0, L, LC, "C", 8192, ZHc)
            self.barrier()
            self.conformer(0, L, LC, LC, 8192, UCFc)
            self.barrier()
            self.s5_branch(0, L, LC, 8192, YSc, False, True)
            self.barrier()
            self.mixer_out(0, L, LC, XC, 2, ZHc, UCFc, YSc, 8192)
            self.ffn(0, LC, XC, 2, False, hoff=L)
            self.ln_arena_off = 0
            self.layer_norm(XC, LC, self.HT, L, lambda dc: self.mod_ap(1, 1, dc, 2), lambda dc: self.mod_ap(1, 0, dc, 2), LN_EPS,
                            extra_reads=[self.modT.res()])
            self.load_tokens(din["x"][bi], L, self.XT)
            for l in range(DEPTH):
                if l == 1:
                    self.barrier()
                    YSc = self.view(4096, [128, 4, LC], BF16)
                    self.s5_branch(1, L, LC, 8192, YSc, False, True, finals_only=True)
                self.ln_arena_off = 0
                self.layer_norm(self.XT, L, self.HT, 0, (lambda dc, l=l, bi=bi: self.mod_ap(l, 1, dc, bi)),
                                (lambda dc, l=l, bi=bi: self.mod_ap(l, 0, dc, bi)), LN_EPS, extra_reads=[self.modT.res()])
                self.barrier()
                ZH = self.view(0, [128, 4, L], BF16)
                UCF = self.view(16384, [128, 4, L], BF16)
                YS = self.view(32768, [128, 4, L], BF16)
                self.hyena_branch(l, 0, L, "L", 16384, ZH)
                self.barrier()
                self.conformer(l, 0, L, 64, 32768, UCF)
                self.barrier()
                self.s5_branch(l, 0, L, 49152, YS, True, False)
                self.barrier()
                self.mixer_out(l, 0, L, self.XT, bi, ZH, UCF, YS, 49152)
                if stop == "mix%d" % l and bi == 0:
                    self.dbg("xt", self.XT[:, :, :], self.XT.res())
                self.ffn(l, L, self.XT, bi, moe=(l == 1))
                if stop == "ffn%d" % l and bi == 0:
                    self.dbg("xt", self.XT[:, :, :], self.XT.res())
            self.store_out(bi)
        self.barrier()
        P.wait_all("sp", [self.out_res, self.out_res_sw])
        self.mark("end")
        P.emit(self.st)
        self.st.close()
        return nc

    def next_ps(self, reserve=False):
        if not hasattr(self, "_reserved"):
            self._reserved = set()
        while True:
            i = self._psi % 8
            self._psi += 1
            if i not in self._reserved:
                break
        if reserve:
            self._reserved.add(i)
        return self.psb[i]

    def release_ps(self, bufs):
        for b in bufs:
            self._reserved.discard(self.psb.index(b))

    def load_tokens(self, src, T, dst, stage_off=0):
        self.mark("load_tokens")
        self.barrier()
        G = min(512, T)
        nj = G // 128
        xstage = [self.view(stage_off + i * 16384, [128, 4, D], F32) for i in range(2)]
        for tg in range(T // G):
            stg = xstage[tg % 2]
            sv = src[tg * G:(tg + 1) * G, :].rearrange("(j p) d -> p j d", p=128)
            for jj in range(nj):
                self.dma("sp" if jj % 2 == 0 else "act", stg[:, jj, :], src[tg * G + jj * 128:tg * G + (jj + 1) * 128, :], writes=[stg.res()])
            for dc in range(8):
                pb = self.next_ps()
                for j in range(nj):
                    self.op("pe", (lambda e, pb=pb, stg=stg, j=j, dc=dc:
                                   e.transpose(pb[:, j * 128:(j + 1) * 128], stg[:, j, dc * 128:(dc + 1) * 128],
                                               self.ident_f[:])),
                            reads=[stg.res(), self.ident_f.res()], writes=[pb.res()])
                d_ap = dst[:, dc, tg * G:(tg + 1) * G]
                if dc % 2 == 0:
                    self.op("dve", (lambda e, d_ap=d_ap, pb=pb: e.tensor_copy(d_ap, pb[:, 0:G])),
                            reads=[pb.res()], writes=[dst.res()])
                else:
                    self.op("act", (lambda e, d_ap=d_ap, pb=pb: e.copy(d_ap, pb[:, 0:G])),
                            reads=[pb.res()], writes=[dst.res()])

    def compute_mods(self):
        self.mark("compute_mods")
        din = self.din
        self.cT = self.sb("cT", [128, 8, 3], F32)
        self.modT = self.sb("modT", [128, DEPTH, 48, 3], F32)
        self.bmodT = self.sb("bmodT", [128, DEPTH, 48], F32)
        rc = self.cT.res()
        for bi in range(NB):
            self.dma("sp", self.cT[:, :, bi], din["c"][bi].rearrange("(j p) -> p j", p=128), writes=[rc])
        self.dma("sp", self.cT[:, :, 2], din["c_ctx"].rearrange("(j p) -> p j", p=128), writes=[rc])
        for l in range(DEPTH):
            self.dma("sp", self.bmodT[:, l, :], din["b_mod"][l].rearrange("(j p) -> p j", p=128),
                     writes=[self.bmodT.res()])
        self.op("act", lambda e: e.activation(out=self.cT[:, :, :], in_=self.cT[:, :, :], func=AF.Silu),
                reads=[rc], writes=[rc])
        self.barrier()
        self.wmod_stage = [self.view(i * 16384, [128, 8, 512], F32) for i in range(2)]
        gi = 0
        for l in range(DEPTH):
            for mg in range(12):
                stg = self.wmod_stage[gi % 2]
                gi += 1
                src = din["w_mod"][l][:, mg * 512:(mg + 1) * 512].rearrange("(k p) m -> p k m", p=128)
                self.dma("sp", stg[:, 0:4, :], src[:, 0:4, :], writes=[stg.res()])
                self.dma("act", stg[:, 4:8, :], src[:, 4:8, :], writes=[stg.res()])
                pb = self.next_ps()
                for mt in range(4):
                    for kc in range(8):
                        self.op("pe", (lambda e, pb=pb, stg=stg, mt=mt, kc=kc:
                                       e.matmul(pb[:, mt * 3:(mt + 1) * 3], stg[:, kc, mt * 128:(mt + 1) * 128],
                                                self.cT[:, kc, :], start=(kc == 0), stop=(kc == 7))),
                                reads=[stg.res(), rc], writes=[pb.res()])
                for mt in range(4):
                    j = mg * 4 + mt
                    self.op("dve", (lambda e, pb=pb, mt=mt, j=j, l=l:
                                    e.tensor_scalar(self.modT[:, l, j, :], pb[:, mt * 3:(mt + 1) * 3],
                                                    self.bmodT[:, l, j:j + 1], None, ALU.add)),
                            reads=[pb.res(), self.bmodT.res()], writes=[self.modT.res()])
        rm = self.modT.res()
        for l in range(DEPTH):
            for i in (1, 4):
                self.op("dve", (lambda e, l=l, i=i: e.tensor_scalar(self.modT[:, l, 8 * i:8 * i + 8, :],
                                                                     self.modT[:, l, 8 * i:8 * i + 8, :],
                                                                     1.0, None, ALU.add)),
                        reads=[rm], writes=[rm])
            for i in (2, 5):
                self.op("dve", (lambda e, l=l, i=i: e.tensor_scalar(self.modT[:, l, 8 * i:8 * i + 8, :],
                                                                     self.modT[:, l, 8 * i:8 * i + 8, :],
                                                                     1.0 / ALPHA, None, ALU.mult)),
                        reads=[rm], writes=[rm])

    def mod_ap(self, l, which, dc, col):
        return self.modT[:, l, 8 * which + dc, col:col + 1]

    def layer_norm(self, src, T, dst, dst_off, scale_fn, bias_fn, eps, extra_reads=(), no_barrier=False, hf32=None, post_group=None):
        if not no_barrier:
            self.barrier()
        ab = getattr(self, "ln_arena_off", 0)
        self.ln_sq = self.view(ab, [128, 8, 512], F32)
        self.ln_mean = self.view(ab + 16384, [128, 512], F32)
        self.ln_rstd = self.view(ab + 18432, [128, 512], F32)
        self.ln_t = [self.view(ab + 20480 + i * 2048, [128, 512], F32) for i in range(2)]
        rs = src.res()
        rd = dst.res()
        ntg = (T + 511) // 512
        for tg in range(ntg):
            n = min(512, T - tg * 512)
            sl = slice(tg * 512, tg * 512 + n)
            sq = self.ln_sq
            self.op("act", (lambda e, sl=sl, n=n: e.activation(out=sq[:, :, 0:n], in_=src[:, :, sl], func=AF.Square)),
                    reads=[rs], writes=[sq.res()])
            pm = self.next_ps()
            pe2 = self.next_ps()
            for dc in range(8):
                self.op("pe", (lambda e, dc=dc, sl=sl, n=n, pm=pm:
                               e.matmul(pm[:, 0:n], self.ones_f[:, :], src[:, dc, sl], start=(dc == 0), stop=(dc == 7))),
                        reads=[rs, self.ones_f.res()], writes=[pm.res()])
            for dc in range(8):
                self.op("pe", (lambda e, dc=dc, n=n, pe2=pe2:
                               e.matmul(pe2[:, 0:n], self.ones_f[:, :], sq[:, dc, 0:n], start=(dc == 0), stop=(dc == 7))),
                        reads=[sq.res(), self.ones_f.res()], writes=[pe2.res()])
            mean = self.ln_mean
            rstd = self.ln_rstd
            self.rstd_from_stats(pm, pe2, mean, rstd, n, eps)
            for dc in range(8):
                t = self.ln_t[dc % 2]
                self.op("dve", (lambda e, dc=dc, sl=sl, n=n, t=t:
                                e.tensor_tensor(t[:, 0:n], src[:, dc, sl], mean[:, 0:n], ALU.subtract)),
                        reads=[rs, mean.res()], writes=[t.res()])
                self.op("dve", (lambda e, n=n, t=t: e.tensor_tensor(t[:, 0:n], t[:, 0:n], rstd[:, 0:n], ALU.mult)),
                        reads=[t.res(), rstd.res()], writes=[t.res()])
                sc_ap = scale_fn(dc)
                bi_ap = bias_fn(dc)
                if hf32 is not None:
                    self.op("act", (lambda e, dc=dc, n=n, t=t, sc_ap=sc_ap, bi_ap=bi_ap:
                                    e.activation(out=hf32[:, dc, 0:n], in_=t[:, 0:n], func=AF.Identity, scale=sc_ap, bias=bi_ap)),
                            reads=[t.res()] + list(extra_reads), writes=[hf32.res()])
                    self.op("pool", (lambda e, dc=dc, n=n, tg=tg: e.tensor_copy(dst[:, dc, dst_off + tg * 512:dst_off + tg * 512 + n], hf32[:, dc, 0:n])),
                            reads=[hf32.res()], writes=[rd])
                else:
                    self.op("act", (lambda e, dc=dc, n=n, t=t, tg=tg, sc_ap=sc_ap, bi_ap=bi_ap:
                                    e.activation(out=dst[:, dc, dst_off + tg * 512:dst_off + tg * 512 + n], in_=t[:, 0:n],
                                                 func=AF.Identity, scale=sc_ap, bias=bi_ap)),
                            reads=[t.res()] + list(extra_reads), writes=[rd])
            if post_group is not None:
                post_group(tg, n)


    def load_w_bf16(self, dst_ap, src_ap, res):
        self.dma("pool", dst_ap, src_ap, writes=[res])

    def load_vec_T(self, dst_ap, src_1d, res):
        self.dma("sp", dst_ap, src_1d.rearrange("(j p) -> p j", p=128), writes=[res])

    def conformer(self, l, hoff, T, RW, ab, out_view):
        self.mark("conformer")
        din = self.din
        CF_OFF = 1536
        nrows = T // RW
        PW = RW + 30
        G = 256
        rpg = G // RW
        ng = T // G
        o = ab
        Wcf = self.view(o, [128, 8, 1024], BF16); o += 16384
        DIAG = self.view(o, [128, 4, 31, 128], BF16); o += 4 * 31 * 128 * 2
        dwT = self.view(o, [128, 4, 31], F32); o += 4 * 31 * 4
        vecs = self.view(o, [128, 3, 4], F32); o += 48
        UPs = [self.view(o + i * 4 * rpg * PW * 2, [128, 4, rpg * PW], BF16) for i in range(2)]; o += 2 * 4 * rpg * PW * 2
        o = (o + 3) // 4 * 4
        Cb = self.view(o, [128, 4, G], F32); o += 4 * G * 4
        SQ = self.view(o, [128, 4, G], F32); o += 4 * G * 4
        mean = self.view(o, [128, G], F32); o += G * 4
        rstd = self.view(o, [128, G], F32); o += G * 4
        tmp = [self.view(o + i * G * 4, [128, G], F32) for i in range(2)]; o += 2 * G * 4
        sig = [self.view(o + i * G * 4, [128, G], F32) for i in range(2)]; o += 2 * G * 4
        for h in range(2):
            self.load_w_bf16(Wcf[:, :, h * 512:(h + 1) * 512],
                             din["w_in"][l][:, CF_OFF + h * 512:CF_OFF + (h + 1) * 512].rearrange("(k p) m -> p k m", p=128),
                             Wcf.res(h))
        for j in range(4):
            self.dma("sp", dwT[:, j, :], din["cf_dw_w"][l][:, j * 128:(j + 1) * 128].rearrange("k p -> p k"),
                     writes=[dwT.res()])
        self.load_vec_T(vecs[:, 0, :], din["cf_dw_b"][l], vecs.res())
        self.load_vec_T(vecs[:, 1, :], din["cf_ln_g"][l], vecs.res())
        self.load_vec_T(vecs[:, 2, :], din["cf_ln_b"][l], vecs.res())
        for UP in UPs:
            self.op("pool", (lambda e, UP=UP: e.memset(UP[:, :, :], 0.0)), writes=[UP.res()])
        for j in range(4):
            for k in range(31):
                eng_ = "act" if k % 4 != 3 else "dve"
                if eng_ == "act":
                    self.op("act", (lambda e, j=j, k=k: e.activation(out=DIAG[:, j, k, :], in_=self.ident_b[:, :], func=AF.Identity,
                                                                     scale=dwT[:, j, k:k + 1])),
                            reads=[dwT.res(), self.ident_b.res()], writes=[DIAG.res(j)])
                else:
                    self.op("dve", (lambda e, j=j, k=k: e.tensor_scalar(DIAG[:, j, k, :], self.ident_b[:, :], dwT[:, j, k:k + 1],
                                                                        None, ALU.mult)),
                            reads=[dwT.res(), self.ident_b.res()], writes=[DIAG.res(j)])
        rH = self.HT.res()
        for g in range(ng):
            UP = UPs[g % 2]
            tsl = slice(hoff + g * G, hoff + (g + 1) * G)
            for j in range(4):
                UPj = UP[:, j, :].rearrange("p (r w) -> p r w", w=PW)
                pa = self.next_ps()
                pg = self.next_ps()
                for kc in range(8):
                    self.op("pe", (lambda e, kc=kc, pa=pa, j=j, tsl=tsl:
                                   e.matmul(pa[:, 0:G], Wcf[:, kc, j * 128:(j + 1) * 128], self.HT[:, kc, tsl],
                                            start=(kc == 0), stop=(kc == 7))),
                            reads=[Wcf.res(0), rH], writes=[pa.res()])
                for kc in range(8):
                    self.op("pe", (lambda e, kc=kc, pg=pg, j=j, tsl=tsl:
                                   e.matmul(pg[:, 0:G], Wcf[:, kc, 512 + j * 128:512 + (j + 1) * 128], self.HT[:, kc, tsl],
                                            start=(kc == 0), stop=(kc == 7))),
                            reads=[Wcf.res(1), rH], writes=[pg.res()])
                sg = sig[j % 2]
                self.op("act", (lambda e, sg=sg, pg=pg: e.activation(out=sg[:, 0:G], in_=pg[:, 0:G], func=AF.Sigmoid)),
                        reads=[pg.res()], writes=[sg.res()])
                self.op("dve", (lambda e, UPj=UPj, pa=pa, sg=sg:
                                e.tensor_tensor(UPj[:, :, 15:15 + RW],
                                                pa[:, 0:G].rearrange("p (r w) -> p r w", w=RW),
                                                sg[:, 0:G].rearrange("p (r w) -> p r w", w=RW), ALU.mult)),
                        reads=[pa.res(), sg.res()], writes=[UP.res()])
            for j in range(4):
                UPj = UP[:, j, :].rearrange("p (r w) -> p r w", w=PW)
                pc = self.next_ps()
                for k in range(31):
                    self.op("pe", (lambda e, k=k, pc=pc, j=j, UPj=UPj:
                                   e.matmul(pc[:, 0:G], DIAG[:, j, k, :], UPj[:, :, k:k + RW],
                                            start=(k == 0), stop=(k == 30))),
                            reads=[DIAG.res(j), UP.res()], writes=[pc.res()])
                self.op("act", (lambda e, j=j, pc=pc: e.activation(out=Cb[:, j, :], in_=pc[:, 0:G], func=AF.Identity,
                                                                bias=vecs[:, 0, j:j + 1], scale=1.0)),
                        reads=[pc.res(), vecs.res()], writes=[Cb.res()])
                self.op("act", (lambda e, j=j, pc=pc: e.activation(out=SQ[:, j, :], in_=pc[:, 0:G], func=AF.Square,
                                                                bias=vecs[:, 0, j:j + 1], scale=1.0)),
                        reads=[pc.res(), vecs.res()], writes=[SQ.res()])
            pm = self.next_ps()
            pe2 = self.next_ps()
            for j in range(4):
                self.op("pe", (lambda e, j=j, pm=pm: e.matmul(pm[:, 0:G], self.ones_cf[:, :], Cb[:, j, :],
                                                             start=(j == 0), stop=(j == 3))),
                        reads=[Cb.res(), self.ones_cf.res()], writes=[pm.res()])
            for j in range(4):
                self.op("pe", (lambda e, j=j, pe2=pe2: e.matmul(pe2[:, 0:G], self.ones_cf[:, :], SQ[:, j, :],
                                                               start=(j == 0), stop=(j == 3))),
                        reads=[SQ.res(), self.ones_cf.res()], writes=[pe2.res()])
            self.rstd_from_stats(pm, pe2, mean, rstd, G, LN_EPS)
            for j in range(4):
                t = tmp[j % 2]
                self.op("dve", (lambda e, j=j, t=t: e.tensor_tensor(t[:, :], Cb[:, j, :], mean[:, :], ALU.subtract)),
                        reads=[Cb.res(), mean.res()], writes=[t.res()])
                self.op("pool", (lambda e, t=t: e.tensor_tensor(t[:, :], t[:, :], rstd[:, :], ALU.mult)),
                        reads=[t.res(), rstd.res()], writes=[t.res()])
                self.op("act", (lambda e, j=j, t=t, g=g:
                                e.activation(out=out_view[:, j, g * G:(g + 1) * G], in_=t[:, :], func=AF.Silu,
                                             scale=vecs[:, 1, j:j + 1], bias=vecs[:, 2, j:j + 1])),
                        reads=[t.res(), vecs.res()], writes=[out_view.res()])

    def rstd_from_stats(self, pm, pe2, mean, rstd, n, eps):
        self.op("act", (lambda e: e.copy(mean[:, 0:n], pm[:, 0:n])), reads=[pm.res()], writes=[mean.res()])
        self.op("dve", (lambda e: e.tensor_tensor(rstd[:, 0:n], mean[:, 0:n], mean[:, 0:n], ALU.mult)),
                reads=[mean.res()], writes=[rstd.res()])
        self.op("dve", (lambda e: e.tensor_tensor(rstd[:, 0:n], pe2[:, 0:n], rstd[:, 0:n], ALU.subtract)),
                reads=[pe2.res(), rstd.res()], writes=[rstd.res()])
        self.op("dve", (lambda e: e.tensor_scalar(rstd[:, 0:n], rstd[:, 0:n], float(eps), None, ALU.add)),
                reads=[rstd.res()], writes=[rstd.res()])
        self.op("act", (lambda e: e.activation(out=rstd[:, 0:n], in_=rstd[:, 0:n], func=AF.Sqrt)),
                reads=[rstd.res()], writes=[rstd.res()])
        self.op("dve", (lambda e: e.reciprocal(rstd[:, 0:n], rstd[:, 0:n])),
                reads=[rstd.res()], writes=[rstd.res()])

    def dbg_bf16(self, name, src_ap, res):
        if name in self.dbg_out:
            self.dma("pool", self.dbg_out[name], src_ap, reads=[res], writes=[Res()])


    def range_reduce(self, eng, x_ap, ti_ap, reads, res):
        PI_C = 3.14159
        self.op(eng, (lambda e: e.tensor_scalar(ti_ap, x_ap, 1.0 / TWO_PI, None, ALU.mult)), reads=reads, writes=[res])
        self.op("dve", (lambda e: e.scalar_tensor_tensor(x_ap, ti_ap, -TWO_PI, x_ap, ALU.mult, ALU.add)),
                reads=[res], writes=[res])
        self.op(eng, (lambda e: e.tensor_scalar(x_ap, x_ap, PI_C, -PI_C, ALU.min, ALU.max)), reads=[res], writes=[res])

    def s5_prologue(self, l):
        self.mark("s5_prologue")
        nc = self.nc
        din = self.din
        self.barrier()
        if not hasattr(self, "s5p"):
            self.s5p = [self.sb("s5p%d" % i, [128, 6, 2, 16], F32) for i in range(DEPTH)]
            self.halfpi = self.sb("halfpi", [128, 1], F32)
            self.op("pool", lambda e: e.memset(self.halfpi[:], math.pi / 2), writes=[self.halfpi.res()])
            self.s5_bmat = [nc.dram_tensor("s5_bmat%d" % i, [128, 16, 2, 2, 128], BF16, kind="Internal").ap() for i in range(DEPTH)]
            self.s5_cmat = [nc.dram_tensor("s5_cmat%d" % i, [128, 16, 2, 2, 128], BF16, kind="Internal").ap() for i in range(DEPTH)]
            self.s5_tab = [nc.dram_tensor("s5_tab%d" % i, [2, 16, 128, 2, L], F32, kind="Internal").ap() for i in range(DEPTH)]
            self.s5_res = [Res() for _ in range(DEPTH)]
        sp = self.s5p[l]
        rs = sp.res()
        o = 0
        raw = self.view(o, [128, 4, 2, 16], F32); o += 4 * 32 * 4
        tmp = self.view(o, [128, 6, 2, 16], F32); o += 6 * 32 * 4
        ti = self.view(o, [128, 2, 16], I32); o += 128
        iota_i = self.view(o, [128, L], I32); o += L * 4
        self.iota_t = self.view(o, [128, L], F32); o += L * 4
        rr = raw.res()
        for d in range(2):
            for gl in range(2):
                ps_ = slice(gl * 64, (gl + 1) * 64)
                self.dma("sp", raw[ps_, 0, d, :], din["s5_a_re"][l][d][gl::2, :].rearrange("k p -> p k"), writes=[rr])
                self.dma("sp", raw[ps_, 1, d, :], din["s5_a_im"][l][d][gl::2, :].rearrange("k p -> p k"), writes=[rr])
                self.dma("sp", raw[ps_, 2, d, :], din["s5_log_dt"][l][d][gl::2].partition_broadcast(64), writes=[rr])
        A = lambda i: raw[:, i, :, :]
        S = lambda i: sp[:, i, :, :]
        Tm = lambda i: tmp[:, i, :, :]
        rt = tmp.res()
        self.op("dve", lambda e: e.tensor_scalar(A(0), A(0), -1e-4, None, ALU.min), reads=[rr], writes=[rr])
        self.op("act", lambda e: e.activation(out=A(2), in_=A(2), func=AF.Exp), reads=[rr], writes=[rr])
        self.op("dve", lambda e: e.tensor_tensor(Tm(0), A(0), A(2), ALU.mult), reads=[rr], writes=[rt])
        self.op("act", lambda e: e.activation(out=S(0), in_=Tm(0), func=AF.Exp), reads=[rt], writes=[rs])
        self.op("dve", lambda e: e.tensor_tensor(S(1), A(1), A(2), ALU.mult), reads=[rr], writes=[rs])
        self.op("dve", lambda e: e.tensor_copy(Tm(1), S(1)), reads=[rs], writes=[rt])
        self.range_reduce("dve", Tm(1), ti[:, :, :], [rt], rt)
        self.op("act", lambda e: e.activation(out=S(3), in_=Tm(1), func=AF.Sin), reads=[rt], writes=[rs])
        self.op("act", lambda e: e.activation(out=Tm(1), in_=Tm(1), func=AF.Abs), reads=[rt], writes=[rt])
        self.op("act", lambda e: e.activation(out=S(2), in_=Tm(1), func=AF.Sin, scale=-1.0, bias=self.halfpi[:, 0:1]),
                reads=[rt, self.halfpi.res()], writes=[rs])
        self.op("dve", lambda e: e.tensor_tensor(Tm(2), S(0), S(2), ALU.mult), reads=[rs], writes=[rt])
        self.op("dve", lambda e: e.tensor_scalar(Tm(2), Tm(2), -1.0, None, ALU.add), reads=[rt], writes=[rt])
        self.op("dve", lambda e: e.tensor_tensor(Tm(3), S(0), S(3), ALU.mult), reads=[rs], writes=[rt])
        self.op("dve", lambda e: e.tensor_tensor(Tm(4), A(0), A(0), ALU.mult), reads=[rr], writes=[rt])
        self.op("dve", lambda e: e.tensor_tensor(Tm(5), A(1), A(1), ALU.mult), reads=[rr], writes=[rt])
        self.op("dve", lambda e: e.tensor_tensor(Tm(4), Tm(4), Tm(5), ALU.add), reads=[rt], writes=[rt])
        self.op("dve", lambda e: e.reciprocal(Tm(4), Tm(4)), reads=[rt], writes=[rt])
        self.op("dve", lambda e: e.tensor_tensor(S(4), Tm(2), A(0), ALU.mult), reads=[rt, rr], writes=[rs])
        self.op("dve", lambda e: e.tensor_tensor(Tm(5), Tm(3), A(1), ALU.mult), reads=[rt, rr], writes=[rt])
        self.op("dve", lambda e: e.tensor_tensor(S(4), S(4), Tm(5), ALU.add), reads=[rt, rs], writes=[rs])
        self.op("dve", lambda e: e.tensor_tensor(S(4), S(4), Tm(4), ALU.mult), reads=[rt, rs], writes=[rs])
        self.op("dve", lambda e: e.tensor_tensor(S(5), Tm(3), A(0), ALU.mult), reads=[rt, rr], writes=[rs])
        self.op("dve", lambda e: e.tensor_tensor(Tm(5), Tm(2), A(1), ALU.mult), reads=[rt, rr], writes=[rt])
        self.op("dve", lambda e: e.tensor_tensor(S(5), S(5), Tm(5), ALU.subtract), reads=[rt, rs], writes=[rs])
        self.op("dve", lambda e: e.tensor_tensor(S(5), S(5), Tm(4), ALU.mult), reads=[rt, rs], writes=[rs])
        if True:
            self.op("pool", lambda e: e.iota(iota_i[:, :], pattern=[[1, L]], base=0, channel_multiplier=0),
                    writes=[iota_i.res()])
            self.op("dve", lambda e: e.tensor_copy(self.iota_t[:, :], iota_i[:, :]), reads=[iota_i.res()],
                    writes=[self.iota_t.res()])
        o = (o + 3) // 4 * 4
        bpad = [self.view(o + i * 1024, [128, 2, 128], F32) for i in range(2)]; o += 2048
        cpad = [self.view(o + i * 1024, [128, 2, 128], F32) for i in range(2)]; o += 2048
        bbar = [self.view(o + i * 512, [128, 2, 128], BF16) for i in range(2)]; o += 1024
        cbf = [self.view(o + i * 512, [128, 2, 128], BF16) for i in range(2)]; o += 1024
        tq = [self.view(o + i * 1024, [128, 2, 128], F32) for i in range(2)]; o += 2048
        mT = [self.view(o + i * 1024, [128, 2, 2, 128], BF16) for i in range(2)]; o += 2048
        it = 0
        for d in range(2):
            for k in range(16):
                bp = bpad[it % 2]; cp = cpad[it % 2]; bb = bbar[it % 2]; cb = cbf[it % 2]; t2 = tq[it % 2]; mt = mT[it % 2]
                it += 1
                self.op("pool", (lambda e, bp=bp: e.memset(bp[:, :, :], 0.0)), writes=[bp.res()])
                self.op("pool", (lambda e, cp=cp: e.memset(cp[:, :, :], 0.0)), writes=[cp.res()])
                for gl in range(2):
                    g = 2 * k + gl
                    c0 = ((2 * k) % 8 + gl) * 16
                    self.dma("sp", bp[gl * 64:(gl + 1) * 64, 0, c0:c0 + 16], din["s5_b_re"][l][d][g], writes=[bp.res()])
                    self.dma("sp", bp[gl * 64:(gl + 1) * 64, 1, c0:c0 + 16], din["s5_b_im"][l][d][g], writes=[bp.res()])
                    self.dma("sp", cp[c0:c0 + 16, 0, gl * 64:(gl + 1) * 64], din["s5_c_re"][l][d][g], writes=[cp.res()])
                    self.dma("sp", cp[c0:c0 + 16, 1, gl * 64:(gl + 1) * 64], din["s5_c_im"][l][d][g], writes=[cp.res()])
                qre = sp[:, 4, d, k:k + 1]
                qim = sp[:, 5, d, k:k + 1]
                self.op("dve", (lambda e, t2=t2, bp=bp, qim=qim: e.tensor_scalar(t2[:, 0, :], bp[:, 1, :], qim, -1.0, ALU.mult, ALU.mult)),
                        reads=[bp.res(), rs], writes=[t2.res()])
                self.op("dve", (lambda e, t2=t2, bp=bp, qim=qim: e.tensor_scalar(t2[:, 1, :], bp[:, 0, :], qim, None, ALU.mult)),
                        reads=[bp.res(), rs], writes=[t2.res()])
                self.op("dve", (lambda e, t2=t2, bp=bp, bb=bb, qre=qre:
                                e.scalar_tensor_tensor(bb[:, 0, :], bp[:, 0, :], qre, t2[:, 0, :], ALU.mult, ALU.add)),
                        reads=[bp.res(), t2.res(), rs], writes=[bb.res()])
                self.op("dve", (lambda e, t2=t2, bp=bp, bb=bb, qre=qre:
                                e.scalar_tensor_tensor(bb[:, 1, :], bp[:, 1, :], qre, t2[:, 1, :], ALU.mult, ALU.add)),
                        reads=[bp.res(), t2.res(), rs], writes=[bb.res()])
                self.op("act", (lambda e, cb=cb, cp=cp: e.copy(cb[:, 0, :], cp[:, 0, :])), reads=[cp.res()], writes=[cb.res()])
                self.op("act", (lambda e, cb=cb, cp=cp: e.mul(cb[:, 1, :], cp[:, 1, :], -1.0)), reads=[cp.res()], writes=[cb.res()])
                pt = self.next_ps()
                ptb = pt[:, 0:256].bitcast(BF16)
                for ri in range(2):
                    self.op("pe", (lambda e, ptb=ptb, bb=bb, ri=ri: e.transpose(ptb[:, ri * 128:(ri + 1) * 128], bb[:, ri, :], self.ident_b[:, :])),
                            reads=[bb.res(), self.ident_b.res()], writes=[pt.res()])
                for ri in range(2):
                    self.op("pe", (lambda e, ptb=ptb, cb=cb, ri=ri: e.transpose(ptb[:, 256 + ri * 128:256 + (ri + 1) * 128], cb[:, ri, :], self.ident_b[:, :])),
                            reads=[cb.res(), self.ident_b.res()], writes=[pt.res()])
                self.op("dve", (lambda e, mt=mt, ptb=ptb: e.tensor_copy(mt[:, :, :, :].rearrange("p a b c -> p (a b c)"), ptb[:, 0:512])),
                        reads=[pt.res()], writes=[mt.res()])
                self.dma("sp", self.s5_bmat[l][:, k, d, :, :], mt[:, 0, :, :], reads=[mt.res()], writes=[mt.res("out")])
                self.dma("sp", self.s5_cmat[l][:, k, d, :, :], mt[:, 1, :, :], reads=[mt.res()], writes=[mt.res("out")])
        o = (o + 3) // 4 * 4
        ph = [self.view(o + i * (L * 4), [128, L], F32) for i in range(2)]; o += 2 * L * 4
        phi = [self.view(o + i * (L * 4), [128, L], I32) for i in range(2)]; o += 2 * L * 4
        cs = [self.view(o + i * (2 * L * 4), [128, 2, L], F32) for i in range(2)]; o += 4 * L * 4
        it = 0
        for d in range(2):
            for k in range(16):
                p_ = ph[it % 2]; pi_ = phi[it % 2]; c_ = cs[it % 2]
                it += 1
                th = sp[:, 1, d, k:k + 1]
                self.op("dve", (lambda e, p_=p_, th=th: e.tensor_scalar(p_[:, :], self.iota_t[:, :], th, None, ALU.mult)),
                        reads=[self.iota_t.res(), rs], writes=[p_.res()])
                self.range_reduce("dve", p_[:, :], pi_[:, :], [p_.res()], p_.res())
                self.op("act", (lambda e, p_=p_, c_=c_: e.activation(out=c_[:, 1, :], in_=p_[:, :], func=AF.Sin)),
                        reads=[p_.res()], writes=[c_.res()])
                self.op("act", (lambda e, p_=p_: e.activation(out=p_[:, :], in_=p_[:, :], func=AF.Abs)),
                        reads=[p_.res()], writes=[p_.res()])
                self.op("act", (lambda e, p_=p_, c_=c_: e.activation(out=c_[:, 0, :], in_=p_[:, :], func=AF.Sin, scale=-1.0,
                                                                     bias=self.halfpi[:, 0:1])),
                        reads=[p_.res(), self.halfpi.res()], writes=[c_.res()])
                self.dma("sp", self.s5_tab[l][d, k], c_[:, :, :], reads=[c_.res()], writes=[c_.res("out")])


    def s5_branch(self, l, hoff, T, ab, out_view, use_init, want_final, finals_only=False):
        self.mark("s5_branch")
        din = self.din
        S5_OFF = 2560
        N = min(512, T)
        nch = T // N
        sp = self.s5p[l]
        rs = sp.res()
        fin = self.s5fin
        o = ab
        uT = out_view
        mats = self.view(o, [128, 2, 4, 2, 2, 128], BF16); o += 8192
        wdiag = self.view(o, [128, 4, 128], BF16); o += 1024
        vecs = self.view(o, [128, 2, 4], F32); o += 32
        ini = self.view(o, [128, 2, 2], F32); o += 16
        cb0 = o
        Ws5 = self.view(o, [128, 8, 512], BF16)
        self.load_w_bf16(Ws5[:, :, :], din["w_in"][l][:, S5_OFF:S5_OFF + 512].rearrange("(k p) m -> p k m", p=128), Ws5.res())
        rH = self.HT.res()
        G = min(512, T)
        for j in range(4):
            for tg in range(T // G):
                pb = self.next_ps()
                for kc in range(8):
                    self.op("pe", (lambda e, kc=kc, pb=pb, j=j, tg=tg:
                                   e.matmul(pb[:, 0:G], Ws5[:, kc, j * 128:(j + 1) * 128],
                                            self.HT[:, kc, hoff + tg * G:hoff + (tg + 1) * G], start=(kc == 0), stop=(kc == 7))),
                            reads=[Ws5.res(), rH], writes=[pb.res()])
                self.op("act", (lambda e, pb=pb, j=j, tg=tg: e.copy(uT[:, j, tg * G:(tg + 1) * G], pb[:, 0:G])),
                        reads=[pb.res()], writes=[uT.res()])
        self.barrier()
        o = cb0
        NB4 = N * 4
        tb = [self.view(o + i * 2 * NB4, [128, 2, N], F32) for i in range(3)]; o += 6 * NB4
        pp = [self.view(o + i * 2 * NB4, [128, 2, N], F32) for i in range(2)]; o += 4 * NB4
        vv = [self.view(o + i * 2 * NB4, [128, 2, N], F32) for i in range(2)]; o += 4 * NB4
        tt = [self.view(o + i * 2 * NB4, [128, 2, N], F32) for i in range(2)]; o += 4 * NB4
        rr_ = self.view(o, [128, 2, N], F32); o += 2 * NB4
        ss = self.view(o, [128, 4, N], BF16); o += N * 8
        carry = self.view(o, [128, 2, 1], F32); o += 8
        self.load_vec_T(vecs[:, 0, :], din["s5_d"][l], vecs.res())
        self.load_vec_T(vecs[:, 1, :], din["s5_b_glu"][l], vecs.res())
        for j in range(4):
            self.op("act", (lambda e, j=j: e.activation(out=wdiag[:, j, :], in_=self.ident_b[:, :], func=AF.Identity, scale=vecs[:, 0, j:j + 1])),
                    reads=[vecs.res(), self.ident_b.res()], writes=[wdiag.res()])
        ci = 0
        nbank = (T + 511) // 512
        for j in range(4):
            self.dma("sp", mats[:, 0, :, :, :, :].rearrange("p a b c d -> p (a b c d)"),
                     self.s5_bmat[l][:, 4 * j:4 * j + 4, :, :, :].rearrange("p a b c d -> p (a b c d)"), writes=[mats.res()])
            self.dma("sp", mats[:, 1, :, :, :, :].rearrange("p a b c d -> p (a b c d)"),
                     self.s5_cmat[l][:, 4 * j:4 * j + 4, :, :, :].rearrange("p a b c d -> p (a b c d)"), writes=[mats.res()])
            acc = [self.next_ps(reserve=True) for _ in range(nbank)]
            started = set()
            units = [(k4, d, cc) for k4 in range(4) for d in range(2) for cc in range(nch)]
            ctxs = {}

            def tload(ui, j=j):
                k4, d, cc = units[ui]
                k = 4 * j + k4
                c = cc if d == 0 else nch - 1 - cc
                tbl = tb[ui % 3]
                tcol = slice(c * N, (c + 1) * N) if d == 0 else slice(T - (c + 1) * N, T - c * N)
                self.dma("sp", tbl[:, :, :], self.s5_tab[l][d, k][:, :, tcol], writes=[tbl.res()])

            def front(ui, j=j, acc=acc):
                k4, d, cc = units[ui]
                k = 4 * j + k4
                c = cc if d == 0 else nch - 1 - cc
                tsl = slice(c * N, (c + 1) * N)
                tbl = tb[ui % 3]; p_ = pp[ui % 2]; v_ = vv[ui % 2]; t_ = tt[ui % 2]
                rv = (lambda ap, d=d: ap if d == 0 else ap[:, ::-1])
                pre = [self.next_ps(), self.next_ps()]
                for ri in range(2):
                    self.op("pe", (lambda e, ri=ri, pre=pre, k4=k4, d=d, tsl=tsl:
                                   e.matmul(pre[ri][:, 0:N], mats[:, 0, k4, d, ri, :], uT[:, j, tsl], start=True, stop=True)),
                            reads=[mats.res(), uT.res()], writes=[pre[ri].res()])
                    self.op("act", (lambda e, ri=ri, pre=pre, p_=p_: e.copy(p_[:, ri, :], pre[ri][:, 0:N])),
                            reads=[pre[ri].res()], writes=[p_.res(ri)])
                cosT = rv(tbl[:, 0, :]); sinT = rv(tbl[:, 1, :])
                self.op("pool", (lambda e, v_=v_, p_=p_, cosT=cosT: e.tensor_tensor(v_[:, 0, :], p_[:, 0, :], cosT, ALU.mult)),
                        reads=[p_.res(0), tbl.res()], writes=[v_.res(0)])
                self.op("pool", (lambda e, t_=t_, p_=p_, sinT=sinT: e.tensor_tensor(t_[:, 0, :], p_[:, 1, :], sinT, ALU.mult)),
                        reads=[p_.res(1), tbl.res()], writes=[t_.res(0)])
                self.op("pool", (lambda e, v_=v_, t_=t_: e.tensor_tensor(v_[:, 0, :], v_[:, 0, :], t_[:, 0, :], ALU.add)),
                        reads=[t_.res(0), v_.res(0)], writes=[v_.res(0)])
                self.op("pool", (lambda e, v_=v_, p_=p_, cosT=cosT: e.tensor_tensor(v_[:, 1, :], p_[:, 1, :], cosT, ALU.mult)),
                        reads=[p_.res(1), tbl.res()], writes=[v_.res(1)])
                self.op("pool", (lambda e, t_=t_, p_=p_, sinT=sinT: e.tensor_tensor(t_[:, 1, :], p_[:, 0, :], sinT, ALU.mult)),
                        reads=[p_.res(0), tbl.res()], writes=[t_.res(1)])
                self.op("pool", (lambda e, v_=v_, t_=t_: e.tensor_tensor(v_[:, 1, :], v_[:, 1, :], t_[:, 1, :], ALU.subtract)),
                        reads=[t_.res(1), v_.res(1)], writes=[v_.res(1)])
                ctxs[ui] = (tbl, v_, t_, cosT, sinT, rv)

            def back(ui, j=j, acc=acc):
                k4, d, cc = units[ui]
                k = 4 * j + k4
                c = cc if d == 0 else nch - 1 - cc
                tbl, v_, t_, cosT, sinT, rv = ctxs.pop(ui)
                r_ = rr_; s_ = ss
                rho = sp[:, 0, d, k:k + 1]
                if cc == 0 and use_init:
                    c1 = sp[:, 2, d, k:k + 1]; s1 = sp[:, 3, d, k:k + 1]
                    self.op("dve", (lambda e, c1=c1: e.tensor_scalar(carry[:, 0, 0:1], fin[:, 0, d, k:k + 1], c1, None, ALU.mult)),
                            reads=[fin.res(), rs], writes=[carry.res()])
                    self.op("dve", (lambda e, s1=s1: e.scalar_tensor_tensor(carry[:, 0, 0:1], fin[:, 1, d, k:k + 1], s1, carry[:, 0, 0:1], ALU.mult, ALU.subtract)),
                            reads=[fin.res(), rs, carry.res()], writes=[carry.res()])
                    self.op("dve", (lambda e: e.tensor_scalar(carry[:, 0, 0:1], carry[:, 0, 0:1], -1.0, None, ALU.mult)),
                            reads=[carry.res()], writes=[carry.res()])
                    self.op("dve", (lambda e, s1=s1: e.tensor_scalar(carry[:, 1, 0:1], fin[:, 0, d, k:k + 1], s1, None, ALU.mult)),
                            reads=[fin.res(), rs], writes=[carry.res()])
                    self.op("dve", (lambda e, c1=c1: e.scalar_tensor_tensor(carry[:, 1, 0:1], fin[:, 1, d, k:k + 1], c1, carry[:, 1, 0:1], ALU.mult, ALU.add)),
                            reads=[fin.res(), rs, carry.res()], writes=[carry.res()])
                for ri in range(2):
                    init_ap = 0.0 if (cc == 0 and not use_init) else carry[:, ri, 0:1]
                    self.op("dve", (lambda e, ri=ri, init_ap=init_ap:
                                    e.tensor_tensor_scan(rv(r_[:, ri, :]), rho.to_broadcast([128, N]), rv(v_[:, ri, :]), init_ap, ALU.mult, ALU.add)),
                            reads=[v_.res(ri), rs, carry.res()], writes=[r_.res()])
                if cc < nch - 1:
                    lc = N - 1 if d == 0 else 0
                    self.op("act", (lambda e, lc=lc: e.copy(carry[:, :, 0:1], r_[:, :, lc:lc + 1])), reads=[r_.res()], writes=[carry.res()])
                if not finals_only:
                  self.op("dve", (lambda e: e.tensor_tensor(s_[:, 0, :], r_[:, 0, :], cosT, ALU.mult)), reads=[r_.res(), tbl.res()], writes=[s_.res()])
                  self.op("dve", (lambda e: e.scalar_tensor_tensor(s_[:, 1, :], r_[:, 1, :], -1.0, sinT, ALU.mult, ALU.mult)),
                          reads=[r_.res(), tbl.res()], writes=[s_.res()])
                  self.op("dve", (lambda e: e.tensor_tensor(s_[:, 2, :], r_[:, 0, :], sinT, ALU.mult)), reads=[r_.res(), tbl.res()], writes=[s_.res()])
                  self.op("dve", (lambda e: e.tensor_tensor(s_[:, 3, :], r_[:, 1, :], cosT, ALU.mult)), reads=[r_.res(), tbl.res()], writes=[s_.res()])
                if want_final and cc == nch - 1:
                    col = N - 1 if d == 0 else 0
                    csl = slice(col, col + 1)
                    tcs = slice(N - 1, N)
                    cF = tbl[:, 0, tcs]; sF = tbl[:, 1, tcs]
                    fr = fin[:, 0, d, k:k + 1]; fi = fin[:, 1, d, k:k + 1]
                    f2 = carry[:, 0, 0:1]
                    self.op("dve", (lambda e: e.tensor_tensor(fr, r_[:, 0, csl], cF, ALU.mult)), reads=[r_.res(), tbl.res()], writes=[fin.res()])
                    self.op("dve", (lambda e: e.tensor_tensor(fi, r_[:, 1, csl], sF, ALU.mult)), reads=[r_.res(), tbl.res()], writes=[fin.res()])
                    self.op("dve", (lambda e: e.tensor_tensor(fr, fr, fi, ALU.subtract)), reads=[fin.res()], writes=[fin.res()])
                    self.op("dve", (lambda e: e.tensor_tensor(fi, r_[:, 0, csl], sF, ALU.mult)), reads=[r_.res(), tbl.res(), fin.res()], writes=[fin.res()])
                    self.op("dve", (lambda e: e.tensor_tensor(f2, r_[:, 1, csl], cF, ALU.mult)), reads=[r_.res(), tbl.res()], writes=[carry.res()])
                    self.op("dve", (lambda e: e.tensor_tensor(fi, fi, f2, ALU.add)), reads=[carry.res(), fin.res()], writes=[fin.res()])
                bnk = (c * N) // 512
                off = (c * N) % 512
                for q in range(0 if finals_only else 4):
                    st_ = bnk not in started
                    started.add(bnk)
                    ri = q // 2
                    self.op("pe", (lambda e, ri=ri, q=q, st_=st_:
                                   e.matmul(acc[bnk][:, off:off + N], mats[:, 1, k4, d, ri, :], s_[:, q, :],
                                            start=st_, stop=False, skip_group_check=True)),
                            reads=[mats.res(), s_.res()], writes=[acc[bnk].res()])

            tload(0)
            if len(units) > 1:
                tload(1)
            front(0)
            for ui in range(len(units)):
                if ui + 2 < len(units):
                    tload(ui + 2)
                if ui + 1 < len(units):
                    front(ui + 1)
                back(ui)
            if finals_only:
                self.release_ps(acc)
                continue
            for c in range(nch):
                bnk = (c * N) // 512
                off = (c * N) % 512
                self.op("pe", (lambda e, j=j, c=c, bnk=bnk, off=off, acc=acc:
                               e.matmul(acc[bnk][:, off:off + N], wdiag[:, j, :], uT[:, j, c * N:(c + 1) * N], start=False, stop=True,
                                        skip_group_check=True)),
                        reads=[wdiag.res(), uT.res()], writes=[acc[bnk].res()])
            for bnk in range(nbank):
                n = min(512, T - bnk * 512)
                self.op("act", (lambda e, bnk=bnk, n=n, j=j, acc=acc: e.activation(out=out_view[:, j, bnk * 512:bnk * 512 + n],
                                                                          in_=acc[bnk][:, 0:n], func=(AF.Copy if "_s5raw" in self.debug else AF.Gelu))),
                        reads=[acc[bnk].res()], writes=[out_view.res()])
            self.release_ps(acc)
        if finals_only:
            return
        self.dbg_bf16("ys5g", out_view[:, :, :], out_view.res())
        sg = [self.view(cb0 + i * 2048, [128, 512], F32) for i in range(4)]
        self.barrier()
        wglu = self.view(cb0 + 8192, [128, 4, 512], BF16)
        self.load_w_bf16(wglu[:, :, :], din["s5_w_glu"][l].rearrange("(k p) m -> p k m", p=128), wglu.res())
        for tg in range((T + 511) // 512):
            n = min(512, T - tg * 512)
            tsl = slice(tg * 512, tg * 512 + n)
            pz = [self.next_ps(reserve=True) for _ in range(4)]
            self.release_ps(pz)
            for jo in range(4):
                for ji in range(4):
                    self.op("pe", (lambda e, jo=jo, ji=ji, tsl=tsl, n=n, pz=pz: e.matmul(pz[jo][:, 0:n], wglu[:, ji, jo * 128:(jo + 1) * 128],
                                                                                  out_view[:, ji, tsl], start=(ji == 0), stop=(ji == 3))),
                            reads=[wglu.res(), out_view.res()], writes=[pz[jo].res()])
            for jo in range(4):
                self.op("act", (lambda e, jo=jo, n=n, pz=pz: e.activation(out=sg[jo][:, 0:n], in_=pz[jo][:, 0:n], func=AF.Sigmoid,
                                                                   bias=vecs[:, 1, jo:jo + 1], scale=1.0)),
                        reads=[pz[jo].res(), vecs.res()], writes=[sg[jo].res()])
            for jo in range(4):
                self.op("dve", (lambda e, jo=jo, n=n, tsl=tsl: e.tensor_tensor(out_view[:, jo, tsl], out_view[:, jo, tsl], sg[jo][:, 0:n], ALU.mult)),
                        reads=[sg[jo].res(), out_view.res()], writes=[out_view.res()])


    def mixer_out(self, l, hoff, T, X, col, ZH, UCF, YS, ab):
        self.mark("mixer_out")
        din = self.din
        GATE_OFF = 3072
        G = min(512, T)
        ntg = T // G
        o = ab
        MIX = self.view(o, [128, 8, T], BF16); o += 8 * T * 2
        Wg = [self.view(o, [128, 8, 3, 128], BF16)] * 2; o += 6144
        Wb = [self.view(o, [128, 4, 3, 128], BF16)] * 2; o += 3072
        sig = [self.view(o + i * 2048, [128, 512], F32) for i in range(3)]; o += 3 * 2048
        mm = sig
        lnv = self.view(o, [128, 2, 8], F32); o += 64
        rH = self.HT.res()
        branches = (ZH, UCF, YS)
        wnames = ("w_hy_out", "w_cf_out", "w_s5_out")
        self.load_vec_T(lnv[:, 0, :], din["ln1_g"][l], lnv.res())
        self.load_vec_T(lnv[:, 1, :], din["ln1_b"][l], lnv.res())
        for dt in range(8):
            wg = Wg[dt % 2]
            wb = Wb[dt % 2]
            for br in range(3):
                c0 = GATE_OFF + br * 1024 + dt * 128
                self.load_w_bf16(wg[:, :, br, :], din["w_in"][l][:, c0:c0 + 128].rearrange("(k p) m -> p k m", p=128), wg.res())
                self.load_w_bf16(wb[:, :, br, :], din[wnames[br]][l][:, dt * 128:(dt + 1) * 128].rearrange("(k p) m -> p k m", p=128), wb.res())
            for tg in range(ntg):
                tsl = slice(tg * G, (tg + 1) * G)
                hsl = slice(hoff + tg * G, hoff + (tg + 1) * G)
                pg = [self.next_ps(reserve=True) for _ in range(3)]
                po = [self.next_ps(reserve=True) for _ in range(3)]
                self.release_ps(pg + po)
                for br in range(3):
                    for kc in range(8):
                        self.op("pe", (lambda e, pg=pg, br=br, kc=kc, wg=wg, hsl=hsl:
                                       e.matmul(pg[br][:, 0:G], wg[:, kc, br, :], self.HT[:, kc, hsl], start=(kc == 0), stop=(kc == 7))),
                                reads=[wg.res(), rH], writes=[pg[br].res()])
                    for kc in range(4):
                        self.op("pe", (lambda e, po=po, br=br, kc=kc, wb=wb, tsl=tsl:
                                       e.matmul(po[br][:, 0:G], wb[:, kc, br, :], branches[br][:, kc, tsl], start=(kc == 0), stop=(kc == 3))),
                                reads=[wb.res(), branches[br].res()], writes=[po[br].res()])
                for br in range(3):
                    self.op("act", (lambda e, br=br, pg=pg: e.activation(out=sig[br][:, 0:G], in_=pg[br][:, 0:G], func=AF.Sigmoid)),
                            reads=[pg[br].res()], writes=[sig[br].res()])
                    self.op("dve", (lambda e, br=br, po=po: e.tensor_tensor(mm[br][:, 0:G], po[br][:, 0:G], sig[br][:, 0:G], ALU.mult)),
                            reads=[po[br].res(), sig[br].res()], writes=[mm[br].res()])
                self.op("pool", (lambda e: e.tensor_tensor(mm[0][:, 0:G], mm[0][:, 0:G], mm[1][:, 0:G], ALU.add)),
                        reads=[mm[0].res(), mm[1].res()], writes=[mm[0].res()])
                self.op("pool", (lambda e, dt=dt, tsl=tsl: e.tensor_tensor(MIX[:, dt, tsl], mm[0][:, 0:G], mm[2][:, 0:G], ALU.add)),
                        reads=[mm[0].res(), mm[2].res()], writes=[MIX.res()])
        self.barrier()
        WO = self.view(0, [128, 8, 1024], BF16)
        self.ln_arena_off = 16384
        self.load_w_bf16(WO[:, :, :], din["w_o"][l].rearrange("(k p) m -> p k m", p=128), WO.res())
        for dt in range(8):
            g1 = self.mod_ap(l, 2, dt, col)
            for tg in range(ntg):
                tsl = slice(tg * G, (tg + 1) * G)
                pb = self.next_ps()
                for kc in range(8):
                    self.op("pe", (lambda e, pb=pb, kc=kc, dt=dt, tsl=tsl:
                                   e.matmul(pb[:, 0:G], WO[:, kc, dt * 128:(dt + 1) * 128], MIX[:, kc, tsl], start=(kc == 0), stop=(kc == 7))),
                            reads=[WO.res(), MIX.res()], writes=[pb.res()])
                self.op("dve", (lambda e, pb=pb, dt=dt, tsl=tsl, g1=g1:
                                e.scalar_tensor_tensor(X[:, dt, tsl], pb[:, 0:G], g1, X[:, dt, tsl], ALU.mult, ALU.add)),
                        reads=[pb.res(), X.res(), self.modT.res()], writes=[X.res()])
        self.layer_norm(X, T, X, 0, lambda dc: lnv[:, 0, dc:dc + 1], lambda dc: lnv[:, 1, dc:dc + 1], LN_EPS / (ALPHA * ALPHA),
                        extra_reads=[lnv.res()], no_barrier=True)

    def ffn(self, l, T, X, col, moe, hoff=0):
        self.mark("ffn")
        din = self.din
        G = min(512, T)
        ntg = T // G
        NTT = T // 128
        self.barrier()
        self.ln_arena_off = 0
        hook = None
        o = 24576 + 16384
        comb = combT = sel = None
        if moe:
            hf32 = self.view(24576, [128, 8, 512], F32)
            rtr = self.view(o, [128, 8, 8], F32); o += 256
            comb = self.view(o, [128, NTT, 8], F32); o += NTT * 32
            rsc = self.view(o, [128, 8, 8], F32); o += 256
            combT = self.view(o, [8, T], F32); o += T * 4
            sel = self.view(o, [8, 8, 128], F32); o += 4096
            self.dma("sp", rtr[:, :, :], din["moe_router"][0].rearrange("(k p) e -> p k e", p=128), writes=[rtr.res()])
            self.op("dve", (lambda e: e.tensor_copy(sel[:, :, :], self.ident_f[0:8, 0:8].unsqueeze(2).to_broadcast([8, 8, 128]))),
                    reads=[self.ident_f.res()], writes=[sel.res()])

            def hook(tg, n):
                for q in range(n // 128):
                    tt = tg * (512 // 128) + q
                    pl = self.next_ps()
                    for kc in range(8):
                        self.op("pe", (lambda e, pl=pl, kc=kc, q=q: e.matmul(pl[:, 0:8], hf32[:, kc, q * 128:(q + 1) * 128], rtr[:, kc, :],
                                                                          start=(kc == 0), stop=(kc == 7))),
                                reads=[hf32.res(), rtr.res()], writes=[pl.res()])
                    lg = rsc[:, 0, :]; m1 = rsc[:, 1, 0:1]; mk1 = rsc[:, 2, :]; msk = rsc[:, 3, :]; m2 = rsc[:, 1, 1:2]; mk2 = rsc[:, 4, :]
                    dd = rsc[:, 1, 2:3]; w1 = rsc[:, 1, 3:4]; w2 = rsc[:, 1, 4:5]
                    rr = rsc.res()
                    self.op("act", (lambda e, pl=pl: e.copy(lg, pl[:, 0:8])), reads=[pl.res()], writes=[rr])
                    self.op("dve", (lambda e: e.tensor_reduce(m1, lg, AX.X, ALU.max)), reads=[rr], writes=[rr])
                    self.op("dve", (lambda e: e.tensor_scalar(mk1, lg, m1, None, ALU.is_equal)), reads=[rr], writes=[rr])
                    self.op("dve", (lambda e: e.scalar_tensor_tensor(msk, mk1, -1e30, lg, ALU.mult, ALU.add)), reads=[rr], writes=[rr])
                    self.op("dve", (lambda e: e.tensor_reduce(m2, msk, AX.X, ALU.max)), reads=[rr], writes=[rr])
                    self.op("dve", (lambda e: e.tensor_scalar(mk2, msk, m2, None, ALU.is_equal)), reads=[rr], writes=[rr])
                    self.op("dve", (lambda e: e.tensor_tensor(dd, m2, m1, ALU.subtract)), reads=[rr], writes=[rr])
                    self.op("act", (lambda e: e.activation(out=dd, in_=dd, func=AF.Exp)), reads=[rr], writes=[rr])
                    self.op("dve", (lambda e: e.tensor_scalar(w1, dd, 1.0, None, ALU.add)), reads=[rr], writes=[rr])
                    self.op("dve", (lambda e: e.reciprocal(w1, w1)), reads=[rr], writes=[rr])
                    self.op("dve", (lambda e: e.tensor_tensor(w2, dd, w1, ALU.mult)), reads=[rr], writes=[rr])
                    self.op("dve", (lambda e: e.tensor_scalar(mk1, mk1, w1, None, ALU.mult)), reads=[rr], writes=[rr])
                    self.op("dve", (lambda e, tt=tt: e.scalar_tensor_tensor(comb[:, tt, :], mk2, w2, mk1, ALU.mult, ALU.add)),
                            reads=[rr], writes=[comb.res()])
                    pt = self.next_ps()
                    self.op("pe", (lambda e, pt=pt, tt=tt: e.transpose(pt[0:8, 0:128], comb[:, tt, :], self.ident_f[:, :])),
                            reads=[comb.res(), self.ident_f.res()], writes=[pt.res()])
                    self.op("act", (lambda e, pt=pt, tt=tt: e.copy(combT[:, tt * 128:(tt + 1) * 128], pt[0:8, 0:128])),
                            reads=[pt.res()], writes=[combT.res()])
        self.layer_norm(X, T, self.HT, hoff, lambda dc: self.mod_ap(l, 4, dc, col), lambda dc: self.mod_ap(l, 3, dc, col), LN_EPS,
                        extra_reads=[self.modT.res()], hf32=(hf32 if moe else None), post_group=hook)
        self.barrier()
        FF = D_EXP if moe else D_FF
        nchunk = FF // 128
        F = 4
        CB = None
        if moe:
            CB = self.view(o, [128, T], F32); o += T * 4
        lnv = self.view(o, [128, 2, 8], F32); o += 64
        W1 = [self.view(o + i * 8192, [128, 8, F * 128], BF16) for i in range(2)]; o += 16384
        W3 = [self.view(o + i * 8192, [128, 8, F * 128], BF16) for i in range(2)]; o += 16384
        W2 = [self.view(i * 8192, [128, F, 1024], BF16) for i in range(2)]
        st = [self.view(16384 + i * 2048, [128, 512], F32) for i in range(2)]
        tt_ = [self.view(20480 + i * 2048, [128, 512], F32) for i in range(2)]
        GT = self.view(24576, [128, F, T], BF16)
        self.load_vec_T(lnv[:, 0, :], din["ln2_g"][l], lnv.res())
        self.load_vec_T(lnv[:, 1, :], din["ln2_b"][l], lnv.res())
        rH = self.HT.res()
        gi = 0
        ii = 0
        for ex in range(NEXP if moe else 1):
            if moe:
                w1d, w3d, w2d = din["moe_w1"][0][ex], din["moe_w3"][0][ex], din["moe_w2"][0][ex]
                for tg in range(ntg):
                    pc = self.next_ps()
                    self.op("pe", (lambda e, pc=pc, ex=ex, tg=tg: e.matmul(pc[:, 0:G], sel[:, ex, :], combT[:, tg * G:(tg + 1) * G], start=True, stop=True)),
                            reads=[sel.res(), combT.res()], writes=[pc.res()])
                    self.op("act", (lambda e, pc=pc, tg=tg: e.copy(CB[:, tg * G:(tg + 1) * G], pc[:, 0:G])), reads=[pc.res()], writes=[CB.res()])
            else:
                w1d, w3d, w2d = din["ffn_w1"][0], din["ffn_w3"][0], din["ffn_w2"][0]
            for g0 in range(0, nchunk, F):
                nf = min(F, nchunk - g0)
                w1 = W1[gi % 2]; w3 = W3[gi % 2]; w2 = W2[gi % 2]
                gi += 1
                c0 = g0 * 128
                self.load_w_bf16(w1[:, :, 0:nf * 128], w1d[:, c0:c0 + nf * 128].rearrange("(k p) m -> p k m", p=128), w1.res())
                self.load_w_bf16(w3[:, :, 0:nf * 128], w3d[:, c0:c0 + nf * 128].rearrange("(k p) m -> p k m", p=128), w3.res())
                self.load_w_bf16(w2[:, 0:nf, :], w2d[c0:c0 + nf * 128, :].rearrange("(f p) m -> p f m", p=128), w2.res())
                for f in range(nf):
                    for tg in range(ntg):
                        tsl = slice(tg * G, (tg + 1) * G)
                        p1 = self.next_ps()
                        p3 = self.next_ps()
                        for kc in range(8):
                            self.op("pe", (lambda e, p1=p1, kc=kc, f=f, w1=w1, tg=tg:
                                           e.matmul(p1[:, 0:G], w1[:, kc, f * 128:(f + 1) * 128], self.HT[:, kc, hoff + tg * G:hoff + (tg + 1) * G], start=(kc == 0), stop=(kc == 7))),
                                    reads=[w1.res(), rH], writes=[p1.res()])
                        for kc in range(8):
                            self.op("pe", (lambda e, p3=p3, kc=kc, f=f, w3=w3, tg=tg:
                                           e.matmul(p3[:, 0:G], w3[:, kc, f * 128:(f + 1) * 128], self.HT[:, kc, hoff + tg * G:hoff + (tg + 1) * G], start=(kc == 0), stop=(kc == 7))),
                                    reads=[w3.res(), rH], writes=[p3.res()])
                        s_ = st[ii % 2]; t_ = tt_[ii % 2]
                        ii += 1
                        self.op("act", (lambda e, s_=s_, p1=p1: e.activation(out=s_[:, 0:G], in_=p1[:, 0:G], func=AF.Silu)),
                                reads=[p1.res()], writes=[s_.res()])
                        if moe:
                            self.op("dve", (lambda e, t_=t_, p3=p3, tsl=tsl: e.tensor_tensor(t_[:, 0:G], p3[:, 0:G], CB[:, tsl], ALU.mult)),
                                    reads=[p3.res(), CB.res()], writes=[t_.res()])
                            self.op("pool", (lambda e, t_=t_, s_=s_, f=f, tsl=tsl: e.tensor_tensor(GT[:, f, tsl], t_[:, 0:G], s_[:, 0:G], ALU.mult)),
                                    reads=[t_.res(), s_.res()], writes=[GT.res()])
                        else:
                            self.op("dve", (lambda e, s_=s_, p3=p3, f=f, tsl=tsl: e.tensor_tensor(GT[:, f, tsl], p3[:, 0:G], s_[:, 0:G], ALU.mult)),
                                    reads=[p3.res(), s_.res()], writes=[GT.res()])
                for dt in range(8):
                    g2 = self.mod_ap(l, 5, dt, col)
                    for tg in range(ntg):
                        tsl = slice(tg * G, (tg + 1) * G)
                        pb = self.next_ps()
                        for f in range(nf):
                            self.op("pe", (lambda e, pb=pb, f=f, dt=dt, w2=w2, tsl=tsl, nf=nf:
                                           e.matmul(pb[:, 0:G], w2[:, f, dt * 128:(dt + 1) * 128], GT[:, f, tsl], start=(f == 0), stop=(f == nf - 1))),
                                    reads=[w2.res(), GT.res()], writes=[pb.res()])
                        self.op("dve", (lambda e, pb=pb, dt=dt, tsl=tsl, g2=g2:
                                        e.scalar_tensor_tensor(X[:, dt, tsl], pb[:, 0:G], g2, X[:, dt, tsl], ALU.mult, ALU.add)),
                                reads=[pb.res(), X.res(), self.modT.res()], writes=[X.res()])
        self.ln_arena_off = 0
        self.layer_norm(X, T, X, 0, lambda dc: lnv[:, 0, dc:dc + 1], lambda dc: lnv[:, 1, dc:dc + 1], LN_EPS / (ALPHA * ALPHA),
                        extra_reads=[lnv.res()])

    def store_out(self, bi):
        self.mark("store_out")
        self.barrier()
        stg = [self.view(i * 16384, [128, 4, D], F32) for i in range(2)]
        for tg in range(L // 512):
            sg_ = stg[tg % 2]
            for j in range(4):
                for half in range(2):
                    pb = self.next_ps()
                    for q in range(4):
                        dc = half * 4 + q
                        self.op("pe", (lambda e, pb=pb, q=q, dc=dc, tg=tg, j=j:
                                       e.transpose(pb[:, q * 128:(q + 1) * 128], self.XT[:, dc, tg * 512 + j * 128:tg * 512 + (j + 1) * 128],
                                                   self.ident_f[:, :])),
                                reads=[self.XT.res(), self.ident_f.res()], writes=[pb.res()])
                    if half == 0:
                        self.op("dve", (lambda e, pb=pb, sg_=sg_, j=j: e.tensor_copy(sg_[:, j, 0:512], pb[:, :])), reads=[pb.res()], writes=[sg_.res()])
                    else:
                        self.op("act", (lambda e, pb=pb, sg_=sg_, j=j: e.copy(sg_[:, j, 512:1024], pb[:, :])), reads=[pb.res()], writes=[sg_.res()])
            dst = self.out[bi, tg * 512:(tg + 1) * 512, :].rearrange("(j p) d -> p j d", p=128)
            self.dma("sp", dst, sg_[:, :, :], reads=[sg_.res()], writes=[sg_.res("out")])


    def hyena_prologue(self, l, T, tag):
        self.mark("hyena_prologue")
        nc = self.nc
        din = self.din
        self.barrier()
        NT = T // 128
        n = 2 * T
        key = (l, tag)
        if not hasattr(self, "khat"):
            self.khat = {}
        kh = nc.dram_tensor("khat_%d_%s" % (l, tag), [2, NT, 128, 2, 512], F32, kind="Internal").ap()
        self.khat[key] = kh
        cst = self.hc[tag]
        o = 0
        z0 = o
        zT = self.view(o, [33, T], F32); o += max(T * 4, 2 * NT * 256 + 8192)
        w1 = self.view(o, [33, 64], F32); o += 256
        w2 = self.view(o, [64, 64], F32); o += 256
        w3 = self.view(o, [64, 2048], F32); o += 8192
        vec = self.view(o, [64, 6], F32); o += 24
        tn = self.view(o, [128, NT], F32); o += NT * 4
        h1 = self.view(o, [64, T], F32); o += T * 4
        h2 = self.view(o, [64, T], F32); o += T * 4
        ti = self.view(o, [64, 512], I32); o += 2048
        b3b = self.view(o, [128, 1024], F32); o += 4096
        dcb = self.view(o, [128, 1024], F32); o += 4096
        biasb = self.view(o, [128, 512], F32); o += 2048
        hrow = [self.view(o + i * 4096, [128, 1024], F32) for i in range(2)]; o += 8192
        win = [self.view(o + i * 2048, [128, 512], F32) for i in range(2)]; o += 4096
        HF = self.view(o, [128, NT, 1024], BF16); o += NT * 2048
        self.dma("sp", zT[:, :], cst["z"], writes=[zT.res()])
        self.dma("sp", w1[:, :], din["hy_f_w1"][l], writes=[w1.res()])
        self.dma("sp", w2[:, :], din["hy_f_w2"][l], writes=[w2.res()])
        self.dma("sp", w3[:, :], din["hy_f_w3"][l], writes=[w3.res()])
        rv_ = vec.res()
        self.dma("sp", vec[:, 0:1], din["hy_f_b1"][l].rearrange("(p o) -> p o", o=1), writes=[rv_])
        self.dma("sp", vec[:, 1:2], din["hy_f_b2"][l].rearrange("(p o) -> p o", o=1), writes=[rv_])
        self.dma("sp", vec[:, 2:3], din["hy_freq"][l][0].rearrange("(p o) -> p o", o=1), writes=[rv_])
        self.dma("sp", vec[:, 3:4], din["hy_freq"][l][1].rearrange("(p o) -> p o", o=1), writes=[rv_])
        self.dma("sp", tn[:, :], cst["tn"], writes=[tn.res()])
        self.op("dve", lambda e: e.tensor_tensor(vec[:, 4:6], vec[:, 0:2], vec[:, 2:4], ALU.mult), reads=[rv_], writes=[rv_])
        self.op("dve", lambda e: e.tensor_scalar(tn[:, :], tn[:, :], -1.0, None, ALU.mult), reads=[tn.res()], writes=[tn.res()])
        G = min(512, T)
        for (src, wt, kk, dst, fcol, bcol) in ((zT, w1, 33, h1, 2, 4), (h1, w2, 64, h2, 3, 5)):
            for tg in range(T // G):
                pb = self.next_ps()
                self.op("pe", (lambda e, pb=pb, wt=wt, src=src, kk=kk, tg=tg:
                               e.matmul(pb[0:64, 0:G], wt[0:kk, :], src[0:kk, tg * G:(tg + 1) * G], start=True, stop=True)),
                        reads=[wt.res(), src.res()], writes=[pb.res()])
                d_ap = dst[:, tg * G:(tg + 1) * G]
                self.op("dve", (lambda e, pb=pb, d_ap=d_ap, fcol=fcol, bcol=bcol:
                                e.tensor_scalar(d_ap, pb[0:64, 0:G], vec[:, fcol:fcol + 1], vec[:, bcol:bcol + 1], ALU.mult, ALU.add)),
                        reads=[pb.res(), rv_], writes=[dst.res()])
                self.range_reduce("dve", d_ap, ti[:, 0:G], [dst.res()], dst.res())
                self.op("act", (lambda e, d_ap=d_ap: e.activation(out=d_ap, in_=d_ap, func=AF.Sin)),
                        reads=[dst.res()], writes=[dst.res()])
        self.barrier()
        fw = [self.view(z0 + i * NT * 256, [128, NT, 128], BF16) for i in range(2)]
        kst = [self.view(z0 + 2 * NT * 256 + i * 4096, [128, 2, 512], F32) for i in range(2)]
        it = 0
        for oo in range(2):
            self.dma("sp", b3b[:, :], din["hy_f_b3"][l][oo * 1024:(oo + 1) * 1024].partition_broadcast(128), writes=[b3b.res()])
            self.dma("sp", dcb[:, :], din["hy_decay"][l][oo * 1024:(oo + 1) * 1024].partition_broadcast(128), writes=[dcb.res()])
            self.dma("sp", biasb[:, :], din["hy_bias"][l][oo].partition_broadcast(128), writes=[biasb.res()])
            self.op("act", lambda e: e.activation(out=dcb[:, :], in_=dcb[:, :], func=AF.Abs), reads=[dcb.res()], writes=[dcb.res()])
            for tt in range(NT):
                hr = hrow[tt % 2]
                for cg in range(2):
                    pb = self.next_ps()
                    self.op("pe", (lambda e, pb=pb, tt=tt, cg=cg, oo=oo:
                                   e.matmul(pb[:, :], h2[:, tt * 128:(tt + 1) * 128], w3[:, oo * 1024 + cg * 512:oo * 1024 + (cg + 1) * 512],
                                            start=True, stop=True)),
                            reads=[h2.res(), w3.res()], writes=[pb.res()])
                    wn = win[cg % 2]
                    self.op("act", (lambda e, wn=wn, cg=cg, tt=tt: e.activation(out=wn[:, :], in_=dcb[:, cg * 512:(cg + 1) * 512], func=AF.Exp,
                                                                            scale=tn[:, tt:tt + 1])),
                            reads=[dcb.res(), tn.res()], writes=[wn.res()])
                    self.op("dve", (lambda e, pb=pb, hr=hr, cg=cg: e.tensor_tensor(hr[:, cg * 512:(cg + 1) * 512], pb[:, :], b3b[:, cg * 512:(cg + 1) * 512], ALU.add)),
                            reads=[pb.res(), b3b.res()], writes=[hr.res()])
                    self.op("pool", (lambda e, hr=hr, wn=wn, cg=cg: e.tensor_tensor(hr[:, cg * 512:(cg + 1) * 512], hr[:, cg * 512:(cg + 1) * 512], wn[:, :], ALU.mult)),
                            reads=[hr.res(), wn.res()], writes=[hr.res()])
                if tt == 0:
                    self.op("pool", (lambda e, hr=hr: e.memset(hr[0:1, 512:1024], 0.0)), reads=[hr.res()], writes=[hr.res()])
                self.op("dve", (lambda e, tt=tt, hr=hr: e.tensor_tensor(HF[:, tt, 0:512], hr[:, 0:512], hr[:, 512:1024], ALU.add)),
                        reads=[hr.res()], writes=[HF.res()])
                self.op("pool", (lambda e, tt=tt, hr=hr: e.tensor_tensor(HF[:, tt, 512:1024], hr[:, 0:512], hr[:, 512:1024], ALU.subtract)),
                        reads=[hr.res()], writes=[HF.res()])
            for j in range(NT):
                ks = kst[j % 2]
                for cs in range(2):
                    f_ = fw[it % 2]
                    it += 1
                    ft = cs * NT + j
                    self.dma("sp", f_[:, :, :], cst["fwd"][ft], writes=[f_.res()])
                    pb = self.next_ps()
                    for c in range(NT):
                        self.op("pe", (lambda e, pb=pb, f_=f_, c=c, cs=cs:
                                       e.matmul(pb[:, :], f_[:, c, :], HF[:, c, cs * 512:cs * 512 + 512],
                                                start=(c == 0), stop=(c == NT - 1))),
                                reads=[f_.res(), HF.res()], writes=[pb.res()])
                    if cs == 0:
                        self.op("dve", (lambda e, ks=ks, pb=pb: e.tensor_tensor(ks[:, 0, :], pb[:, :], biasb[:, :], ALU.add)),
                                reads=[pb.res(), biasb.res()], writes=[ks.res()])
                        self.op("act", (lambda e, ks=ks: e.mul(ks[:, 0, :], ks[:, 0, :], 2.0 / n)),
                                reads=[ks.res()], writes=[ks.res()])
                    else:
                        self.op("act", (lambda e, ks=ks, pb=pb: e.mul(ks[:, 1, :], pb[:, :], 2.0 / n)),
                                reads=[pb.res()], writes=[ks.res()])
                self.dma("sp", kh[oo, j], ks[:, :, :], reads=[ks.res()], writes=[ks.res("out")])

    def hyena_inproj_conv(self, l, col0, hoff, T, Wh, shw, stage, ftmp, dst, dst_is_tok):
        din = self.din
        self.load_w_bf16(Wh[:, :, :], din["w_in"][l][:, col0:col0 + 512].rearrange("(k p) m -> p k m", p=128), Wh.res())
        rH = self.HT.res()
        G = min(512, T)
        cbuf = ftmp["cbuf"]
        for j in range(4):
            jt = col0 // 128 + j
            self.op("pool", (lambda e: e.memset(stage[:, 0:1], 0.0)), writes=[stage.res()])
            self.op("pool", (lambda e: e.memset(stage[:, T + 1:T + 2], 0.0)), writes=[stage.res()])
            for tg in range(T // G):
                pb = self.next_ps()
                for kc in range(8):
                    self.op("pe", (lambda e, kc=kc, pb=pb, j=j, tg=tg:
                                   e.matmul(pb[:, 0:G], Wh[:, kc, j * 128:(j + 1) * 128],
                                            self.HT[:, kc, hoff + tg * G:hoff + (tg + 1) * G], start=(kc == 0), stop=(kc == 7))),
                            reads=[Wh.res(), rH], writes=[pb.res()])
                self.op("act", (lambda e, pb=pb, tg=tg: e.copy(stage[:, 1 + tg * G:1 + (tg + 1) * G], pb[:, 0:G])),
                        reads=[pb.res()], writes=[stage.res()])
            w0 = shw[:, jt, 0:1]; w1 = shw[:, jt, 1:2]; w2 = shw[:, jt, 2:3]; bb = shw[:, jt, 3:4]
            tmp = ftmp["tmp"]
            self.op("dve", (lambda e, w1=w1, bb=bb: e.tensor_scalar(tmp[:, 0:T], stage[:, 1:T + 1], w1, bb, ALU.mult, ALU.add)),
                    reads=[stage.res(), shw.res()], writes=[tmp.res()])
            self.op("dve", (lambda e, w0=w0: e.scalar_tensor_tensor(tmp[:, 0:T], stage[:, 0:T], w0, tmp[:, 0:T], ALU.mult, ALU.add)),
                    reads=[stage.res(), shw.res(), tmp.res()], writes=[tmp.res()])
            o_ap = cbuf[:, j, 0:T] if dst_is_tok else dst[:, j, 0:T]
            o_res = cbuf.res() if dst_is_tok else dst.res()
            self.op("dve", (lambda e, w2=w2, o_ap=o_ap: e.scalar_tensor_tensor(o_ap, stage[:, 2:T + 2], w2, tmp[:, 0:T], ALU.mult, ALU.add)),
                    reads=[stage.res(), shw.res(), tmp.res()], writes=[o_res])
        if dst_is_tok:
            for tt in range(T // 128):
                pb = self.next_ps()
                pbb = pb[:, 0:256].bitcast(BF16)
                for j in range(4):
                    self.op("pe", (lambda e, pbb=pbb, j=j, tt=tt: e.transpose(pbb[:, j * 128:(j + 1) * 128], cbuf[:, j, tt * 128:(tt + 1) * 128],
                                                                              self.ident_b[:, :])),
                            reads=[cbuf.res(), self.ident_b.res()], writes=[pb.res()])
                if tt % 2 == 0:
                    self.op("dve", (lambda e, pbb=pbb, tt=tt: e.tensor_copy(dst[:, tt, :], pbb[:, 0:512])), reads=[pb.res()], writes=[dst.res()])
                else:
                    self.op("act", (lambda e, pbb=pbb, tt=tt: e.copy(dst[:, tt, :], pbb[:, 0:512])), reads=[pb.res()], writes=[dst.res()])

    def hyena_fwd_mul(self, l, tag, oo, T, U, YH, fw, kst, ctmp):
        NT = T // 128
        cst = self.hc[tag]
        kh = self.khat[(l, tag)]
        it = 0
        for j in range(NT):
            ks = kst[j % 2]
            self.dma("sp", ks[:, :, :], kh[oo, j], writes=[ks.res()])
            pab = []
            for cs in range(2):
                f_ = fw[it % 2]
                it += 1
                self.dma("sp", f_[:, 0:NT, :], cst["fwd"][cs * NT + j], writes=[f_.res()])
                pb = self.next_ps(reserve=True)
                pab.append(pb)
                for c in range(NT):
                    self.op("pe", (lambda e, pb=pb, f_=f_, c=c: e.matmul(pb[:, :], f_[:, c, :], U[:, c, :], start=(c == 0), stop=(c == NT - 1))),
                            reads=[f_.res(), U.res()], writes=[pb.res()])
            self.release_ps(pab)
            pa, pbm = pab
            t0, t1, t2, t3 = (ctmp[:, i, :] for i in range(4))
            rc = ctmp.res()
            self.op("dve", (lambda e, pa=pa, ks=ks, t0=t0: e.tensor_tensor(t0, pa[:, :], ks[:, 0, :], ALU.mult)), reads=[pa.res(), ks.res()], writes=[rc])
            self.op("dve", (lambda e, pbm=pbm, ks=ks, t1=t1: e.tensor_tensor(t1, pbm[:, :], ks[:, 1, :], ALU.mult)), reads=[pbm.res(), ks.res()], writes=[rc])
            self.op("dve", (lambda e, pa=pa, ks=ks, t2=t2: e.tensor_tensor(t2, pa[:, :], ks[:, 1, :], ALU.mult)), reads=[pa.res(), ks.res()], writes=[rc])
            self.op("dve", (lambda e, pbm=pbm, ks=ks, t3=t3: e.tensor_tensor(t3, pbm[:, :], ks[:, 0, :], ALU.mult)), reads=[pbm.res(), ks.res()], writes=[rc])
            self.op("pool", (lambda e, j=j, t0=t0, t1=t1: e.tensor_tensor(YH[:, j, :], t0, t1, ALU.subtract)), reads=[rc], writes=[YH.res()])
            self.op("pool", (lambda e, j=j, t2=t2, t3=t3: e.tensor_tensor(YH[:, NT + j, :], t2, t3, ALU.add)), reads=[rc], writes=[YH.res()])

    def hyena_branch(self, l, hoff, T, tag, ab, out_view):
        self.mark("hyena_branch")
        din = self.din
        NT = T // 128
        NF = 2 * NT
        cst = self.hc[tag]
        o = ab
        VT = self.view(o, [128, NT, 512], BF16); o += NT * 1024
        X1 = View(out_view.t.rearrange("p a b -> p (a b)").rearrange("p (n c) -> p n c", c=512))
        X1._r = out_view._r
        YH = self.view(o, [128, NF, 512], BF16); yh0 = o; o += NF * 1024
        ctmp = self.view(o, [128, 4, 512], F32); ct0 = o; o += 8192
        shw = self.view(o, [128, 12, 4], F32); o += 192
        r1 = o
        Wh = self.view(r1, [128, 8, 512], BF16)
        stage = self.view(r1 + 8192, [128, T + 2], F32)
        tmp = self.view(ct0, [128, T], F32)
        tmp._r = ctmp._r
        cbuf = self.view(yh0, [128, 4, T], BF16)
        cbuf._r = YH._r
        ft = {"tmp": tmp, "cbuf": cbuf}
        o2 = r1
        fw = [self.view(o2 + i * NF * 256, [128, NF, 128], BF16) for i in range(2)]; o2 += 2 * NF * 256
        kst = [self.view(o2 + i * 4096, [128, 2, 512], F32) for i in range(2)]; o2 += 8192
        ivb = [self.view(r1 + i * 8 * min(512, T) * 2, [128, 8, min(512, T)], BF16) for i in range(2)]
        for k in range(3):
            self.dma("sp", shw[:, :, k], din["hy_short_w"][l][k].rearrange("(j p) -> p j", p=128), writes=[shw.res()])
        self.dma("sp", shw[:, :, 3], din["hy_short_b"][l].rearrange("(j p) -> p j", p=128), writes=[shw.res()])
        self.hyena_inproj_conv(l, 1024, hoff, T, Wh, shw, stage, ft, VT, True)
        self.hyena_inproj_conv(l, 0, hoff, T, Wh, shw, stage, ft, X1, True)
        self.barrier()
        self.hyena_fwd_mul(l, tag, 0, T, VT, YH, fw, kst, ctmp)
        for tt in range(NT):
            f_ = fw[tt % 2]
            self.dma("sp", f_[:, :, :], cst["inva"][tt], writes=[f_.res()])
            pb = self.next_ps()
            for c in range(NF):
                self.op("pe", (lambda e, pb=pb, f_=f_, c=c: e.matmul(pb[:, :], f_[:, c, :], YH[:, c, :], start=(c == 0), stop=(c == NF - 1))),
                        reads=[f_.res(), YH.res()], writes=[pb.res()])
            self.op("dve", (lambda e, pb=pb, tt=tt: e.tensor_tensor(X1[:, tt, :], pb[:, :], X1[:, tt, :], ALU.mult)),
                    reads=[pb.res(), X1.res()], writes=[X1.res()])
        self.hyena_fwd_mul(l, tag, 1, T, X1, YH, fw, kst, ctmp)
        self.barrier()
        X2 = View(VT.t.rearrange("p a b -> p (a b)").rearrange("p (j t) -> p j t", j=4))
        self.hyena_inproj_conv(l, 512, hoff, T, Wh, shw, stage, ft, X2, False)
        self.barrier()
        G = min(512, T)
        for tg in range(T // G):
            pj = [self.next_ps(reserve=True) for _ in range(4)]
            self.release_ps(pj)
            ngrp = (NF + 7) // 8
            for gq in range(ngrp):
                nq = min(8, NF - gq * 8)
                iv = ivb[(tg * ngrp + gq) % 2]
                self.dma("sp", iv[:, 0:nq, :], cst["invb"][tg][:, gq * 8:gq * 8 + nq, :], writes=[iv.res()])
                for j in range(4):
                    for q in range(nq):
                        c = gq * 8 + q
                        self.op("pe", (lambda e, pj=pj, j=j, q=q, c=c, iv=iv: e.matmul(pj[j][:, 0:G], YH[:, c, j * 128:(j + 1) * 128], iv[:, q, :],
                                                                                     start=(c == 0), stop=(c == NF - 1))),
                                reads=[iv.res(), YH.res()], writes=[pj[j].res()])
            for j in range(4):
                self.op("dve", (lambda e, pj=pj, j=j, tg=tg: e.tensor_tensor(out_view[:, j, tg * G:(tg + 1) * G], pj[j][:, 0:G],
                                                                            X2[:, j, tg * G:(tg + 1) * G], ALU.mult)),
                        reads=[pj[j].res(), X2.res()], writes=[out_view.res()])


def _dft_consts(T):
    import ml_dtypes
    n = 2 * T
    NT = T // 128
    t = np.arange(T, dtype=np.float64)[:, None]
    f = np.arange(T, dtype=np.float64)[None, :]
    ang = 2.0 * np.pi * (f + 0.5) * t / n
    fwd = np.concatenate([np.cos(ang), np.sin(ang)], axis=1)
    fwd_t = fwd.reshape(NT, 128, 2 * NT, 128).transpose(2, 1, 0, 3)
    inv = fwd.T
    inva = inv.reshape(2 * NT, 128, NT, 128).transpose(2, 1, 0, 3)
    G = min(512, T)
    invb = inv.reshape(2 * NT, 128, T // G, G).transpose(2, 1, 0, 3)
    bf = ml_dtypes.bfloat16
    tt_ = np.arange(T, dtype=np.float32)[:, None]
    tn = tt_ / max(T - 1, 1)
    bands = np.arange(1, 17, dtype=np.float32)
    a2 = tt_ * bands * np.float32(2.0 * math.pi / T)
    z = np.concatenate([tn, np.cos(a2), np.sin(a2)], axis=-1).astype(np.float32)
    return {"fwd": np.ascontiguousarray(fwd_t).astype(bf), "inva": np.ascontiguousarray(inva).astype(bf),
            "invb": np.ascontiguousarray(invb).astype(bf), "z": np.ascontiguousarray(z.T),
            "tn": np.ascontiguousarray(tn.reshape(NT, 128).T)}


_CONSTS = {}


def _consts():
    if not _CONSTS:
        for tag, T in (("L", L), ("C", LC)):
            for k_, v_ in _dft_consts(T).items():
                _CONSTS["hc_%s_%s" % (tag, k_)] = v_
    return _CONSTS


def _shapes(inputs):
    shp = {n: tuple(inputs[n].shape) for n in INPUT_NAMES}
    shp["x"] = (NB, L, D)
    shp["c"] = (NB, D)
    shp["ctx"] = (NB, LC, D)
    return shp


def run(inputs, debug=None, cores=8):
    inputs = {k: np.ascontiguousarray(np.asarray(v, dtype=np.float32)) for k, v in inputs.items()}
    k = K(_shapes(inputs), debug=debug)
    nc = k.build()
    in_maps = []
    for ci in range(cores):
        m = {n: inputs[n] for n in INPUT_NAMES}
        m["x"] = inputs["x"][ci * NB:(ci + 1) * NB]
        m["c"] = inputs["c"][ci * NB:(ci + 1) * NB]
        m["ctx"] = inputs["ctx"][ci * NB:(ci + 1) * NB]
        m.update(_consts())
        in_maps.append(m)
    res = run_bass_kernel_spmd(nc, in_maps, core_ids=list(range(cores)))
    return res


def kernel(**inputs):
    res = run(inputs)
    return np.concatenate([r["out"] for r in res.results], axis=0)
```
